# Optimizing a Trainium2 kernel written in Bass

```python
import jax
import jax.numpy as jnp
from jax import lax
import numpy as np

D_MODEL = 1024
BATCH = 8
SEQ = 2048
DEPTH = 2

GRID_W = 64
CTX_LEN = 256

A_HEADS = 8
A_HEAD_DIM = 64
D_A = A_HEADS * A_HEAD_DIM
LORA_W = 64
LORA_A = 64
LORA_G = 128

B_HEADS = 8
B_HEAD_DIM = 64
D_B = B_HEADS * B_HEAD_DIM
WIN_H = 8
WIN_W = 16

N_EXPERTS = 16
N_GROUPS = 4
EXPERTS_PER_GROUP = N_EXPERTS // N_GROUPS
TOP_K = 2
D_EXPERT = 512

RMS_EPS = 1e-6
GN_EPS = 64e-5

IN_SPLITS = (D_A, D_A, D_A, 2 * LORA_W, 2 * LORA_A, LORA_G, D_B, D_B, D_B, D_MODEL, D_MODEL)
IN_COLS = sum(IN_SPLITS)
IN_OFFSETS = tuple(int(o) for o in np.cumsum(IN_SPLITS)[:-1])

kernel_name = 'hybrid_rwkv7_natten_moe_dit'


def rms_norm(x, g):
    xf = x.astype(jnp.float32)
    y = xf * lax.rsqrt(jnp.mean(xf * xf, axis=-1, keepdims=True) + RMS_EPS)
    return (y * g.astype(jnp.float32)).astype(x.dtype)


def modulate(h, shift, scale):
    return h * (1.0 + scale) + shift


def split_cols(z):
    return jnp.split(z, IN_OFFSETS, axis=-1)


def rwkv_prep(z, w0, w_lora_b, a0, a_lora_b, k_k, k_a):
    f = jnp.float32
    k, v, lw, la = z[1].astype(f), z[2].astype(f), z[3].astype(f), z[4].astype(f)
    bsz, t = k.shape[:2]
    heads = lambda u: u.reshape(bsz, t, A_HEADS, A_HEAD_DIM)
    lw = jnp.tanh(lw).reshape(bsz, t, 2, LORA_W)
    wl = w0.astype(f) + jnp.einsum('btzr,zrc->btzc', lw, w_lora_b.astype(f))
    decay = jnp.exp(-jnp.exp(-jax.nn.softplus(-wl) - 0.5))
    la = la.reshape(bsz, t, 2, LORA_A)
    a = jax.nn.sigmoid(a0.astype(f) + jnp.einsum('btzr,zrc->btzc', la, a_lora_b.astype(f)))
    kk = heads(k * k_k.astype(f))
    kk = kk * lax.rsqrt(jnp.maximum(jnp.sum(kk * kk, axis=-1, keepdims=True), 1e-12))
    k_dir = k[:, :, None, :] * (1.0 + (a - 1.0) * k_a.astype(f))
    per_dir = [(heads(decay[:, :, d]), heads(k_dir[:, :, d]), heads(a[:, :, d])) for d in range(2)]
    return heads(k), heads(v), kk, per_dir


def rwkv_scan(s0, r, decay, k, v, kk, a, reverse, emit):
    def step(s, inp):
        w_t, k_t, v_t, kk_t, a_t = inp[:5]
        s_kk = jnp.einsum('bhvk,bhk->bhv', s, kk_t)
        s = (s * w_t[:, :, None, :]
             - s_kk[..., None] * (kk_t * a_t)[:, :, None, :]
             + v_t[..., None] * k_t[:, :, None, :])
        y = jnp.einsum('bhvk,bhk->bhv', s, inp[5]) if emit else None
        return s, y
    seq = (decay, k, v, kk, a) + ((r,) if emit else ())
    xs = tuple(jnp.moveaxis(u, 1, 0) for u in seq)
    s_final, ys = lax.scan(step, s0, xs, reverse=reverse)
    return s_final, (jnp.moveaxis(ys, 0, 1) if emit else None)


def rwkv_out(y, r, k, v, lg, r_k, g_lora_b, ln_g, ln_b, out_dtype):
    f = jnp.float32
    bsz, t = y.shape[:2]
    mu = jnp.mean(y, axis=-1, keepdims=True)
    var = jnp.mean(jnp.square(y - mu), axis=-1, keepdims=True)
    yn = ((y - mu) * lax.rsqrt(var + GN_EPS)).reshape(bsz, t, D_A) * ln_g.astype(f) + ln_b.astype(f)
    bonus = (jnp.sum(r * k * r_k.astype(f), axis=-1, keepdims=True) * v).reshape(bsz, t, D_A)
    g = jax.nn.sigmoid(lg.astype(f)) @ g_lora_b.astype(f)
    return ((yn + bonus) * g).astype(out_dtype)


def rwkv_mixer(z_c, z_x, w0, w_lora_b, a0, a_lora_b, g_lora_b, k_k, k_a, r_k, ln_g, ln_b, emit_ctx):
    f = jnp.float32
    k_c, v_c, kk_c, dir_c = rwkv_prep(z_c, w0, w_lora_b, a0, a_lora_b, k_k, k_a)
    k_x, v_x, kk_x, dir_x = rwkv_prep(z_x, w0, w_lora_b, a0, a_lora_b, k_k, k_a)
    bsz, seq = z_x[0].shape[:2]
    r_x = z_x[0].astype(f).reshape(bsz, seq, A_HEADS, A_HEAD_DIM)
    r_c = z_c[0].astype(f).reshape(bsz, z_c[0].shape[1], A_HEADS, A_HEAD_DIM) if emit_ctx else None
    y_x, y_c = 0.0, 0.0
    for d in range(2):
        rev = d == 1
        s0 = jnp.zeros((bsz, A_HEADS, A_HEAD_DIM, A_HEAD_DIM), f)
        w_c, kd_c, a_c = dir_c[d]
        s_c, yc_d = rwkv_scan(s0, r_c, w_c, kd_c, v_c, kk_c, a_c, rev, emit_ctx)
        w_x, kd_x, a_x = dir_x[d]
        _, yx_d = rwkv_scan(s_c, r_x, w_x, kd_x, v_x, kk_x, a_x, rev, True)
        y_x = y_x + yx_d
        if emit_ctx:
            y_c = y_c + yc_d
    out_x = rwkv_out(y_x, r_x, k_x, v_x, z_x[5], r_k, g_lora_b, ln_g, ln_b, z_x[0].dtype)
    out_c = rwkv_out(y_c, r_c, k_c, v_c, z_c[5], r_k, g_lora_b, ln_g, ln_b, z_c[0].dtype) if emit_ctx else None
    return out_c, out_x


def na_mixer(z_c, z_x, q_g, k_g, rpb, need_ctx):
    f = jnp.float32
    scale = B_HEAD_DIM ** -0.5

    def heads(u):
        bsz, t = u.shape[:2]
        return u.reshape(bsz, t, B_HEADS, B_HEAD_DIM).transpose(0, 2, 1, 3)

    k_c, v_c = rms_norm(heads(z_c[7]), k_g), heads(z_c[8])
    q_x = rms_norm(heads(z_x[6]), q_g) * scale
    k_x, v_x = rms_norm(heads(z_x[7]), k_g), heads(z_x[8])
    bsz, _, seq, _ = q_x.shape
    rows = seq // GRID_W
    kh = min(WIN_H, rows)
    kw = WIN_W
    grid = lambda u: u.reshape(bsz, B_HEADS, rows, GRID_W, B_HEAD_DIM)
    q_grid, k_grid, v_grid = grid(q_x), grid(k_x), grid(v_x)

    i = jnp.arange(rows)
    key_rows = jnp.clip(i - kh // 2, 0, rows - kh)[:, None] + jnp.arange(kh)[None, :]
    k_blk = k_grid[:, :, key_rows]
    v_blk = v_grid[:, :, key_rows]
    s_lat = jnp.einsum('bhiqd,bhiakd->bhiqak', q_grid, k_blk).astype(f)

    j = jnp.arange(GRID_W)
    col_start = jnp.clip(j - kw // 2, 0, GRID_W - kw)
    in_win = (j[None, :] >= col_start[:, None]) & (j[None, :] < col_start[:, None] + kw)
    d_row = key_rows - i[:, None] + (WIN_H - 1)
    d_col = jnp.clip(j[None, :] - j[:, None], -(WIN_W - 1), WIN_W - 1) + (WIN_W - 1)
    bias = rpb[:, d_row[:, None, :, None], d_col[None, :, None, :]].astype(f)
    s_lat = jnp.where(in_win[:, None, :], s_lat + bias[None], -jnp.inf)

    s_ctx = jnp.einsum('bhiqd,bhcd->bhiqc', q_grid, k_c).astype(f)
    n_lat = kh * GRID_W
    logits = jnp.concatenate([s_lat.reshape(bsz, B_HEADS, rows, GRID_W, n_lat), s_ctx], axis=-1)
    p = jax.nn.softmax(logits, axis=-1).astype(v_x.dtype)
    p_lat = p[..., :n_lat].reshape(bsz, B_HEADS, rows, GRID_W, kh, GRID_W)
    o = (jnp.einsum('bhiqak,bhiakd->bhiqd', p_lat, v_blk)
         + jnp.einsum('bhiqc,bhcd->bhiqd', p[..., n_lat:], v_c))
    y_x = o.transpose(0, 2, 3, 1, 4).reshape(bsz, seq, D_B)

    y_c = None
    if need_ctx:
        q_c = rms_norm(heads(z_c[6]), q_g) * scale
        p_c = jax.nn.softmax(jnp.einsum('bhqd,bhkd->bhqk', q_c, k_c).astype(f), axis=-1).astype(v_c.dtype)
        y_c = jnp.einsum('bhqk,bhkd->bhqd', p_c, v_c).transpose(0, 2, 1, 3).reshape(bsz, -1, D_B)
    return y_c, y_x


def branch_merge(gate_a, gate_b, y_a, y_b, proj_a, proj_b, w_out):
    merged = jax.nn.sigmoid(gate_a) * (y_a @ proj_a) + jax.nn.sigmoid(gate_b) * (y_b @ proj_b)
    return merged @ w_out


def moe(h, router_w, router_bias, w1, w3, w2):
    f = jnp.float32
    shp = h.shape
    t = h.reshape(-1, shp[-1])
    n = t.shape[0]
    scores = jax.nn.sigmoid(t.astype(f) @ router_w.astype(f))
    sel = scores + router_bias.astype(f)
    grp_score = lax.top_k(sel.reshape(n, N_GROUPS, EXPERTS_PER_GROUP), TOP_K)[0].sum(-1)
    best = jnp.argmax(grp_score, axis=-1)
    in_group = (jnp.arange(N_EXPERTS) // EXPERTS_PER_GROUP)[None, :] == best[:, None]
    _, top_idx = lax.top_k(jnp.where(in_group, sel, -jnp.inf), TOP_K)
    top_s = jnp.take_along_axis(scores, top_idx, axis=-1)
    wts = top_s / jnp.sum(top_s, axis=-1, keepdims=True)
    gate = jnp.sum(jax.nn.one_hot(top_idx, N_EXPERTS, dtype=f) * wts[..., None], axis=1).astype(t.dtype)
    out = jnp.zeros_like(t)
    for e in range(N_EXPERTS):
        hid = jax.nn.silu(t @ w1[e]) * (t @ w3[e])
        out = out + gate[:, e:e + 1] * (hid @ w2[e])
    return out.reshape(shp)


def setup_inputs(seed: int = 0) -> dict:
    key = jax.random.key(seed)
    ks = iter(jax.random.split(key, 40))
    D = D_MODEL

    def nrm(shape, s):
        return jax.random.normal(next(ks), shape, jnp.float32) * s

    inp = {}
    inp['x'] = nrm((BATCH, SEQ, D), 1.0)
    inp['c'] = nrm((BATCH, D), 1.0)
    inp['ctx'] = nrm((BATCH, CTX_LEN, D), 1.0)
    inp['c_ctx'] = nrm((D,), 1.0)
    inp['mod_w'] = nrm((DEPTH, D, 6 * D), 0.5 * D ** -0.5)
    inp['mod_b'] = nrm((DEPTH, 6 * D), 0.02)
    inp['norm1_g'] = 1.0 + nrm((DEPTH, D), 0.05)
    inp['norm2_g'] = 1.0 + nrm((DEPTH, D), 0.05)
    inp['w_in'] = nrm((DEPTH, D, IN_COLS), D ** -0.5)
    inp['rw_w0'] = jax.random.uniform(next(ks), (DEPTH, 2, D_A), jnp.float32, -6.0, 1.0)
    inp['rw_w_lora_b'] = nrm((DEPTH, 2, LORA_W, D_A), 0.5 * LORA_W ** -0.5)
    inp['rw_a0'] = nrm((DEPTH, 2, D_A), 0.5)
    inp['rw_a_lora_b'] = nrm((DEPTH, 2, LORA_A, D_A), 0.5 * LORA_A ** -0.5)
    inp['rw_g_lora_b'] = nrm((DEPTH, LORA_G, D_A), LORA_G ** -0.5)
    inp['rw_k_k'] = 0.85 + nrm((DEPTH, D_A), 0.05)
    inp['rw_k_a'] = 1.0 + nrm((DEPTH, D_A), 0.05)
    inp['rw_r_k'] = nrm((DEPTH, A_HEADS, A_HEAD_DIM), 0.1)
    inp['rw_ln_g'] = 1.0 + nrm((DEPTH, D_A), 0.05)
    inp['rw_ln_b'] = nrm((DEPTH, D_A), 0.02)
    inp['na_q_g'] = 1.0 + nrm((DEPTH, B_HEAD_DIM), 0.1)
    inp['na_k_g'] = 1.0 + nrm((DEPTH, B_HEAD_DIM), 0.1)
    inp['na_rpb'] = nrm((DEPTH, B_HEADS, 2 * WIN_H - 1, 2 * WIN_W - 1), 0.5)
    inp['proj_a'] = nrm((DEPTH, D_A, D), D_A ** -0.5)
    inp['proj_b'] = nrm((DEPTH, D_B, D), D_B ** -0.5)
    inp['w_out'] = nrm((DEPTH, D, D), D ** -0.5)
    inp['router_w'] = nrm((D, N_EXPERTS), D ** -0.5)
    inp['router_bias'] = nrm((N_EXPERTS,), 0.01)
    inp['moe_w1'] = nrm((DEPTH, N_EXPERTS, D, D_EXPERT), D ** -0.5)
    inp['moe_w3'] = nrm((DEPTH, N_EXPERTS, D, D_EXPERT), D ** -0.5)
    inp['moe_w2'] = nrm((DEPTH, N_EXPERTS, D_EXPERT, D), D_EXPERT ** -0.5)
    return inp


def reference(x, c, ctx, c_ctx, mod_w, mod_b, norm1_g, norm2_g, w_in,
              rw_w0, rw_w_lora_b, rw_a0, rw_a_lora_b, rw_g_lora_b, rw_k_k, rw_k_a, rw_r_k,
              rw_ln_g, rw_ln_b, na_q_g, na_k_g, na_rpb, proj_a, proj_b, w_out,
              router_w, router_bias, moe_w1, moe_w3, moe_w2):
    for l in range(DEPTH):
        last = l == DEPTH - 1
        m_x = [m[:, None, :] for m in jnp.split(jax.nn.silu(c) @ mod_w[l] + mod_b[l], 6, axis=-1)]
        m_c = jnp.split(jax.nn.silu(c_ctx) @ mod_w[l] + mod_b[l], 6, axis=-1)

        h_x = modulate(rms_norm(x, norm1_g[l]), m_x[0], m_x[1])
        h_c = modulate(rms_norm(ctx, norm1_g[l]), m_c[0], m_c[1])
        z_x = split_cols(h_x @ w_in[l])
        z_c = split_cols(h_c @ w_in[l])
        ya_c, ya_x = rwkv_mixer(z_c, z_x, rw_w0[l], rw_w_lora_b[l], rw_a0[l], rw_a_lora_b[l],
                                rw_g_lora_b[l], rw_k_k[l], rw_k_a[l], rw_r_k[l], rw_ln_g[l], rw_ln_b[l],
                                emit_ctx=not last)
        yb_c, yb_x = na_mixer(z_c, z_x, na_q_g[l], na_k_g[l], na_rpb[l], need_ctx=not last)
        x = x + m_x[2] * branch_merge(z_x[9], z_x[10], ya_x, yb_x, proj_a[l], proj_b[l], w_out[l])

        h2_x = modulate(rms_norm(x, norm2_g[l]), m_x[3], m_x[4])
        x = x + m_x[5] * moe(h2_x, router_w, router_bias, moe_w1[l], moe_w3[l], moe_w2[l])

        if not last:
            ctx = ctx + m_c[2] * branch_merge(z_c[9], z_c[10], ya_c, yb_c, proj_a[l], proj_b[l], w_out[l])
            h2_c = modulate(rms_norm(ctx, norm2_g[l]), m_c[3], m_c[4])
            ctx = ctx + m_c[5] * moe(h2_c, router_w, router_bias, moe_w1[l], moe_w3[l], moe_w2[l])
    return x
```

```python
import numpy as np
import concourse.bass as bass
import concourse.mybir as mybir
from concourse.bass_utils import run_bass_kernel_spmd
from contextlib import ExitStack

F32 = mybir.dt.float32
BF16 = mybir.dt.bfloat16
ALU = mybir.AluOpType
AF = mybir.ActivationFunctionType
AX = mybir.AxisListType

L = 2
NT = 2304
NCTX = 256
NCH = 36
BLKS = [(0, 256), (256, 512), (768, 512), (1280, 512), (1792, 512)]
RMS_EPS = 1e-6
GN_EPS = 64e-5
DEC_C = -0.6065306597126334
NEG = -30000.0

SEM_LIMIT = 30000
NDMA = 16
NSW = 12
import os as _os0
ELT_ENG = _os0.environ.get('ELT_ENG', 'dve')
SW_CLEAR = False


class Res:
    __slots__ = ("lw", "rd", "grp")

    def __init__(self):
        self.lw = None
        self.rd = {}
        self.grp = None


class PEProxy:
    def __init__(self, prog):
        self.P = prog
        self.pe = prog.nc.tensor
        self.cur_w = None

    def _chk(self, k_ap):
        grp = (k_ap.base_partition(), k_ap.partition_size())
        for x in self.cur_w:
            if x.grp is not None and x.grp != grp and not x.rd:
                self.P.fence_pe()
            x.grp = grp

    def matmul(self, out, lhsT, rhs, **kw):
        self._chk(lhsT)
        return self.pe.matmul(out, lhsT=lhsT, rhs=rhs, **kw)

    def transpose(self, out, in_, identity):
        self._chk(in_)
        return self.pe.transpose(out, in_, identity)


class Tl:
    def __init__(self, t):
        self.t = t
        self.r = Res()


class Prog:
    def __init__(self, nc, stack):
        self.nc = nc
        self.stack = stack
        self.engs = {"pe": nc.tensor, "dve": nc.vector, "act": nc.scalar,
                     "pool": nc.gpsimd, "sp": nc.sync}
        self.nsem = 0
        self.sem = {k: self._newsem(k) for k in self.engs}
        self.cnt = {k: 0 for k in self.engs}
        self.known = {k: {} for k in self.engs}
        self.dma_sems = [self._newsem("dma") for _ in range(NDMA)]
        self.dma_cnt = [0] * NDMA
        self.dma_next = 0
        self.n_ins = 0
        self.n_wait = 0
        self.last_ev = {}
        self.pep = PEProxy(self)

    def _newsem(self, name):
        self.nsem += 1
        return self.stack.enter_context(self.nc.semaphore(f"{name}_{self.nsem}"))

    def _learn(self, eng, sem, val, snap):
        kn = self.known[eng]
        k = id(sem)
        if kn.get(k, 0) < val:
            kn[k] = val
        for k2, v2 in snap.items():
            if kn.get(k2, 0) < v2:
                kn[k2] = v2

    def _wait(self, eng, deps):
        e = self.engs[eng]
        kn = self.known[eng]
        todo = [d for d in deps if d is not None and not (d[2] == "pe" and eng == "pe")]
        todo.sort(key=lambda d: -d[1])
        for (sem, val, src, snap) in todo:
            if kn.get(id(sem), 0) >= val:
                continue
            e.wait_ge(sem, val)
            self.n_wait += 1
            self._learn(eng, sem, val, snap)

    @staticmethod
    def _deps(r, w):
        deps = []
        for x in r:
            deps.append(x.lw)
        for x in w:
            deps.append(x.lw)
            deps.extend(x.rd.values())
        return deps

    def op(self, eng, fn, r=(), w=()):
        r = [x.r if isinstance(x, Tl) else x for x in r]
        w = [x.r if isinstance(x, Tl) else x for x in w]
        self._wait(eng, self._deps(r, w))
        if eng == "pe":
            self.pep.cur_w = w
            ins = fn(self.pep)
        else:
            ins = fn(self.engs[eng])
        if self.cnt[eng] >= SEM_LIMIT:
            self.sem[eng] = self._newsem(eng)
            self.cnt[eng] = 0
        self.cnt[eng] += 1
        ins.then_inc(self.sem[eng], 1)
        ev = (self.sem[eng], self.cnt[eng], eng, dict(self.known[eng]))
        self.last_ev[eng] = ev
        for x in r:
            x.rd[eng] = ev
        for x in w:
            x.lw = ev
            x.rd = {}
        self.n_ins += 1
        return ins

    def dma(self, q, out, in_, r=(), w=(), **kw):
        r = [x.r if isinstance(x, Tl) else x for x in r]
        w = [x.r if isinstance(x, Tl) else x for x in w]
        deps = self._deps(r, w)
        i = self.dma_next
        self.dma_next = (i + 1) % NDMA
        sem = self.dma_sems[i]
        if self.dma_cnt[i] > 0:
            deps.append((sem, self.dma_cnt[i], "dma", {}))
        self._wait(q, deps)
        ins = self.engs[q].dma_start(out=out, in_=in_, **kw)
        self.dma_cnt[i] += 16
        ins.then_inc(sem, 16)
        ev = (sem, self.dma_cnt[i], "dma", dict(self.known[q]))
        key = ("dma", i)
        for x in r:
            x.rd[key] = ev
        for x in w:
            x.lw = ev
            x.rd = {}
        self.n_ins += 1
        return ins

    def fence_pe(self):
        ev = self.last_ev.get("pe")
        if ev is not None:
            sem, val, _, snap = ev
            self.engs["pe"].wait_ge(sem, val)
            self._learn("pe", sem, val, snap)
            self.n_wait += 1

    def barrier(self):
        evs = list(self.last_ev.values())
        for i in range(NDMA):
            if self.dma_cnt[i] > 0:
                evs.append((self.dma_sems[i], self.dma_cnt[i], "dma", {}))
        for e in self.engs:
            self._wait(e, [x for x in evs if x[2] != e])

    def finish(self, res_list):
        deps = [x.r.lw if isinstance(x, Tl) else x.lw for x in res_list]
        self._wait("sp", deps)


def _cm(a, K):
    a = np.asarray(a)
    return np.ascontiguousarray(a.reshape(K, 128, -1).transpose(1, 0, 2))


def _consts():
    c = {}
    s = np.arange(64)[:, None]
    t = np.arange(64)[None, :]
    mc = np.zeros((64, 2, 128), np.float32)
    mc[:, 0, :64] = (s < t)
    mc[:, 0, 64:] = (s <= t)
    mc[:, 1, :64] = (s > t)
    mc[:, 1, 64:] = (s >= t)
    c["c_maskc"] = mc
    ma = np.zeros((64, 2, 64), np.float32)
    ma[:, 0, :] = (t < s)
    ma[:, 1, :] = (t > s)
    c["c_maska"] = ma
    c["c_maska2"] = np.ascontiguousarray(ma.transpose(1, 0, 2).reshape(128, 64))
    c["c_ident"] = np.eye(128, dtype=np.float32)
    rm = np.ones((128, 512), np.float32)
    rm[:, ::64] = 0.0
    c["c_rmask"] = rm
    bo = np.zeros((128, 128), np.float32)
    bo[:64, :64] = 1.0
    bo[64:, 64:] = 1.0
    c["c_bones"] = bo
    c["c_ones"] = np.ones((128, 128), np.float32)
    sel = np.zeros((16, 16, 128), np.float32)
    for e in range(16):
        sel[e, e, :] = 1.0
    c["c_sel16"] = sel
    jk = np.arange(64)[:, None]
    jq = np.arange(64)[None, :]
    cs = np.clip(jq - 8, 0, 48)
    inwin = (jk >= cs) & (jk < cs + 16)
    nm = np.where(inwin, 0.0, NEG).astype(np.float32)
    c["c_namask"] = np.ascontiguousarray(np.concatenate([nm, nm], 0))
    return c


def _prep(inp):
    g = {k: np.asarray(v) for k, v in inp.items()}
    sh = {}
    sh["modw"] = np.ascontiguousarray(g["mod_w"].reshape(L, 8, 128, 6, 1024).transpose(0, 3, 2, 1, 4))
    sh["modb"] = np.ascontiguousarray(g["mod_b"].reshape(L, 48, 128).transpose(0, 2, 1))
    sh["g12"] = np.ascontiguousarray(
        np.stack([g["norm1_g"], g["norm2_g"]], 1).reshape(L, 2, 8, 128).transpose(0, 3, 1, 2))
    Wc = np.stack([_cm(g["w_in"][l], 8) for l in range(L)])
    sh["w_lx"] = np.ascontiguousarray(Wc[:, :, :, 1536:1920])
    rkv = np.zeros((L, 8, 128, 8, 320), np.float32)
    wqk = np.zeros((L, 8, 128, 8, 128), np.float32)
    for h in range(8):
        r_ = Wc[:, :, :, h * 64:(h + 1) * 64]
        k_ = Wc[:, :, :, 512 + h * 64:512 + (h + 1) * 64]
        v_ = Wc[:, :, :, 1024 + h * 64:1024 + (h + 1) * 64]
        rkv[:, h] = np.concatenate([r_, r_, k_, k_, v_], -1)
        wqk[:, h] = np.concatenate([Wc[:, :, :, 1920 + h * 64:1920 + (h + 1) * 64],
                                    Wc[:, :, :, 2432 + h * 64:2432 + (h + 1) * 64]], -1)
    sh["w_rkv"] = rkv
    sh["w_qk"] = wqk
    sh["w_vb"] = np.ascontiguousarray(Wc[:, :, :, 2944:3456])
    sh["w_g"] = np.ascontiguousarray(Wc[:, :, :, 3456:5504])
    wlb = np.zeros((L, 8, 128, 128), np.float32)
    alb = np.zeros((L, 8, 128, 128), np.float32)
    rwp = np.zeros((L, 128, 8, 7), np.float32)
    for h in range(8):
        hs = slice(h * 64, (h + 1) * 64)
        for d in range(2):
            ds = slice(d * 64, (d + 1) * 64)
            wlb[:, h, ds, ds] = g["rw_w_lora_b"][:, d, :, hs]
            alb[:, h, ds, ds] = g["rw_a_lora_b"][:, d, :, hs]
            rwp[:, ds, h, 0] = g["rw_w0"][:, d, hs]
            rwp[:, ds, h, 1] = g["rw_a0"][:, d, hs]
            rwp[:, ds, h, 2] = g["rw_k_k"][:, hs]
            rwp[:, ds, h, 3] = g["rw_k_a"][:, hs]
            rwp[:, ds, h, 4] = g["rw_r_k"][:, h, :]
            rwp[:, ds, h, 5] = g["rw_ln_g"][:, hs]
            rwp[:, ds, h, 6] = g["rw_ln_b"][:, hs]
    sh["wlb"] = wlb
    sh["alb"] = alb
    sh["rwp"] = rwp
    sh["glb"] = np.ascontiguousarray(g["rw_g_lora_b"])
    sh["nap"] = np.ascontiguousarray(np.stack([g["na_q_g"], g["na_k_g"]], -1))
    jk = np.arange(64)[:, None]
    jq = np.arange(64)[None, :]
    dcol = np.clip(jk - jq, -15, 15) + 15
    rp = g["na_rpb"][:, :, :, dcol]
    sh["rpbT"] = np.ascontiguousarray(rp.transpose(0, 1, 3, 2, 4))
    sh["pa"] = np.stack([_cm(g["proj_a"][l], 4) for l in range(L)])
    sh["pb"] = np.stack([_cm(g["proj_b"][l], 4) for l in range(L)])
    sh["wo"] = np.stack([_cm(g["w_out"][l], 8) for l in range(L)])
    sh["rw"] = _cm(g["router_w"], 8)
    sh["rbias"] = np.ascontiguousarray(np.broadcast_to(g["router_bias"][None, :], (128, 16)))
    sh["w1"] = np.ascontiguousarray(g["moe_w1"].reshape(L, 16, 8, 128, 512).transpose(0, 1, 3, 2, 4))
    sh["w3"] = np.ascontiguousarray(g["moe_w3"].reshape(L, 16, 8, 128, 512).transpose(0, 1, 3, 2, 4))
    sh["w2"] = np.ascontiguousarray(g["moe_w2"].reshape(L, 16, 4, 128, 1024).transpose(0, 1, 3, 2, 4))
    sh.update(_consts())
    maps = []
    for b in range(8):
        m = dict(sh)
        cat = np.concatenate([g["ctx"][b], g["x"][b]], 0)
        m["xin"] = _cm(np.ascontiguousarray(cat.T), 8)
        m["cs"] = _cm(np.stack([g["c"][b], g["c_ctx"]], -1), 8)
        maps.append(m)
    return maps


SHAPES = {
    "xin": [128, 8, NT], "cs": [128, 8, 2],
    "modw": [L, 6, 128, 8, 1024], "modb": [L, 128, 48], "g12": [L, 128, 2, 8],
    "w_lx": [L, 128, 8, 384], "w_rkv": [L, 8, 128, 8, 320], "w_qk": [L, 8, 128, 8, 128],
    "w_vb": [L, 128, 8, 512], "w_g": [L, 128, 8, 2048],
    "wlb": [L, 8, 128, 128], "alb": [L, 8, 128, 128], "rwp": [L, 128, 8, 7], "glb": [L, 128, 512],
    "nap": [L, 64, 2], "rpbT": [L, 8, 64, 15, 64],
    "pa": [L, 128, 4, 1024], "pb": [L, 128, 4, 1024], "wo": [L, 128, 8, 1024],
    "rw": [128, 8, 16], "rbias": [128, 16],
    "w1": [L, 16, 128, 8, 512], "w3": [L, 16, 128, 8, 512], "w2": [L, 16, 128, 4, 1024],
    "c_maskc": [64, 2, 128], "c_maska": [64, 2, 64], "c_maska2": [128, 64], "c_ident": [128, 128], "c_rmask": [128, 512],
    "c_bones": [128, 128], "c_ones": [128, 128], "c_sel16": [16, 16, 128], "c_namask": [128, 64],
}


class Ctx:
    pass


def build(n_layers=L, dbg=(), skip=()):
    nc = bass.Bass("TRN2", target_bir_lowering=False)
    D = {k: nc.dram_tensor(k, s, F32, kind="ExternalInput").ap() for k, s in SHAPES.items()}
    yout = nc.dram_tensor("yout", [128, 8, NT - NCTX], F32, kind="ExternalOutput").ap()
    xres = nc.dram_tensor("xres", [128, 8, NT], F32, kind="Internal").ap()
    dbg_out = {}
    for name in dbg:
        dbg_out[name] = nc.dram_tensor("dbg_" + name, [128, 8, NT], F32, kind="ExternalOutput").ap()
    K = Ctx()
    K.nc, K.D, K.dbg = nc, D, dbg_out
    K.skip = skip
    with ExitStack() as st:
        P = Prog(nc, st)
        K.P = P

        K.nsb = 0

        def sb(stack, name, shape, dt):
            K.nsb += 1
            return Tl(stack.enter_context(nc.sbuf_tensor(f"s{K.nsb}_{name}", shape, dt)))

        K.sb = sb
        K.PS = [Tl(st.enter_context(nc.psum_tensor(f"ps{i}", [128, 512], F32))) for i in range(7)]
        K.PSB = Tl(st.enter_context(nc.psum_tensor("psb", [128, 1024], BF16)))
        K.ps_i = 0

        def ps():
            t = K.PS[K.ps_i % 5]
            K.ps_i += 1
            return t

        K.ps = ps
        K.PSL = K.PS[6]
        K.PSL2 = K.PS[5]
        K.identf = sb(st, "identf", [128, 128], F32)
        K.identb = sb(st, "identb", [128, 128], BF16)
        K.onesb = sb(st, "onesb", [128, 128], BF16)
        K.onesf = sb(st, "onesf", [128, 128], F32)
        K.bones = sb(st, "bones", [128, 128], F32)
        K.maskc = sb(st, "maskc", [64, 2, 128], F32)
        K.maska = sb(st, "maska", [64, 2, 64], F32)
        K.maska2 = sb(st, "maska2", [128, 64], F32)
        K.rmask = sb(st, "rmask", [128, 512], F32)
        K.namask = sb(st, "namask", [128, 64], F32)
        K.rwt = sb(st, "rwt", [128, 8, 16], F32)
        K.rbias = sb(st, "rbias", [128, 16], F32)
        K.cs = sb(st, "cs", [128, 8, 2], F32)
        K.csb = sb(st, "csb", [128, 8, 2], BF16)
        for t, nm in [(K.identf, "c_ident"), (K.onesf, "c_ones"), (K.bones, "c_bones"), (K.maskc, "c_maskc"),
                      (K.maska, "c_maska"), (K.maska2, "c_maska2"), (K.rmask, "c_rmask"), (K.namask, "c_namask"),
                      (K.rwt, "rw"), (K.rbias, "rbias"), (K.cs, "cs")]:
            P.dma("sp", t.t[:], D[nm], w=[t])
        P.dma("pool", K.identb.t[:], D["c_ident"], w=[K.identb])
        P.dma("pool", K.onesb.t[:], D["c_ones"], w=[K.onesb])
        K.maskcb = sb(st, "maskcb", [64, 2, 128], BF16)
        K.maskab = sb(st, "maskab", [64, 2, 64], BF16)
        P.dma("pool", K.maskcb.t[:], D["c_maskc"], w=[K.maskcb])
        P.dma("pool", K.maskab.t[:], D["c_maska"], w=[K.maskab])
        P.op("act", lambda e: e.activation(out=K.csb.t[:], in_=K.cs.t[:], func=AF.Silu), r=[K.cs], w=[K.csb])
        K.mt = sb(st, "mt", [128, 48, 2], F32)
        K.mul1 = sb(st, "mul1", [128, 8, 2], F32)
        K.mul2 = sb(st, "mul2", [128, 8, 2], F32)
        K.g12 = sb(st, "g12", [128, 2, 8], F32)
        K.modb = sb(st, "modb", [128, 48], F32)
        K.HT = sb(st, "HT", [128, 8, NT], BF16)

        for l in range(n_layers):
            K.l = l
            with ExitStack() as ph:
                XT = sb(ph, "XT", [128, 8, NT], F32)
                P.dma("sp", XT.t[:], D["xin"] if l == 0 else xres, w=[XT])
                phase_adaln(K, ph)
                phase_norm(K, ph, XT, K.mul1, 0, None)
                if "h%d" % l in dbg_out:
                    dump_bf(K, ph, K.HT, dbg_out["h%d" % l])
                if l == 0:
                    P.dma("sp", xres, XT.t[:], r=[XT], w=[Res()])
                P.barrier()
            with ExitStack() as ph:
                YA = sb(ph, "YA", [128, 4, NT], BF16)
                if "rwkv" in K.skip:
                    P.op("dve", lambda e: e.memset(YA.t[:], 0.0), w=[YA])
                else:
                    with ExitStack() as ph2:
                        phase_rwkv(K, ph2, YA)
                        P.barrier()
                if "ya%d" % l in dbg_out:
                    with ExitStack() as ph2:
                        dump_bf(K, ph2, YA, dbg_out["ya%d" % l], 4)
                        P.barrier()
                YB = sb(ph, "YB", [128, 4, NT], BF16)
                if "na" in K.skip:
                    P.op("dve", lambda e: e.memset(YB.t[:], 0.0), w=[YB])
                else:
                    with ExitStack() as ph2:
                        phase_na(K, ph2, YB)
                        P.barrier()
                if "yb%d" % l in dbg_out:
                    with ExitStack() as ph2:
                        dump_bf(K, ph2, YB, dbg_out["yb%d" % l], 4)
                        P.barrier()
                with ExitStack() as ph2:
                    phase_merge(K, ph2, YA, YB, xres)
                    P.barrier()
            with ExitStack() as ph:
                XT = sb(ph, "XT2", [128, 8, NT], F32)
                P.dma("sp", XT.t[:], xres, w=[XT])
                if "xm%d" % l in dbg_out:
                    P.dma("sp", dbg_out["xm%d" % l], XT.t[:], r=[XT], w=[Res()])
                if "moe" not in K.skip:
                    phase_moe(K, ph, XT)
                if l == n_layers - 1:
                    ry = Res()
                    P.dma("sp", yout, XT.t[:, :, NCTX:NT], r=[XT], w=[ry])
                    P.finish([ry])
                else:
                    rx = Res()
                    P.dma("sp", xres, XT.t[:], r=[XT], w=[rx])
                if "xo%d" % l in dbg_out:
                    P.dma("sp", dbg_out["xo%d" % l], XT.t[:], r=[XT], w=[Res()])
                P.barrier()
        print("instructions", P.n_ins, "waits", P.n_wait, "sems", P.nsem)
    return nc


def dump_bf(K, ph, src, dst, nchunk=8):
    P = K.P
    tmp = K.sb(ph, "dbgtmp", [128, NT], F32)
    for c in range(nchunk):
        P.op("dve", lambda e: e.tensor_copy(out=tmp.t[:], in_=src.t[:, c, :]), r=[src], w=[tmp])
        P.dma("sp", dst[:, c, :], tmp.t[:], r=[tmp], w=[Res()])


def phase_adaln(K, ph):
    P, D, l = K.P, K.D, K.l
    P.dma("sp", K.g12.t[:], D["g12"][l], w=[K.g12])
    P.dma("sp", K.modb.t[:], D["modb"][l], w=[K.modb])
    MW = [K.sb(ph, f"mw{i}", [128, 8, 1024], BF16) for i in range(2)]
    pm = K.ps()
    for g in range(6):
        mw = MW[g % 2]
        P.dma("pool", mw.t[:], D["modw"][l, g], w=[mw])
        for j in range(8):
            o = (g * 8 + j) * 2
            for kc in range(8):
                P.op("pe", lambda e: e.matmul(pm.t[:, o:o + 2], lhsT=mw.t[:, kc, j * 128:(j + 1) * 128],
                                              rhs=K.csb.t[:, kc, :], start=(kc == 0), stop=(kc == 7)),
                     r=[mw, K.csb], w=[pm])
    P.op("dve", lambda e: e.tensor_tensor(out=K.mt.t[:], in0=pm.t[:, 0:96].rearrange("p (j c) -> p j c", c=2),
                                          in1=K.modb.t[:].unsqueeze(2).to_broadcast([128, 48, 2]), op=ALU.add),
         r=[pm, K.modb], w=[K.mt])
    for (mul, gi, so) in [(K.mul1, 0, 8), (K.mul2, 1, 32)]:
        P.op("dve", lambda e: e.tensor_scalar_add(out=mul.t[:], in0=K.mt.t[:, so:so + 8, :], scalar1=1.0),
             r=[K.mt], w=[mul])
        P.op("dve", lambda e: e.tensor_tensor(out=mul.t[:], in0=mul.t[:],
                                              in1=K.g12.t[:, gi, :].unsqueeze(2).to_broadcast([128, 8, 2]),
                                              op=ALU.mult), r=[mul, K.g12], w=[mul])


def phase_norm(K, ph, XT, mul, shift_off, h2f_cb):
    P = K.P
    SQ = K.sb(ph, "n_sq", [128, 8, 512], BF16)
    RS = K.sb(ph, "n_rs", [128, 512], F32)
    TMP = [K.sb(ph, f"n_tmp{i}", [128, 512], F32) for i in range(2)]
    HF = K.sb(ph, "n_hf", [128, 8, 512], F32) if h2f_cb is not None else None
    for (n0, nb) in BLKS:
        col = 1 if n0 == 0 else 0
        P.op("act", lambda e: e.activation(out=SQ.t[:, :, :nb], in_=XT.t[:, :, n0:n0 + nb], func=AF.Square),
             r=[XT], w=[SQ])
        pa = K.ps()
        for c in range(8):
            P.op("pe", lambda e: e.matmul(pa.t[:, :nb], lhsT=K.onesb.t[:], rhs=SQ.t[:, c, :nb],
                                          start=(c == 0), stop=(c == 7)), r=[K.onesb, SQ], w=[pa])
        P.op("act", lambda e: e.activation(out=RS.t[:, :nb], in_=pa.t[:, :nb], func=AF.Sqrt,
                                           bias=RMS_EPS, scale=1.0 / 1024), r=[pa], w=[RS])
        P.op("dve", lambda e: e.reciprocal(out=RS.t[:, :nb], in_=RS.t[:, :nb]), r=[RS], w=[RS])
        for c in range(8):
            tmp = TMP[c % 2]
            P.op("dve", lambda e: e.scalar_tensor_tensor(out=tmp.t[:, :nb], in0=XT.t[:, c, n0:n0 + nb],
                                                         scalar=mul.t[:, c, col:col + 1], in1=RS.t[:, :nb],
                                                         op0=ALU.mult, op1=ALU.mult),
                 r=[XT, mul, RS], w=[tmp])
            P.op("act", lambda e: e.activation(out=K.HT.t[:, c, n0:n0 + nb], in_=tmp.t[:, :nb], func=AF.Identity,
                                               bias=K.mt.t[:, shift_off + c, col:col + 1], scale=1.0),
                 r=[tmp, K.mt], w=[K.HT])
            if HF is not None:
                P.op("act", lambda e: e.activation(out=HF.t[:, c, :nb], in_=tmp.t[:, :nb], func=AF.Identity,
                                                   bias=K.mt.t[:, shift_off + c, col:col + 1], scale=1.0),
                     r=[tmp, K.mt], w=[HF])
        if h2f_cb is not None:
            h2f_cb(n0, nb, HF)


def phase_rwkv(K, ph, YA):
    P, D, l, HT, sb = K.P, K.D, K.l, K.HT, K.sb
    WLX = sb(ph, "wlx", [128, 8, 384], BF16)
    GLB = sb(ph, "glb", [128, 512], BF16)
    RWP = sb(ph, "rwp", [128, 8, 7], F32)
    OMKA = sb(ph, "omka", [128, 8], F32)
    P.dma("pool", WLX.t[:], D["w_lx"][l], w=[WLX])
    P.dma("pool", GLB.t[:], D["glb"][l], w=[GLB])
    P.dma("sp", RWP.t[:], D["rwp"][l], w=[RWP])
    P.op("dve", lambda e: e.tensor_scalar(out=OMKA.t[:], in0=RWP.t[:, :, 3], scalar1=-1.0, scalar2=1.0,
                                          op0=ALU.mult, op1=ALU.add), r=[RWP], w=[OMKA])
    TLW = sb(ph, "tlw", [128, NT], BF16)
    LA = sb(ph, "la", [128, NT], BF16)
    SLG = sb(ph, "slg", [128, NT], BF16)
    for (n0, nb) in BLKS:
        for j, (dst, fn) in enumerate([(TLW, AF.Tanh), (LA, AF.Identity), (SLG, AF.Sigmoid)]):
            p = K.ps()
            for kc in range(8):
                P.op("pe", lambda e: e.matmul(p.t[:, :nb], lhsT=WLX.t[:, kc, j * 128:(j + 1) * 128],
                                              rhs=HT.t[:, kc, n0:n0 + nb], start=(kc == 0), stop=(kc == 7)),
                     r=[WLX, HT], w=[p])
            P.op("act", lambda e: e.activation(out=dst.t[:, n0:n0 + nb], in_=p.t[:, :nb], func=fn), r=[p], w=[dst])

    import os as _os
    STOP = int(_os.environ.get("RW_STOP", "99"))
    P.op("dve", lambda e: e.memset(YA.t[:], 0.0), w=[YA])
    if STOP == 0:
        return
    WR = [sb(ph, f"wr{i}", [128, 8, 320], BF16) for i in range(1)]
    WL = [sb(ph, f"wl{i}", [128, 128], BF16) for i in range(2)]
    AL = [sb(ph, f"al{i}", [128, 128], BF16) for i in range(2)]
    QQ = sb(ph, "qq", [128, NCH, 128], BF16)
    KB = sb(ph, "kb", [128, NT], BF16)
    KKt = sb(ph, "kk", [128, NT], BF16)
    KH = sb(ph, "kh", [64, NCH, 128], BF16)
    BH = sb(ph, "bh", [64, NCH, 128], BF16)
    EG = sb(ph, "eg", [128, NCH], F32)
    EGS = sb(ph, "egs", [128, NCH], F32)
    BS = sb(ph, "bs", [64, NT], BF16)
    V64 = sb(ph, "v64", [64, NCH, 64], BF16)
    VT = sb(ph, "vt", [64, NT], BF16)
    YTd = [sb(ph, f"yt{d}", [64, NT], BF16) for d in range(2)]
    SCR = []
    for pi_ in range(2):
        sc = {n: sb(ph, f"rs{pi_}_" + n, [128, 512], F32) for n in ["rf", "kf", "af", "lw", "g", "s1", "s2", "s3"]}
        sc["kht"] = sb(ph, f"rs{pi_}_kht", [128, 512], BF16)
        sc["bht"] = sb(ph, f"rs{pi_}_bht", [128, 512], BF16)
        sc["tot"] = sb(ph, f"rs{pi_}_tot", [128, 8], F32)
        SCR.append(sc)
    Rf, Kf, Af, LW, G, S1, S2, S3 = [SCR[0][n] for n in ["rf", "kf", "af", "lw", "g", "s1", "s2", "s3"]]
    NBT = 4
    for t__ in (QQ, KB, KKt, KH, BH, EG, EGS, V64):
        t__.rb = [Res() for _ in BLKS]
    bof = lambda c: 0 if c < 4 else 1 + (c - 4) // 8
    NSET = 3
    ATm = [[sb(ph, f"atm{i}{d}", [64, NBT, 128], BF16) for d in range(2)] for i in range(NSET)]
    BTm = [[sb(ph, f"btm{i}{d}", [64, NBT, 128], BF16) for d in range(2)] for i in range(NSET)]
    SSs = [[sb(ph, f"ss{q}{i}", [128, NBT, 192], BF16) for i in range(2)] for q in range(2)]
    SSrs = [[[{k: Res() for k in "MXA"} for d in range(2)] for i in range(2)] for q in range(2)]
    X5 = [[sb(ph, f"x5{i}{d}", [64, NBT, 64], BF16) for d in range(2)] for i in range(NSET)]
    T1 = sb(ph, "t1", [64, 2, 64], BF16)
    NU = sb(ph, "nu", [64, 2, 64], BF16)
    Hf = sb(ph, "hf", [128, 64], F32)
    Hb = sb(ph, "hb", [128, 64], BF16)
    order1 = [3, 2, 1, 0] + list(range(35, 3, -1))

    def v3(ap, j=64):
        return ap.rearrange("p (c j) -> p c j", j=j)

    for h in range(8):
        wr, wl, al = WR[0], WL[h % 2], AL[h % 2]
        P.dma("pool", wr.t[:], D["w_rkv"][l, h], w=[wr])
        P.dma("pool", wl.t[:], D["wlb"][l, h], w=[wl])
        P.dma("pool", al.t[:], D["alb"][l, h], w=[al])
        prm = lambda i, lo=0, hi=128: RWP.t[lo:hi, h, i:i + 1]
        def gen_elem(bi, par_):
            sc_ = SCR[par_]
            Rf, Kf, Af, LW, G, S1, S2, S3 = [sc_[n] for n in ["rf", "kf", "af", "lw", "g", "s1", "s2", "s3"]]
            KHT, BHT, TOT = sc_["kht"], sc_["bht"], sc_["tot"]
            for bi in [bi]:
                n0, nb = BLKS[bi]
                ncb, c0 = nb // 64, n0 // 64
                blk = slice(n0, n0 + nb)

                def proj(dst, lo, hi, M, eng, outdt_t=None):
                    p = K.ps()
                    for kc in range(8):
                        P.op("pe", lambda e: e.matmul(p.t[0:M, :nb], lhsT=wr.t[:, kc, lo:hi], rhs=HT.t[:, kc, blk],
                                                      start=(kc == 0), stop=(kc == 7)), r=[wr, HT], w=[p])
                    if eng == "act":
                        P.op("act", lambda e: e.activation(out=dst, in_=p.t[0:M, :nb], func=AF.Identity), r=[p],
                             w=[outdt_t])
                    else:
                        P.op("dve", lambda e: e.tensor_copy(out=dst, in_=p.t[0:M, :nb]), r=[p], w=[outdt_t])

                yield
                proj(Rf.t[:, :nb], 0, 128, 128, "act", Rf)
                yield
                proj(Kf.t[:, :nb], 128, 256, 128, "dve", Kf)
                yield
                proj(VT.t[:, blk], 256, 320, 64, "act", VT)
                yield
                p = K.ps()
                P.op("pe", lambda e: e.matmul(p.t[:, :nb], lhsT=wl.t[:], rhs=TLW.t[:, blk], start=True, stop=True),
                     r=[wl, TLW], w=[p])
                P.op("act", lambda e: e.activation(out=LW.t[:, :nb], in_=p.t[:, :nb], func=AF.Sigmoid,
                                                   bias=prm(0), scale=1.0), r=[p, RWP], w=[LW])
                yield
                p = K.ps()
                P.op("pe", lambda e: e.matmul(p.t[:, :nb], lhsT=al.t[:], rhs=LA.t[:, blk], start=True, stop=True),
                     r=[al, LA], w=[p])
                P.op("act", lambda e: e.activation(out=Af.t[:, :nb], in_=p.t[:, :nb], func=AF.Sigmoid,
                                                   bias=prm(1), scale=1.0), r=[p, RWP], w=[Af])
                yield
                P.op("dve", lambda e: e.tensor_scalar_mul(out=S1.t[:, :nb], in0=Kf.t[:, :nb], scalar1=prm(2)),
                     r=[Kf, RWP], w=[S1])
                P.op("act", lambda e: e.activation(out=S2.t[:, :nb], in_=S1.t[:, :nb], func=AF.Square), r=[S1], w=[S2])
                yield
                p = K.ps()
                P.op("pe", lambda e: e.matmul(p.t[:, :nb], lhsT=K.bones.t[:], rhs=S2.t[:, :nb], start=True, stop=True),
                     r=[K.bones, S2], w=[p])
                P.op("act", lambda e: e.activation(out=S2.t[:, :nb], in_=p.t[:, :nb], func=AF.Sqrt), r=[p], w=[S2])
                P.op("dve", lambda e: e.tensor_scalar_max(out=S2.t[:, :nb], in0=S2.t[:, :nb], scalar1=1e-6),
                     r=[S2], w=[S2])
                yield
                P.op("dve", lambda e: e.reciprocal(out=S2.t[:, :nb], in_=S2.t[:, :nb]), r=[S2], w=[S2])
                P.op("dve", lambda e: e.tensor_mul(out=S1.t[:, :nb], in0=S1.t[:, :nb], in1=S2.t[:, :nb]),
                     r=[S1, S2], w=[S1])
                P.op("dve", lambda e: e.tensor_scalar(out=S2.t[:, :nb], in0=Af.t[:, :nb], scalar1=prm(3),
                                                      scalar2=OMKA.t[:, h:h + 1], op0=ALU.mult, op1=ALU.add),
                     r=[Af, RWP, OMKA], w=[S2])
                yield
                P.op("dve", lambda e: e.tensor_mul(out=S2.t[:, :nb], in0=S2.t[:, :nb], in1=Kf.t[:, :nb]),
                     r=[S2, Kf], w=[S2])
                P.op("dve", lambda e: e.tensor_mul(out=S3.t[:, :nb], in0=S1.t[:, :nb], in1=Af.t[:, :nb]),
                     r=[S1, Af], w=[S3])
                yield
                P.op("dve", lambda e: e.scalar_tensor_tensor(out=Af.t[0:64, :nb], in0=Rf.t[0:64, :nb], scalar=prm(4, 0, 64),
                                                             in1=Kf.t[0:64, :nb], op0=ALU.mult, op1=ALU.mult),
                     r=[Rf, Kf, RWP], w=[Af])
                yield
                p = K.ps()
                P.op("pe", lambda e: e.matmul(p.t[0:64, :nb], lhsT=K.bones.t[0:64, 0:64], rhs=Af.t[0:64, :nb],
                                              start=True, stop=True), r=[K.bones, Af], w=[p])
                P.op("act", lambda e: e.activation(out=BS.t[:, blk], in_=p.t[0:64, :nb], func=AF.Identity),
                     r=[p], w=[BS])
                if STOP == 1:
                    continue
                yield
                P.op("dve", lambda e: e.tensor_scalar_mul(out=LW.t[:, :nb], in0=LW.t[:, :nb], scalar1=DEC_C),
                     r=[LW], w=[LW])
                P.op("dve", lambda e: e.tensor_tensor_scan(out=G.t[:, :nb], data0=K.rmask.t[:, :nb], data1=LW.t[:, :nb],
                                                           initial=0.0, op0=ALU.mult, op1=ALU.add),
                     r=[K.rmask, LW], w=[G])
                P.op("dve", lambda e: e.tensor_copy(out=TOT.t[64:128, :ncb], in_=v3(G.t[64:128, :nb])[:, :, 63]),
                     r=[G], w=[TOT])
                yield
                P.op("dve", lambda e: e.tensor_sub(out=Af.t[64:128, :nb], in0=LW.t[64:128, :nb], in1=G.t[64:128, :nb]),
                     r=[LW, G], w=[Af])
                P.op("dve", lambda e: e.tensor_tensor(out=v3(G.t[64:128, :nb]), in0=v3(Af.t[64:128, :nb]),
                                                      in1=TOT.t[64:128, :ncb].unsqueeze(2).to_broadcast([64, ncb, 64]),
                                                      op=ALU.add), r=[Af, TOT], w=[G])
                P.op("dve", lambda e: e.tensor_sub(out=LW.t[:, :nb], in0=G.t[:, :nb], in1=LW.t[:, :nb]),
                     r=[G, LW], w=[LW])
                yield
                P.op("act", lambda e: e.activation(out=LW.t[:, :nb], in_=LW.t[:, :nb], func=AF.Exp), r=[LW], w=[LW])
                P.op("dve", lambda e: e.tensor_mul(out=QQ.t[:, c0:c0 + ncb, 0:64], in0=v3(S1.t[:, :nb]),
                                                   in1=v3(LW.t[:, :nb])), r=[S1, LW], w=[QQ.rb[bi]])
                P.op("act", lambda e: e.activation(out=S1.t[:, :nb], in_=G.t[:, :nb], func=AF.Exp), r=[G], w=[S1])
                yield
                P.op("dve", lambda e: e.tensor_mul(out=QQ.t[:, c0:c0 + ncb, 64:128], in0=v3(Rf.t[:, :nb]),
                                                   in1=v3(S1.t[:, :nb])), r=[Rf, S1], w=[QQ.rb[bi]])
                P.op("dve", lambda e: e.tensor_copy(out=EG.t[0:64, c0:c0 + ncb], in_=v3(S1.t[0:64, :nb])[:, :, 63]),
                     r=[S1], w=[EG.rb[bi]])
                P.op("dve", lambda e: e.tensor_copy(out=EG.t[64:128, c0:c0 + ncb], in_=v3(S1.t[64:128, :nb])[:, :, 0]),
                     r=[S1], w=[EG.rb[bi]])
                s_hi = (3 - c0) if c0 < 4 else (39 - c0)
                s_lo = s_hi - ncb
                osl = slice(s_hi, (s_lo if s_lo >= 0 else None), -1)
                P.op("act", lambda e: e.activation(out=EGS.t[0:64, c0:c0 + ncb], in_=v3(S1.t[0:64, :nb])[:, :, 63],
                                                   func=AF.Identity), r=[S1], w=[EGS.rb[bi]])
                P.op("act", lambda e: e.activation(out=EGS.t[64:128, osl], in_=v3(S1.t[64:128, :nb])[:, :, 0],
                                                   func=AF.Identity), r=[S1], w=[EGS.rb[bi]])
                yield
                P.op("act", lambda e: e.activation(out=LW.t[:, :nb], in_=G.t[:, :nb], func=AF.Exp, scale=-1.0),
                     r=[G], w=[LW])
                P.op(ELT_ENG, lambda e: e.tensor_mul(out=KB.t[:, blk], in0=S3.t[:, :nb], in1=LW.t[:, :nb]),
                     r=[S3, LW], w=[KB.rb[bi]])
                P.op(ELT_ENG, lambda e: e.tensor_mul(out=KKt.t[:, blk], in0=S2.t[:, :nb], in1=LW.t[:, :nb]),
                     r=[S2, LW], w=[KKt.rb[bi]])
                egb = EG.t[:, c0:c0 + ncb].unsqueeze(2).to_broadcast([128, ncb, 64])
                yield
                P.op(ELT_ENG, lambda e: e.tensor_tensor(out=v3(KHT.t[:, :nb]), in0=v3(KKt.t[:, blk]), in1=egb, op=ALU.mult),
                     r=[KKt.rb[bi], EG.rb[bi]], w=[KHT])
                P.op(ELT_ENG, lambda e: e.tensor_tensor(out=v3(BHT.t[:, :nb]), in0=v3(KB.t[:, blk]), in1=egb, op=ALU.mult),
                     r=[KB.rb[bi], EG.rb[bi]], w=[BHT])
                if STOP == 2:
                    continue
                yield
                for g0 in range(0, ncb, 8):
                    g1 = min(ncb, g0 + 8)
                    p = K.ps()
                    for j in range(g0, g1):
                        c = c0 + j
                        for kc in range(8):
                            P.op("pe", lambda e: e.matmul(p.t[0:64, (j - g0) * 64:(j - g0 + 1) * 64],
                                                          lhsT=HT.t[:, kc, c * 64:(c + 1) * 64], rhs=wr.t[:, kc, 256:320],
                                                          start=(kc == 0), stop=(kc == 7)), r=[HT, wr], w=[p])
                    P.op("act", lambda e: e.activation(out=V64.t[:, c0 + g0:c0 + g1, :],
                                                       in_=v3(p.t[0:64, 0:(g1 - g0) * 64]), func=AF.Identity),
                         r=[p], w=[V64.rb[bi]])
                    for (src, dst) in [(KHT, KH), (BHT, BH)]:
                        for j in range(g0, g1):
                            c = c0 + j
                            P.op("pe", lambda e: e.transpose(K.PSB.t[0:64, (j - g0) * 128:(j - g0 + 1) * 128],
                                                             src.t[:, j * 64:(j + 1) * 64], K.identb.t[:]),
                                 r=[src, K.identb], w=[K.PSB])
                        P.op("dve", lambda e: e.tensor_copy(out=dst.t[:, c0 + g0:c0 + g1, :],
                                                            in_=v3(K.PSB.t[0:64, 0:(g1 - g0) * 128], 128)),
                             r=[K.PSB], w=[dst.rb[bi]])

                yield

        P.op("dve", lambda e: e.memset(Hf.t[:], 0.0), w=[Hf])
        P.op("dve", lambda e: e.memset(Hb.t[:], 0.0), w=[Hb])
        dP = [slice(0, 64), slice(64, 128)]
        cdir = [list(range(NCH)), order1]
        batches = [list(range(i, min(NCH, i + NBT))) for i in range(0, NCH, NBT)]
        idbb = K.identb.t[0:64, 0:64].unsqueeze(1).to_broadcast([64, NBT, 64])

        def gen_prep(b):
            par = b % NSET
            SS, SSr = SSs[b % 2], SSrs[b % 2]
            bt = batches[b]
            nb_ = len(bt)
            cur = [0, 0]

            def cp(o, i_, rd, wr):
                P.op("act", lambda e: e.activation(out=o, in_=i_, func=AF.Identity), r=rd, w=wr)

            for d in range(2):
                q = dP[d]
                atm, btm = ATm[par][d], BTm[par][d]
                t_, r_ = SS[cur[d]].t, SSr[cur[d]][d]
                p1, p2, p3 = K.ps(), K.ps(), K.ps()
                for j, i in enumerate(bt):
                    c = cdir[d][i]
                    ck = slice(c * 64, c * 64 + 64)
                    P.op("pe", lambda e: e.matmul(p1.t[0:64, j * 128:(j + 1) * 128], lhsT=KB.t[q, ck],
                                                  rhs=QQ.t[q, c, :], start=True, stop=True), r=[KB.rb[bof(c)], QQ.rb[bof(c)]], w=[p1])
                    P.op("pe", lambda e: e.matmul(p2.t[0:64, j * 128:(j + 1) * 128], lhsT=KKt.t[q, ck],
                                                  rhs=QQ.t[q, c, :], start=True, stop=True), r=[KKt.rb[bof(c)], QQ.rb[bof(c)]], w=[p2])
                    P.op("pe", lambda e: e.matmul(p3.t[q, j * 64:(j + 1) * 64], lhsT=QQ.t[q, c, 0:64],
                                                  rhs=KB.t[q, ck], start=True, stop=True), r=[KB.rb[bof(c)], QQ.rb[bof(c)]], w=[p3])
                mcf = K.maskc.t[:, d, :].unsqueeze(1).to_broadcast([64, nb_, 128])
                maf = K.maska2.t[q, :].unsqueeze(1).to_broadcast([64, nb_, 64])
                idq = K.identb.t[q, q].unsqueeze(1).to_broadcast([64, nb_, 64])
                P.op("dve", lambda e: e.tensor_tensor(out=atm.t[:, :nb_, :], in0=v3(p1.t[0:64, 0:nb_ * 128], 128),
                                                      in1=mcf, op=ALU.mult), r=[p1, K.maskc], w=[atm])
                P.op("dve", lambda e: e.tensor_tensor(out=t_[q, :nb_, 128:192], in0=v3(p3.t[q, 0:nb_ * 64]),
                                                      in1=maf, op=ALU.mult), r=[p3, K.maska2], w=[r_["A"]])
                cp(t_[q, :nb_, 64:128], atm.t[:, :nb_, 0:64], [atm], [r_["M"]])
                P.op("dve", lambda e: e.tensor_tensor(out=btm.t[:, :nb_, :], in0=v3(p2.t[0:64, 0:nb_ * 128], 128),
                                                      in1=mcf, op=ALU.mult), r=[p2, K.maskc], w=[btm])
                P.op("dve", lambda e: e.tensor_tensor(out=t_[q, :nb_, 0:64], in0=idq, in1=t_[q, :nb_, 64:128],
                                                      op=ALU.subtract), r=[K.identb, r_["M"]], w=[r_["X"]])
                yield
            for lv in range(1, 6):
                last = lv == 5
                for d in range(2):
                    q = dP[d]
                    t_, r_ = SS[cur[d]].t, SSr[cur[d]][d]
                    nxt = 1 - cur[d]
                    tn, rn = SS[nxt].t, SSr[nxt][d]
                    pj = K.ps()
                    for j in range(nb_):
                        P.op("pe", lambda e: e.matmul(pj.t[q, j * 128 + 64:j * 128 + 128], lhsT=t_[q, j, 64:128],
                                                      rhs=t_[q, j, 128:192], start=True, stop=True), r=[r_["M"], r_["A"]], w=[pj])
                        if not last:
                            P.op("pe", lambda e: e.matmul(pj.t[q, j * 128:j * 128 + 64], lhsT=t_[q, j, 128:192],
                                                          rhs=t_[q, j, 64:128], start=True, stop=True), r=[r_["M"], r_["A"]], w=[pj])
                    pv = v3(pj.t[q, 0:nb_ * 128], 128)
                    if not last:
                        cp(tn[q, :nb_, 64:192], pv, [pj], [rn["M"], rn["A"]])
                    else:
                        cp(tn[q, :nb_, 128:192], pv[:, :, 64:128], [pj], [rn["A"]])
                    yield
                for d in range(2):
                    q = dP[d]
                    t_, r_ = SS[cur[d]].t, SSr[cur[d]][d]
                    nxt = 1 - cur[d]
                    tn, rn = SS[nxt].t, SSr[nxt][d]
                    px = K.ps()
                    for j in range(nb_):
                        P.op("pe", lambda e: e.matmul(px.t[q, j * 64:(j + 1) * 64], lhsT=tn[q, j, 128:192],
                                                      rhs=t_[q, j, 0:64], start=True, stop=True), r=[rn["A"], r_["X"]], w=[px])
                    xo = X5[par][d].t[:, :nb_, :] if last else tn[q, :nb_, 0:64]
                    xr = X5[par][d] if last else rn["X"]
                    P.op("dve", lambda e: e.tensor_tensor(out=xo, in0=v3(px.t[q, 0:nb_ * 64]), in1=t_[q, :nb_, 0:64],
                                                          op=ALU.add), r=[px, r_["X"]], w=[xr])
                    cur[d] = nxt
                    yield

        def gen_chain(b):
            par = b % NSET
            for j, i in enumerate(batches[b]):
                cd = (cdir[0][i], cdir[1][i])
                ck = [slice(cd[d] * 64, cd[d] * 64 + 64) for d in range(2)]
                atm = [ATm[par][d] for d in range(2)]
                btm = [BTm[par][d] for d in range(2)]
                x5 = [X5[par][d] for d in range(2)]
                pt, py, phh = K.ps(), K.ps(), K.ps()

                def mm(o, l_, r_, st_, sp_, rd, wr):
                    P.op("pe", lambda e: e.matmul(o, lhsT=l_, rhs=r_, start=st_, stop=sp_), r=rd, w=[wr])

                for d in range(2):
                    mm(pt.t[0:64, d * 64:(d + 1) * 64], btm[d].t[:, j, 0:64], V64.t[:, cd[d], :], True, False, [btm[d], V64.rb[bof(cd[d])]], pt)
                    mm(pt.t[0:64, d * 64:(d + 1) * 64], QQ.t[dP[d], cd[d], 0:64], Hb.t[dP[d], :], False, True, [QQ.rb[bof(cd[d])], Hb], pt)
                P.op("act", lambda e: e.activation(out=T1.t[:], in_=v3(pt.t[0:64, 0:128]), func=AF.Identity),
                     r=[pt], w=[T1])
                yield
                pu = K.ps()
                for d in range(2):
                    mm(pu.t[0:64, d * 64:(d + 1) * 64], x5[d].t[:, j, :], T1.t[:, d, :], True, True, [x5[d], T1], pu)
                P.op("dve", lambda e: e.tensor_scalar_mul(out=NU.t[:], in0=v3(pu.t[0:64, 0:128]), scalar1=-1.0),
                     r=[pu], w=[NU])
                yield
                for d in range(2):
                    mm(phh.t[dP[d], 0:64], KH.t[:, cd[d], dP[d]], V64.t[:, cd[d], :], True, False, [KH.rb[bof(cd[d])], V64.rb[bof(cd[d])]], phh)
                    mm(phh.t[dP[d], 0:64], BH.t[:, cd[d], dP[d]], NU.t[:, d, :], False, True, [BH.rb[bof(cd[d])], NU], phh)
                for d in range(2):
                    o = py.t[0:64, d * 64:(d + 1) * 64]
                    mm(o, V64.t[:, cd[d], :], btm[d].t[:, j, 64:128], True, False, [V64.rb[bof(cd[d])], btm[d]], py)
                    mm(o, NU.t[:, d, :], atm[d].t[:, j, 64:128], False, False, [NU, atm[d]], py)
                    mm(o, Hb.t[dP[d], :], QQ.t[dP[d], cd[d], 64:128], False, True, [Hb, QQ.rb[bof(cd[d])]], py)
                egr = [EGS.rb[bof(cd[0])], EGS.rb[bof(cd[1])]]
                P.op("dve", lambda e: e.scalar_tensor_tensor(out=Hb.t[:], in0=Hf.t[:], scalar=EGS.t[:, i:i + 1],
                                                             in1=phh.t[:, 0:64], op0=ALU.mult, op1=ALU.add),
                     r=[Hf, phh] + egr, w=[Hb])
                P.op("dve", lambda e: e.scalar_tensor_tensor(out=Hf.t[:], in0=Hf.t[:], scalar=EGS.t[:, i:i + 1],
                                                             in1=phh.t[:, 0:64], op0=ALU.mult, op1=ALU.add),
                     r=[Hf, phh] + egr, w=[Hf])
                for d in range(2):
                    P.op("act", lambda e: e.activation(out=YTd[d].t[:, ck[d]], in_=py.t[0:64, d * 64:(d + 1) * 64],
                                                       func=AF.Identity), r=[py], w=[YTd[d]])
                yield

        def drive(gens):
            gens = list(gens)
            while gens:
                for g in list(gens):
                    try:
                        next(g)
                    except StopIteration:
                        gens.remove(g)

        nbt_ = len(batches)
        preps = {}
        prep_done = set()
        st_ = {"next_prep": 0, "next_chain": 0, "fin_chain": 0, "chain": None}
        e_pend = [0, 1, 4, 2, 3]
        e_act = []
        e_free = [0, 1]
        e_done = set()
        req_ = lambda k: {0} if k == 0 else ({0, 1, 4} if k < 3 else {0, 1, 2, 3, 4})
        while st_["fin_chain"] < nbt_:
            while e_pend and e_free:
                pr_ = e_free.pop(0)
                bi_ = e_pend.pop(0)
                e_act.append((gen_elem(bi_, pr_), pr_, bi_))
            while (st_["next_prep"] < nbt_ and len(preps) < 2 and st_["next_prep"] < st_["fin_chain"] + NSET
                   and req_(st_["next_prep"]) <= e_done):
                k_ = st_["next_prep"]
                preps[k_] = gen_prep(k_)
                st_["next_prep"] += 1
            if st_["chain"] is None and st_["next_chain"] in prep_done:
                st_["chain"] = gen_chain(st_["next_chain"])
                st_["next_chain"] += 1
            if st_["chain"] is not None:
                try:
                    next(st_["chain"])
                except StopIteration:
                    st_["chain"] = None
                    st_["fin_chain"] += 1
            for k_ in list(preps):
                try:
                    next(preps[k_])
                except StopIteration:
                    del preps[k_]
                    prep_done.add(k_)
            for it_ in list(e_act):
                try:
                    next(it_[0])
                except StopIteration:
                    e_act.remove(it_)
                    e_free.append(it_[1])
                    e_done.add(it_[2])
        if STOP <= 5:
            continue
        hp = slice((h % 2) * 64, (h % 2) * 64 + 64)
        o64 = K.bones.t[0:64, 0:64]
        def gen_out(bi, par_):
            sc_ = SCR[par_]
            S1, S2, S3 = sc_["s1"], sc_["s2"], sc_["s3"]
            for (n0, nb) in [BLKS[bi]]:
                blk = slice(n0, n0 + nb)
                yield
                p = K.ps()
                P.op("dve", lambda e: e.tensor_add(out=S3.t[0:64, :nb], in0=YTd[0].t[:, blk], in1=YTd[1].t[:, blk]),
                     r=[YTd[0], YTd[1]], w=[S3])
                P.op("pe", lambda e: e.matmul(p.t[0:64, :nb], lhsT=o64, rhs=S3.t[0:64, :nb], start=True, stop=True),
                     r=[K.bones, S3], w=[p])
                P.op("dve", lambda e: e.scalar_tensor_tensor(out=S1.t[0:64, :nb], in0=p.t[0:64, :nb], scalar=-1.0 / 64,
                                                             in1=S3.t[0:64, :nb], op0=ALU.mult, op1=ALU.add),
                     r=[p, S3], w=[S1])
                P.op("act", lambda e: e.activation(out=S2.t[0:64, :nb], in_=S1.t[0:64, :nb], func=AF.Square),
                     r=[S1], w=[S2])
                yield
                p = K.ps()
                P.op("pe", lambda e: e.matmul(p.t[0:64, :nb], lhsT=o64, rhs=S2.t[0:64, :nb], start=True, stop=True),
                     r=[K.bones, S2], w=[p])
                P.op("act", lambda e: e.activation(out=S2.t[0:64, :nb], in_=p.t[0:64, :nb], func=AF.Sqrt,
                                                   bias=GN_EPS, scale=1.0 / 64), r=[p], w=[S2])
                P.op("dve", lambda e: e.reciprocal(out=S2.t[0:64, :nb], in_=S2.t[0:64, :nb]), r=[S2], w=[S2])
                P.op("dve", lambda e: e.tensor_mul(out=S1.t[0:64, :nb], in0=S1.t[0:64, :nb], in1=S2.t[0:64, :nb]),
                     r=[S1, S2], w=[S1])
                P.op("act", lambda e: e.activation(out=S1.t[0:64, :nb], in_=S1.t[0:64, :nb], func=AF.Identity,
                                                   bias=prm(6, 0, 64), scale=prm(5, 0, 64)), r=[S1, RWP], w=[S1])
                P.op("dve", lambda e: e.tensor_mul(out=S3.t[0:64, :nb], in0=BS.t[:, blk], in1=VT.t[:, blk]),
                     r=[BS, VT], w=[S3])
                P.op("dve", lambda e: e.tensor_add(out=S1.t[0:64, :nb], in0=S1.t[0:64, :nb], in1=S3.t[0:64, :nb]),
                     r=[S1, S3], w=[S1])
                yield
                p = K.ps()
                P.op("pe", lambda e: e.matmul(p.t[0:64, :nb], lhsT=GLB.t[:, h * 64:(h + 1) * 64], rhs=SLG.t[:, blk],
                                              start=True, stop=True), r=[GLB, SLG], w=[p])
                P.op("dve", lambda e: e.tensor_mul(out=YA.t[hp, h // 2, blk], in0=S1.t[0:64, :nb], in1=p.t[0:64, :nb]),
                     r=[S1, p], w=[YA])
                yield

        pend_ = list(range(len(BLKS)))
        act2 = []
        free2 = [0, 1]
        while pend_ or act2:
            while pend_ and free2:
                pr_ = free2.pop(0)
                act2.append((gen_out(pend_.pop(0), pr_), pr_))
            for it_ in list(act2):
                try:
                    next(it_[0])
                except StopIteration:
                    act2.remove(it_)
                    free2.append(it_[1])


def phase_na(K, ph, YB):
    P, D, l, HT, sb = K.P, K.D, K.l, K.HT, K.sb

    def v3(ap, j=64):
        return ap.rearrange("p (c j) -> p c j", j=j)

    WVB = sb(ph, "wvb", [128, 8, 512], BF16)
    P.dma("pool", WVB.t[:], D["w_vb"][l], w=[WVB])
    NAP = sb(ph, "nap", [64, 2], F32)
    P.dma("sp", NAP.t[:], D["nap"][l], w=[NAP])
    P.op("dve", lambda e: e.tensor_scalar_mul(out=NAP.t[:, 0:1], in0=NAP.t[:, 0:1], scalar1=0.125), r=[NAP], w=[NAP])
    VAe = sb(ph, "vae", [128, 18, 8, 65], BF16)
    VAo = sb(ph, "vao", [128, 17, 8, 65], BF16)
    P.op("dve", lambda e: e.memset(VAe.t[:, :, :, 64:65], 1.0), w=[VAe])
    P.op("dve", lambda e: e.memset(VAo.t[:, :, :, 64:65], 1.0), w=[VAo])
    cnt = 0
    for (va, off, n_) in [(VAe, 0, 18), (VAo, 64, 17)]:
        for c in range(n_):
            p = K.ps()
            t0 = off + c * 128
            for kc in range(8):
                P.op("pe", lambda e: e.matmul(p.t[:, :], lhsT=HT.t[:, kc, t0:t0 + 128], rhs=WVB.t[:, kc, :],
                                              start=(kc == 0), stop=(kc == 7)), r=[HT, WVB], w=[p])
            if cnt % 2 == 0:
                P.op("act", lambda e: e.activation(out=va.t[:, c, :, 0:64], in_=v3(p.t[:, :]), func=AF.Identity),
                     r=[p], w=[va])
            else:
                P.op("dve", lambda e: e.tensor_copy(out=va.t[:, c, :, 0:64], in_=v3(p.t[:, :])), r=[p], w=[va])
            cnt += 1

    def vtile(ck):
        return (VAe, ck // 2) if ck % 2 == 0 else (VAo, (ck - 1) // 2)

    WQK = [sb(ph, f"wqk{i}", [128, 8, 128], BF16) for i in range(2)]
    BIAS = [[sb(ph, f"bias{i}{par}", [128, 7, 64], F32) for par in range(2)] for i in range(2)]
    QT = sb(ph, "qt", [64, NT], BF16)
    KT = sb(ph, "kt", [64, NT], BF16)
    SQ = [sb(ph, f"na_sq{i}", [64, 512], F32) for i in range(2)]
    RS = [sb(ph, f"na_rs{i}", [64, 512], F32) for i in range(2)]
    TM = [sb(ph, f"na_tm{i}", [64, 512], F32) for i in range(2)]
    SB_ = [sb(ph, f"na_sb{i}", [128, 4, 64], F32) for i in range(2)]
    PT = [sb(ph, f"na_pt{i}", [128, 6, 64], BF16) for i in range(2)]
    RD = sb(ph, "na_rd", [65, 512], F32)
    RB = sb(ph, "na_rb", [64, 512], F32)
    o64 = K.bones.t[0:64, 0:64]
    for h in range(8):
        wqk, bias = WQK[h % 2], BIAS[h % 2]
        hp = slice((h % 2) * 64, (h % 2) * 64 + 64)
        P.dma("pool", wqk.t[:], D["w_qk"][l, h], w=[wqk])
        for par in range(2):
            for e_ in range(2):
                s0 = par + e_
                P.dma("sp", bias[par].t[e_ * 64:(e_ + 1) * 64, :, :], D["rpbT"][l, h][:, s0:s0 + 13:2, :], w=[bias[par]])
            P.op("dve", lambda e: e.tensor_tensor(out=bias[par].t[:], in0=bias[par].t[:],
                                                  in1=K.namask.t[:].unsqueeze(1).to_broadcast([128, 7, 64]), op=ALU.add),
                 r=[bias[par], K.namask], w=[bias[par]])
        for (n0, nb) in BLKS:
            blk = slice(n0, n0 + nb)
            dsts = [QT, KT]
            pp = [K.ps(), K.ps()]
            for qi in range(2):
                for kc in range(8):
                    P.op("pe", lambda e: e.matmul(pp[qi].t[0:64, :nb], lhsT=wqk.t[:, kc, qi * 64:(qi + 1) * 64],
                                                  rhs=HT.t[:, kc, blk], start=(kc == 0), stop=(kc == 7)),
                         r=[wqk, HT], w=[pp[qi]])
            for qi in range(2):
                P.op("act", lambda e: e.activation(out=SQ[qi].t[:, :nb], in_=pp[qi].t[0:64, :nb], func=AF.Square),
                     r=[pp[qi]], w=[SQ[qi]])
            p2 = [K.ps(), K.ps()]
            for qi in range(2):
                P.op("pe", lambda e: e.matmul(p2[qi].t[0:64, :nb], lhsT=o64, rhs=SQ[qi].t[:, :nb], start=True, stop=True),
                     r=[K.bones, SQ[qi]], w=[p2[qi]])
            for qi in range(2):
                P.op("act", lambda e: e.activation(out=RS[qi].t[:, :nb], in_=p2[qi].t[0:64, :nb], func=AF.Sqrt,
                                                   bias=RMS_EPS, scale=1.0 / 64), r=[p2[qi]], w=[RS[qi]])
            for qi in range(2):
                P.op("dve", lambda e: e.reciprocal(out=RS[qi].t[:, :nb], in_=RS[qi].t[:, :nb]), r=[RS[qi]], w=[RS[qi]])
            for qi in range(2):
                P.op("dve", lambda e: e.tensor_mul(out=TM[qi].t[:, :nb], in0=pp[qi].t[0:64, :nb], in1=RS[qi].t[:, :nb]),
                     r=[pp[qi], RS[qi]], w=[TM[qi]])
            for qi in range(2):
                P.op("act", lambda e: e.activation(out=dsts[qi].t[:, blk], in_=TM[qi].t[:, :nb], func=AF.Identity,
                                                   scale=NAP.t[:, qi:qi + 1]), r=[TM[qi], NAP], w=[dsts[qi]])
        groups = [([0, 1, 2, 3], True)] + [(list(range(4 + g * 8, 12 + g * 8)), False) for g in range(4)]
        items = []
        for gi, (qcs, isctx) in enumerate(groups):
            for qi_, cq in enumerate(qcs):
                items.append((gi, qi_, cq, isctx, len(qcs), qcs[0]))
        POs = [K.PSL, K.PSL2]
        state = {}

        def emit_S(n):
            gi, qi_, cq, isctx, ng, q0 = items[n]
            qs = slice(cq * 64, cq * 64 + 64)
            pt = PT[n % 2]
            sbt = SB_[n % 2]
            kcs = []
            if not isctx:
                i = cq - 4
                r0 = min(max(i - 4, 0), 24)
                d0 = r0 - i + 7
                par, m0 = d0 % 2, d0 // 2
                pS = K.ps()
                for m in range(4):
                    ck = 4 + r0 + 2 * m
                    kcs.append(ck)
                    P.op("pe", lambda e: e.matmul(pS.t[:, m * 64:(m + 1) * 64], lhsT=KT.t[:, ck * 64:ck * 64 + 128],
                                                  rhs=QT.t[:, qs], start=True, stop=True), r=[KT, QT], w=[pS])
                P.op("dve", lambda e: e.tensor_tensor(out=sbt.t[:], in0=v3(pS.t[:, 0:256]), in1=bias[par].t[:, m0:m0 + 4, :],
                                                      op=ALU.add), r=[pS, bias[par]], w=[sbt])
                P.op("act", lambda e: e.activation(out=pt.t[:, 0:4, :], in_=sbt.t[:], func=AF.Exp), r=[sbt], w=[pt])
            pC = K.ps()
            for a in range(2):
                P.op("pe", lambda e: e.matmul(pC.t[:, a * 64:(a + 1) * 64], lhsT=KT.t[:, a * 128:(a + 1) * 128],
                                              rhs=QT.t[:, qs], start=True, stop=True), r=[KT, QT], w=[pC])
            P.op("act", lambda e: e.activation(out=pt.t[:, 4:6, :], in_=v3(pC.t[:, 0:128]), func=AF.Exp),
                 r=[pC], w=[pt])
            state[n] = kcs

        def emit_PV(n):
            gi, qi_, cq, isctx, ng, q0 = items[n]
            pt = PT[n % 2]
            po = POs[gi % 2]
            kcs = state.pop(n)
            keys = [(m, kcs[m]) for m in range(len(kcs))] + [(4, 0), (5, 2)]
            for n_, (slot, ck) in enumerate(keys):
                va, vi = vtile(ck)
                P.op("pe", lambda e: e.matmul(po.t[0:65, qi_ * 64:(qi_ + 1) * 64], lhsT=va.t[:, vi, h, :],
                                              rhs=pt.t[:, slot, :], start=(n_ == 0), stop=(n_ == len(keys) - 1)),
                     r=[va, pt], w=[po])
            if qi_ == ng - 1:
                W = ng * 64
                n0 = q0 * 64
                P.op("dve", lambda e: e.reciprocal(out=RD.t[64:65, :W], in_=po.t[64:65, :W]), r=[po], w=[RD])
                pb = K.ps()
                P.op("pe", lambda e: e.matmul(pb.t[0:64, :W], lhsT=K.onesf.t[64:65, 0:64], rhs=RD.t[64:65, :W],
                                              start=True, stop=True), r=[K.onesf, RD], w=[pb])
                P.op("act", lambda e: e.activation(out=RB.t[:, :W], in_=pb.t[0:64, :W], func=AF.Identity), r=[pb], w=[RB])
                P.op("dve", lambda e: e.tensor_mul(out=YB.t[hp, h // 2, n0:n0 + W], in0=po.t[0:64, :W], in1=RB.t[:, :W]),
                     r=[po, RB], w=[YB])

        emit_S(0)
        for n in range(len(items)):
            if n + 1 < len(items):
                emit_S(n + 1)
            emit_PV(n)


def phase_merge(K, ph, YA, YB, xres):
    P, D, l, HT, sb = K.P, K.D, K.l, K.HT, K.sb
    WG = sb(ph, "wg", [128, 8, 2048], BF16)
    PA = sb(ph, "pa", [128, 4, 1024], BF16)
    PB = sb(ph, "pb", [128, 4, 1024], BF16)
    WO = sb(ph, "wo", [128, 8, 1024], BF16)
    for i in range(4):
        P.dma("pool", WG.t[:, :, i * 512:(i + 1) * 512], D["w_g"][l][:, :, i * 512:(i + 1) * 512], w=[WG])
    P.dma("pool", PA.t[:], D["pa"][l], w=[PA])
    P.dma("pool", PB.t[:], D["pb"][l], w=[PB])
    P.dma("pool", WO.t[:], D["wo"][l], w=[WO])
    MG = sb(ph, "mg", [128, 8, 512], BF16)
    XB = [sb(ph, f"xb{i}", [128, 8, 512], F32) for i in range(1)]
    SA = sb(ph, "m_sa", [128, 512], F32)
    SBt = sb(ph, "m_sb", [128, 512], F32)
    M1 = sb(ph, "m_m1", [128, 512], F32)
    M2 = sb(ph, "m_m2", [128, 512], F32)
    for bi, (n0, nb) in enumerate(BLKS):
        blk = slice(n0, n0 + nb)
        col = 1 if n0 == 0 else 0
        xb = XB[0]
        P.dma("sp", xb.t[:, :, :nb], xres[:, :, blk], w=[xb])
        for oc in range(8):
            ocs = slice(oc * 128, (oc + 1) * 128)
            pga, pgb, ppa, ppb = K.ps(), K.ps(), K.ps(), K.ps()
            for kc in range(8):
                P.op("pe", lambda e: e.matmul(pga.t[:, :nb], lhsT=WG.t[:, kc, oc * 128:(oc + 1) * 128],
                                              rhs=HT.t[:, kc, blk], start=(kc == 0), stop=(kc == 7)), r=[WG, HT], w=[pga])
            for kc in range(8):
                P.op("pe", lambda e: e.matmul(pgb.t[:, :nb], lhsT=WG.t[:, kc, 1024 + oc * 128:1024 + (oc + 1) * 128],
                                              rhs=HT.t[:, kc, blk], start=(kc == 0), stop=(kc == 7)), r=[WG, HT], w=[pgb])
            for kc in range(4):
                P.op("pe", lambda e: e.matmul(ppa.t[:, :nb], lhsT=PA.t[:, kc, ocs], rhs=YA.t[:, kc, blk],
                                              start=(kc == 0), stop=(kc == 3)), r=[PA, YA], w=[ppa])
            for kc in range(4):
                P.op("pe", lambda e: e.matmul(ppb.t[:, :nb], lhsT=PB.t[:, kc, ocs], rhs=YB.t[:, kc, blk],
                                              start=(kc == 0), stop=(kc == 3)), r=[PB, YB], w=[ppb])
            P.op("act", lambda e: e.activation(out=SA.t[:, :nb], in_=pga.t[:, :nb], func=AF.Sigmoid), r=[pga], w=[SA])
            P.op("act", lambda e: e.activation(out=SBt.t[:, :nb], in_=pgb.t[:, :nb], func=AF.Sigmoid), r=[pgb], w=[SBt])
            P.op("dve", lambda e: e.tensor_mul(out=M1.t[:, :nb], in0=SA.t[:, :nb], in1=ppa.t[:, :nb]), r=[SA, ppa], w=[M1])
            P.op("dve", lambda e: e.tensor_mul(out=M2.t[:, :nb], in0=SBt.t[:, :nb], in1=ppb.t[:, :nb]), r=[SBt, ppb], w=[M2])
            P.op("dve", lambda e: e.tensor_add(out=MG.t[:, oc, :nb], in0=M1.t[:, :nb], in1=M2.t[:, :nb]),
                 r=[M1, M2], w=[MG])
        for oc in range(8):
            po = K.ps()
            for kc in range(8):
                P.op("pe", lambda e: e.matmul(po.t[:, :nb], lhsT=WO.t[:, kc, oc * 128:(oc + 1) * 128],
                                              rhs=MG.t[:, kc, :nb], start=(kc == 0), stop=(kc == 7)), r=[WO, MG], w=[po])
            P.op("dve", lambda e: e.scalar_tensor_tensor(out=xb.t[:, oc, :nb], in0=po.t[:, :nb],
                                                         scalar=K.mt.t[:, 16 + oc, col:col + 1], in1=xb.t[:, oc, :nb],
                                                         op0=ALU.mult, op1=ALU.add), r=[po, K.mt, xb], w=[xb])
        P.dma("sp", xres[:, :, blk], xb.t[:, :, :nb], r=[xb], w=[Res()])


def phase_moe(K, ph, XT):
    P, D, l, HT, sb = K.P, K.D, K.l, K.HT, K.sb
    GT = sb(ph, "gt", [16, NT], BF16)
    K.sel16 = sb(ph, "sel16", [16, 16, 128], BF16)
    P.dma("pool", K.sel16.t[:], D["c_sel16"], w=[K.sel16])
    RT = [sb(ph, f"r_{n}", [128, 16], F32) for n in ["s", "sel", "msk", "num"]]
    R4 = [sb(ph, f"r4_{i}", [128, 4], F32) for i in range(10)]
    R1 = [sb(ph, f"r1_{i}", [128, 1], F32) for i in range(3)]
    GTM = sb(ph, "gtm", [128, 4, 16], F32)

    def router(n0, nb, HF):
        S, SEL, MSK, NUM = RT
        ntile = nb // 128
        for ti in range(ntile):
            pr = K.ps()
            for c in range(8):
                P.op("pe", lambda e: e.matmul(pr.t[:, 0:16], lhsT=HF.t[:, c, ti * 128:(ti + 1) * 128], rhs=K.rwt.t[:, c, :],
                                              start=(c == 0), stop=(c == 7)), r=[HF, K.rwt], w=[pr])
            P.op("act", lambda e: e.activation(out=S.t[:], in_=pr.t[:, 0:16], func=AF.Sigmoid), r=[pr], w=[S])
            P.op("dve", lambda e: e.tensor_add(out=SEL.t[:], in0=S.t[:], in1=K.rbias.t[:]), r=[S, K.rbias], w=[SEL])
            sv = SEL.t[:].rearrange("p (g j) -> p g j", j=4)
            a, b, c_, d_ = [sv[:, :, j] for j in range(4)]
            pq, qq, rr, ss, m1, t1, t2, m2, gs, gm = R4

            def tt(o, x, y, op, rd, wr):
                P.op("dve", lambda e: e.tensor_tensor(out=o, in0=x, in1=y, op=op), r=rd, w=wr)

            tt(pq.t[:], a, b, ALU.max, [SEL], [pq])
            tt(qq.t[:], a, b, ALU.min, [SEL], [qq])
            tt(rr.t[:], c_, d_, ALU.max, [SEL], [rr])
            tt(ss.t[:], c_, d_, ALU.min, [SEL], [ss])
            tt(m1.t[:], pq.t[:], rr.t[:], ALU.max, [pq, rr], [m1])
            tt(t1.t[:], pq.t[:], rr.t[:], ALU.min, [pq, rr], [t1])
            tt(t2.t[:], qq.t[:], ss.t[:], ALU.max, [qq, ss], [t2])
            tt(m2.t[:], t1.t[:], t2.t[:], ALU.max, [t1, t2], [m2])
            tt(gs.t[:], m1.t[:], m2.t[:], ALU.add, [m1, m2], [gs])
            gmax, den, rden = R1
            P.op("dve", lambda e: e.tensor_reduce(out=gmax.t[:], in_=gs.t[:], axis=AX.X, op=ALU.max), r=[gs], w=[gmax])
            P.op("dve", lambda e: e.tensor_scalar(out=gm.t[:], in0=gs.t[:], scalar1=gmax.t[:, 0:1], scalar2=None,
                                                  op0=ALU.is_ge), r=[gs, gmax], w=[gm])
            mv = MSK.t[:].rearrange("p (g j) -> p g j", j=4)
            P.op("dve", lambda e: e.tensor_tensor(out=mv, in0=sv, in1=m2.t[:].unsqueeze(2).to_broadcast([128, 4, 4]),
                                                  op=ALU.is_ge), r=[SEL, m2], w=[MSK])
            P.op("dve", lambda e: e.tensor_tensor(out=mv, in0=mv, in1=gm.t[:].unsqueeze(2).to_broadcast([128, 4, 4]),
                                                  op=ALU.mult), r=[MSK, gm], w=[MSK])
            tt(NUM.t[:], MSK.t[:], S.t[:], ALU.mult, [MSK, S], [NUM])
            P.op("dve", lambda e: e.tensor_reduce(out=den.t[:], in_=NUM.t[:], axis=AX.X, op=ALU.add), r=[NUM], w=[den])
            P.op("dve", lambda e: e.reciprocal(out=rden.t[:], in_=den.t[:]), r=[den], w=[rden])
            P.op("dve", lambda e: e.tensor_scalar_mul(out=GTM.t[:, ti, :], in0=NUM.t[:], scalar1=rden.t[:, 0:1]),
                 r=[NUM, rden], w=[GTM])
        pT = K.ps()
        for ti in range(ntile):
            P.op("pe", lambda e: e.transpose(pT.t[0:16, ti * 128:(ti + 1) * 128], GTM.t[:, ti, :], K.identf.t[:]),
                 r=[GTM, K.identf], w=[pT])
        P.op("act", lambda e: e.activation(out=GT.t[:, n0:n0 + nb], in_=pT.t[0:16, :nb], func=AF.Identity), r=[pT], w=[GT])

    with ExitStack() as nph:
        phase_norm(K, nph, XT, K.mul2, 24, router)
        P.barrier()
    W1 = [sb(ph, f"w1_{i}", [128, 8, 512], BF16) for i in range(2)]
    W3 = [sb(ph, f"w3_{i}", [128, 8, 512], BF16) for i in range(2)]
    W2 = [sb(ph, f"w2_{i}", [128, 4, 1024], BF16) for i in range(2)]
    GB = sb(ph, "gb", [128, 512], F32)
    SL = [sb(ph, f"sl{i}", [128, 512], F32) for i in range(2)]
    T2 = [sb(ph, f"t2{i}", [128, 512], F32) for i in range(2)]
    HG = sb(ph, "hg", [128, 4, 512], BF16)

    def load(e):
        P.dma("pool", W1[e % 2].t[:], D["w1"][l, e], w=[W1[e % 2]])
        P.dma("pool", W3[e % 2].t[:], D["w3"][l, e], w=[W3[e % 2]])
        P.dma("pool", W2[e % 2].t[:], D["w2"][l, e], w=[W2[e % 2]])

    import os as _os
    NOLOAD = _os.environ.get("MOE_NOLOAD") == "1"
    load(0)
    for ex in range(16):
        if ex + 1 < 16 and not (NOLOAD and ex >= 1):
            load(ex + 1)
        w1, w3, w2 = W1[ex % 2], W3[ex % 2], W2[ex % 2]
        for (n0, nb) in BLKS:
            blk = slice(n0, n0 + nb)
            col = 1 if n0 == 0 else 0
            pg = K.ps()
            P.op("pe", lambda e: e.matmul(pg.t[:, :nb], lhsT=K.sel16.t[:, ex, :], rhs=GT.t[:, blk], start=True, stop=True),
                 r=[K.sel16, GT], w=[pg])
            P.op("act", lambda e: e.activation(out=GB.t[:, :nb], in_=pg.t[:, :nb], func=AF.Identity), r=[pg], w=[GB])
            for hc in range(4):
                hs = slice(hc * 128, (hc + 1) * 128)
                p1, p3 = K.ps(), K.ps()
                for kc in range(8):
                    P.op("pe", lambda e: e.matmul(p1.t[:, :nb], lhsT=w1.t[:, kc, hs], rhs=HT.t[:, kc, blk],
                                                  start=(kc == 0), stop=(kc == 7)), r=[w1, HT], w=[p1])
                for kc in range(8):
                    P.op("pe", lambda e: e.matmul(p3.t[:, :nb], lhsT=w3.t[:, kc, hs], rhs=HT.t[:, kc, blk],
                                                  start=(kc == 0), stop=(kc == 7)), r=[w3, HT], w=[p3])
                sl, t2 = SL[hc % 2], T2[hc % 2]
                P.op("act", lambda e: e.activation(out=sl.t[:, :nb], in_=p1.t[:, :nb], func=AF.Silu), r=[p1], w=[sl])
                P.op("dve", lambda e: e.tensor_mul(out=t2.t[:, :nb], in0=sl.t[:, :nb], in1=p3.t[:, :nb]), r=[sl, p3], w=[t2])
                P.op("dve", lambda e: e.tensor_mul(out=HG.t[:, hc, :nb], in0=t2.t[:, :nb], in1=GB.t[:, :nb]),
                     r=[t2, GB], w=[HG])
            for oc in range(8):
                po = K.ps()
                for hc in range(4):
                    P.op("pe", lambda e: e.matmul(po.t[:, :nb], lhsT=w2.t[:, hc, oc * 128:(oc + 1) * 128],
                                                  rhs=HG.t[:, hc, :nb], start=(hc == 0), stop=(hc == 3)), r=[w2, HG], w=[po])
                P.op("dve", lambda e: e.scalar_tensor_tensor(out=XT.t[:, oc, blk], in0=po.t[:, :nb],
                                                             scalar=K.mt.t[:, 40 + oc, col:col + 1], in1=XT.t[:, oc, blk],
                                                             op0=ALU.mult, op1=ALU.add), r=[po, K.mt, XT], w=[XT])


def kernel(**inputs):
    maps = _prep(inputs)
    nc = build()
    res = run_bass_kernel_spmd(nc, maps, core_ids=list(range(8)))
    out = np.zeros((8, 2048, 1024), np.float32)
    for b in range(8):
        y = res.results[b]["yout"]
        out[b] = y.transpose(2, 1, 0).reshape(2048, 1024)
    return out
```

```python
import numpy as np
import concourse.bass as bass
import concourse.mybir as mybir
from concourse.bass_utils import run_bass_kernel_spmd
from contextlib import ExitStack

F32 = mybir.dt.float32
BF16 = mybir.dt.bfloat16
ALU = mybir.AluOpType
AF = mybir.ActivationFunctionType
AX = mybir.AxisListType

L = 2
NT = 2304
NCTX = 256
NCH = 36
BLKS = [(0, 256), (256, 512), (768, 512), (1280, 512), (1792, 512)]
RMS_EPS = 1e-6
GN_EPS = 64e-5
DEC_C = -0.6065306597126334
NEG = -30000.0

SEM_LIMIT = 30000
NDMA = 16
NSW = 12
import os as _os0
ELT_ENG = _os0.environ.get('ELT_ENG', 'dve')
SW_CLEAR = False


class Res:
    __slots__ = ("lw", "rd", "grp")

    def __init__(self):
        self.lw = None
        self.rd = {}
        self.grp = None


class PEProxy:
    def __init__(self, prog):
        self.P = prog
        self.pe = prog.nc.tensor
        self.cur_w = None

    def _chk(self, k_ap):
        grp = (k_ap.base_partition(), k_ap.partition_size())
        for x in self.cur_w:
            if x.grp is not None and x.grp != grp and not x.rd:
                self.P.fence_pe()
            x.grp = grp

    def matmul(self, out, lhsT, rhs, **kw):
        self._chk(lhsT)
        return self.pe.matmul(out, lhsT=lhsT, rhs=rhs, **kw)

    def transpose(self, out, in_, identity):
        self._chk(in_)
        return self.pe.transpose(out, in_, identity)


class Tl:
    def __init__(self, t):
        self.t = t
        self.r = Res()


class Prog:
    def __init__(self, nc, stack):
        self.nc = nc
        self.stack = stack
        self.engs = {"pe": nc.tensor, "dve": nc.vector, "act": nc.scalar,
                     "pool": nc.gpsimd, "sp": nc.sync}
        self.nsem = 0
        self.sem = {k: self._newsem(k) for k in self.engs}
        self.cnt = {k: 0 for k in self.engs}
        self.known = {k: {} for k in self.engs}
        self.dma_sems = [self._newsem("dma") for _ in range(NDMA)]
        self.dma_cnt = [0] * NDMA
        self.dma_next = 0
        self.n_ins = 0
        self.n_wait = 0
        self.last_ev = {}
        self.pep = PEProxy(self)

    def _newsem(self, name):
        self.nsem += 1
        return self.stack.enter_context(self.nc.semaphore(f"{name}_{self.nsem}"))

    def _learn(self, eng, sem, val, snap):
        kn = self.known[eng]
        k = id(sem)
        if kn.get(k, 0) < val:
            kn[k] = val
        for k2, v2 in snap.items():
            if kn.get(k2, 0) < v2:
                kn[k2] = v2

    def _wait(self, eng, deps):
        e = self.engs[eng]
        kn = self.known[eng]
        todo = [d for d in deps if d is not None and not (d[2] == "pe" and eng == "pe")]
        todo.sort(key=lambda d: -d[1])
        for (sem, val, src, snap) in todo:
            if kn.get(id(sem), 0) >= val:
                continue
            e.wait_ge(sem, val)
            self.n_wait += 1
            self._learn(eng, sem, val, snap)

    @staticmethod
    def _deps(r, w):
        deps = []
        for x in r:
            deps.append(x.lw)
        for x in w:
            deps.append(x.lw)
            deps.extend(x.rd.values())
        return deps

    def op(self, eng, fn, r=(), w=()):
        r = [x.r if isinstance(x, Tl) else x for x in r]
        w = [x.r if isinstance(x, Tl) else x for x in w]
        self._wait(eng, self._deps(r, w))
        if eng == "pe":
            self.pep.cur_w = w
            ins = fn(self.pep)
        else:
            ins = fn(self.engs[eng])
        if self.cnt[eng] >= SEM_LIMIT:
            self.sem[eng] = self._newsem(eng)
            self.cnt[eng] = 0
        self.cnt[eng] += 1
        ins.then_inc(self.sem[eng], 1)
        ev = (self.sem[eng], self.cnt[eng], eng, dict(self.known[eng]))
        self.last_ev[eng] = ev
        for x in r:
            x.rd[eng] = ev
        for x in w:
            x.lw = ev
            x.rd = {}
        self.n_ins += 1
        return ins

    def dma(self, q, out, in_, r=(), w=(), **kw):
        r = [x.r if isinstance(x, Tl) else x for x in r]
        w = [x.r if isinstance(x, Tl) else x for x in w]
        deps = self._deps(r, w)
        i = self.dma_next
        self.dma_next = (i + 1) % NDMA
        sem = self.dma_sems[i]
        if self.dma_cnt[i] > 0:
            deps.append((sem, self.dma_cnt[i], "dma", {}))
        self._wait(q, deps)
        ins = self.engs[q].dma_start(out=out, in_=in_, **kw)
        self.dma_cnt[i] += 16
        ins.then_inc(sem, 16)
        ev = (sem, self.dma_cnt[i], "dma", dict(self.known[q]))
        key = ("dma", i)
        for x in r:
            x.rd[key] = ev
        for x in w:
            x.lw = ev
            x.rd = {}
        self.n_ins += 1
        return ins

    def fence_pe(self):
        ev = self.last_ev.get("pe")
        if ev is not None:
            sem, val, _, snap = ev
            self.engs["pe"].wait_ge(sem, val)
            self._learn("pe", sem, val, snap)
            self.n_wait += 1

    def barrier(self):
        evs = list(self.last_ev.values())
        for i in range(NDMA):
            if self.dma_cnt[i] > 0:
                evs.append((self.dma_sems[i], self.dma_cnt[i], "dma", {}))
        for e in self.engs:
            self._wait(e, [x for x in evs if x[2] != e])

    def finish(self, res_list):
        deps = [x.r.lw if isinstance(x, Tl) else x.lw for x in res_list]
        self._wait("sp", deps)


def _cm(a, K):
    a = np.asarray(a)
    return np.ascontiguousarray(a.reshape(K, 128, -1).transpose(1, 0, 2))


def _consts():
    c = {}
    s = np.arange(64)[:, None]
    t = np.arange(64)[None, :]
    mc = np.zeros((64, 2, 128), np.float32)
    mc[:, 0, :64] = (s < t)
    mc[:, 0, 64:] = (s <= t)
    mc[:, 1, :64] = (s > t)
    mc[:, 1, 64:] = (s >= t)
    c["c_maskc"] = mc
    ma = np.zeros((64, 2, 64), np.float32)
    ma[:, 0, :] = (t < s)
    ma[:, 1, :] = (t > s)
    c["c_maska"] = ma
    c["c_maska2"] = np.ascontiguousarray(ma.transpose(1, 0, 2).reshape(128, 64))
    c["c_ident"] = np.eye(128, dtype=np.float32)
    rm = np.ones((128, 512), np.float32)
    rm[:, ::64] = 0.0
    c["c_rmask"] = rm
    bo = np.zeros((128, 128), np.float32)
    bo[:64, :64] = 1.0
    bo[64:, 64:] = 1.0
    c["c_bones"] = bo
    c["c_ones"] = np.ones((128, 128), np.float32)
    sel = np.zeros((16, 16, 128), np.float32)
    for e in range(16):
        sel[e, e, :] = 1.0
    c["c_sel16"] = sel
    jk = np.arange(64)[:, None]
    jq = np.arange(64)[None, :]
    cs = np.clip(jq - 8, 0, 48)
    inwin = (jk >= cs) & (jk < cs + 16)
    nm = np.where(inwin, 0.0, NEG).astype(np.float32)
    c["c_namask"] = np.ascontiguousarray(np.concatenate([nm, nm], 0))
    return c


def _prep(inp):
    g = {k: np.asarray(v) for k, v in inp.items()}
    sh = {}
    sh["modw"] = np.ascontiguousarray(g["mod_w"].reshape(L, 8, 128, 6, 1024).transpose(0, 3, 2, 1, 4))
    sh["modb"] = np.ascontiguousarray(g["mod_b"].reshape(L, 48, 128).transpose(0, 2, 1))
    sh["g12"] = np.ascontiguousarray(
        np.stack([g["norm1_g"], g["norm2_g"]], 1).reshape(L, 2, 8, 128).transpose(0, 3, 1, 2))
    Wc = np.stack([_cm(g["w_in"][l], 8) for l in range(L)])
    sh["w_lx"] = np.ascontiguousarray(Wc[:, :, :, 1536:1920])
    rkv = np.zeros((L, 8, 128, 8, 320), np.float32)
    wqk = np.zeros((L, 8, 128, 8, 128), np.float32)
    for h in range(8):
        r_ = Wc[:, :, :, h * 64:(h + 1) * 64]
        k_ = Wc[:, :, :, 512 + h * 64:512 + (h + 1) * 64]
        v_ = Wc[:, :, :, 1024 + h * 64:1024 + (h + 1) * 64]
        rkv[:, h] = np.concatenate([r_, r_, k_, k_, v_], -1)
        wqk[:, h] = np.concatenate([Wc[:, :, :, 1920 + h * 64:1920 + (h + 1) * 64],
                                    Wc[:, :, :, 2432 + h * 64:2432 + (h + 1) * 64]], -1)
    sh["w_rkv"] = rkv
    sh["w_qk"] = wqk
    sh["w_vb"] = np.ascontiguousarray(Wc[:, :, :, 2944:3456])
    sh["w_g"] = np.ascontiguousarray(Wc[:, :, :, 3456:5504])
    wlb = np.zeros((L, 8, 128, 128), np.float32)
    alb = np.zeros((L, 8, 128, 128), np.float32)
    rwp = np.zeros((L, 128, 8, 7), np.float32)
    for h in range(8):
        hs = slice(h * 64, (h + 1) * 64)
        for d in range(2):
            ds = slice(d * 64, (d + 1) * 64)
            wlb[:, h, ds, ds] = g["rw_w_lora_b"][:, d, :, hs]
            alb[:, h, ds, ds] = g["rw_a_lora_b"][:, d, :, hs]
            rwp[:, ds, h, 0] = g["rw_w0"][:, d, hs]
            rwp[:, ds, h, 1] = g["rw_a0"][:, d, hs]
            rwp[:, ds, h, 2] = g["rw_k_k"][:, hs]
            rwp[:, ds, h, 3] = g["rw_k_a"][:, hs]
            rwp[:, ds, h, 4] = g["rw_r_k"][:, h, :]
            rwp[:, ds, h, 5] = g["rw_ln_g"][:, hs]
            rwp[:, ds, h, 6] = g["rw_ln_b"][:, hs]
    sh["wlb"] = wlb
    sh["alb"] = alb
    sh["rwp"] = rwp
    sh["glb"] = np.ascontiguousarray(g["rw_g_lora_b"])
    sh["nap"] = np.ascontiguousarray(np.stack([g["na_q_g"], g["na_k_g"]], -1))
    jk = np.arange(64)[:, None]
    jq = np.arange(64)[None, :]
    dcol = np.clip(jk - jq, -15, 15) + 15
    rp = g["na_rpb"][:, :, :, dcol]
    sh["rpbT"] = np.ascontiguousarray(rp.transpose(0, 1, 3, 2, 4))
    sh["pa"] = np.stack([_cm(g["proj_a"][l], 4) for l in range(L)])
    sh["pb"] = np.stack([_cm(g["proj_b"][l], 4) for l in range(L)])
    sh["wo"] = np.stack([_cm(g["w_out"][l], 8) for l in range(L)])
    sh["rw"] = _cm(g["router_w"], 8)
    sh["rbias"] = np.ascontiguousarray(np.broadcast_to(g["router_bias"][None, :], (128, 16)))
    sh["w1"] = np.ascontiguousarray(g["moe_w1"].reshape(L, 16, 8, 128, 512).transpose(0, 1, 3, 2, 4))
    sh["w3"] = np.ascontiguousarray(g["moe_w3"].reshape(L, 16, 8, 128, 512).transpose(0, 1, 3, 2, 4))
    sh["w2"] = np.ascontiguousarray(g["moe_w2"].reshape(L, 16, 4, 128, 1024).transpose(0, 1, 3, 2, 4))
    sh.update(_consts())
    maps = []
    for b in range(8):
        m = dict(sh)
        cat = np.concatenate([g["ctx"][b], g["x"][b]], 0)
        m["xin"] = _cm(np.ascontiguousarray(cat.T), 8)
        m["cs"] = _cm(np.stack([g["c"][b], g["c_ctx"]], -1), 8)
        maps.append(m)
    return maps


SHAPES = {
    "xin": [128, 8, NT], "cs": [128, 8, 2],
    "modw": [L, 6, 128, 8, 1024], "modb": [L, 128, 48], "g12": [L, 128, 2, 8],
    "w_lx": [L, 128, 8, 384], "w_rkv": [L, 8, 128, 8, 320], "w_qk": [L, 8, 128, 8, 128],
    "w_vb": [L, 128, 8, 512], "w_g": [L, 128, 8, 2048],
    "wlb": [L, 8, 128, 128], "alb": [L, 8, 128, 128], "rwp": [L, 128, 8, 7], "glb": [L, 128, 512],
    "nap": [L, 64, 2], "rpbT": [L, 8, 64, 15, 64],
    "pa": [L, 128, 4, 1024], "pb": [L, 128, 4, 1024], "wo": [L, 128, 8, 1024],
    "rw": [128, 8, 16], "rbias": [128, 16],
    "w1": [L, 16, 128, 8, 512], "w3": [L, 16, 128, 8, 512], "w2": [L, 16, 128, 4, 1024],
    "c_maskc": [64, 2, 128], "c_maska": [64, 2, 64], "c_maska2": [128, 64], "c_ident": [128, 128], "c_rmask": [128, 512],
    "c_bones": [128, 128], "c_ones": [128, 128], "c_sel16": [16, 16, 128], "c_namask": [128, 64],
}


class Ctx:
    pass


def build(n_layers=L, dbg=(), skip=()):
    nc = bass.Bass("TRN2", target_bir_lowering=False)
    D = {k: nc.dram_tensor(k, s, F32, kind="ExternalInput").ap() for k, s in SHAPES.items()}
    yout = nc.dram_tensor("yout", [128, 8, NT - NCTX], F32, kind="ExternalOutput").ap()
    xres = nc.dram_tensor("xres", [128, 8, NT], F32, kind="Internal").ap()
    dbg_out = {}
    for name in dbg:
        dbg_out[name] = nc.dram_tensor("dbg_" + name, [128, 8, NT], F32, kind="ExternalOutput").ap()
    K = Ctx()
    K.nc, K.D, K.dbg = nc, D, dbg_out
    K.skip = skip
    with ExitStack() as st:
        P = Prog(nc, st)
        K.P = P

        K.nsb = 0

        def sb(stack, name, shape, dt):
            K.nsb += 1
            return Tl(stack.enter_context(nc.sbuf_tensor(f"s{K.nsb}_{name}", shape, dt)))

        K.sb = sb
        K.PS = [Tl(st.enter_context(nc.psum_tensor(f"ps{i}", [128, 512], F32))) for i in range(7)]
        K.PSB = Tl(st.enter_context(nc.psum_tensor("psb", [128, 1024], BF16)))
        K.ps_i = 0

        def ps():
            t = K.PS[K.ps_i % 5]
            K.ps_i += 1
            return t

        K.ps = ps
        K.PSL = K.PS[6]
        K.PSL2 = K.PS[5]
        K.identf = sb(st, "identf", [128, 128], F32)
        K.identb = sb(st, "identb", [128, 128], BF16)
        K.onesb = sb(st, "onesb", [128, 128], BF16)
        K.onesf = sb(st, "onesf", [128, 128], F32)
        K.bones = sb(st, "bones", [128, 128], F32)
        K.maskc = sb(st, "maskc", [64, 2, 128], F32)
        K.maska = sb(st, "maska", [64, 2, 64], F32)
        K.maska2 = sb(st, "maska2", [128, 64], F32)
        K.rmask = sb(st, "rmask", [128, 512], F32)
        K.namask = sb(st, "namask", [128, 64], F32)
        K.rwt = sb(st, "rwt", [128, 8, 16], F32)
        K.rbias = sb(st, "rbias", [128, 16], F32)
        K.cs = sb(st, "cs", [128, 8, 2], F32)
        K.csb = sb(st, "csb", [128, 8, 2], BF16)
        for t, nm in [(K.identf, "c_ident"), (K.onesf, "c_ones"), (K.bones, "c_bones"), (K.maskc, "c_maskc"),
                      (K.maska, "c_maska"), (K.maska2, "c_maska2"), (K.rmask, "c_rmask"), (K.namask, "c_namask"),
                      (K.rwt, "rw"), (K.rbias, "rbias"), (K.cs, "cs")]:
            P.dma("sp", t.t[:], D[nm], w=[t])
        P.dma("pool", K.identb.t[:], D["c_ident"], w=[K.identb])
        P.dma("pool", K.onesb.t[:], D["c_ones"], w=[K.onesb])
        K.maskcb = sb(st, "maskcb", [64, 2, 128], BF16)
        K.maskab = sb(st, "maskab", [64, 2, 64], BF16)
        P.dma("pool", K.maskcb.t[:], D["c_maskc"], w=[K.maskcb])
        P.dma("pool", K.maskab.t[:], D["c_maska"], w=[K.maskab])
        P.op("act", lambda e: e.activation(out=K.csb.t[:], in_=K.cs.t[:], func=AF.Silu), r=[K.cs], w=[K.csb])
        K.mt = sb(st, "mt", [128, 48, 2], F32)
        K.mul1 = sb(st, "mul1", [128, 8, 2], F32)
        K.mul2 = sb(st, "mul2", [128, 8, 2], F32)
        K.g12 = sb(st, "g12", [128, 2, 8], F32)
        K.modb = sb(st, "modb", [128, 48], F32)
        K.HT = sb(st, "HT", [128, 8, NT], BF16)

        for l in range(n_layers):
            K.l = l
            with ExitStack() as ph:
                XT = sb(ph, "XT", [128, 8, NT], F32)
                P.dma("sp", XT.t[:], D["xin"] if l == 0 else xres, w=[XT])
                phase_adaln(K, ph)
                phase_norm(K, ph, XT, K.mul1, 0, None)
                if "h%d" % l in dbg_out:
                    dump_bf(K, ph, K.HT, dbg_out["h%d" % l])
                if l == 0:
                    P.dma("sp", xres, XT.t[:], r=[XT], w=[Res()])
                P.barrier()
            with ExitStack() as ph:
                YA = sb(ph, "YA", [128, 4, NT], BF16)
                if "rwkv" in K.skip:
                    P.op("dve", lambda e: e.memset(YA.t[:], 0.0), w=[YA])
                else:
                    with ExitStack() as ph2:
                        phase_rwkv(K, ph2, YA)
                        P.barrier()
                if "ya%d" % l in dbg_out:
                    with ExitStack() as ph2:
                        dump_bf(K, ph2, YA, dbg_out["ya%d" % l], 4)
                        P.barrier()
                YB = sb(ph, "YB", [128, 4, NT], BF16)
                if "na" in K.skip:
                    P.op("dve", lambda e: e.memset(YB.t[:], 0.0), w=[YB])
                else:
                    with ExitStack() as ph2:
                        phase_na(K, ph2, YB)
                        P.barrier()
                if "yb%d" % l in dbg_out:
                    with ExitStack() as ph2:
                        dump_bf(K, ph2, YB, dbg_out["yb%d" % l], 4)
                        P.barrier()
                with ExitStack() as ph2:
                    phase_merge(K, ph2, YA, YB, xres)
                    P.barrier()
            with ExitStack() as ph:
                XT = sb(ph, "XT2", [128, 8, NT], F32)
                P.dma("sp", XT.t[:], xres, w=[XT])
                if "xm%d" % l in dbg_out:
                    P.dma("sp", dbg_out["xm%d" % l], XT.t[:], r=[XT], w=[Res()])
                if "moe" not in K.skip:
                    phase_moe(K, ph, XT)
                if l == n_layers - 1:
                    ry = Res()
                    P.dma("sp", yout, XT.t[:, :, NCTX:NT], r=[XT], w=[ry])
                    P.finish([ry])
                else:
                    rx = Res()
                    P.dma("sp", xres, XT.t[:], r=[XT], w=[rx])
                if "xo%d" % l in dbg_out:
                    P.dma("sp", dbg_out["xo%d" % l], XT.t[:], r=[XT], w=[Res()])
                P.barrier()
        print("instructions", P.n_ins, "waits", P.n_wait, "sems", P.nsem)
    return nc


def dump_bf(K, ph, src, dst, nchunk=8):
    P = K.P
    tmp = K.sb(ph, "dbgtmp", [128, NT], F32)
    for c in range(nchunk):
        P.op("dve", lambda e: e.tensor_copy(out=tmp.t[:], in_=src.t[:, c, :]), r=[src], w=[tmp])
        P.dma("sp", dst[:, c, :], tmp.t[:], r=[tmp], w=[Res()])


def phase_adaln(K, ph):
    P, D, l = K.P, K.D, K.l
    P.dma("sp", K.g12.t[:], D["g12"][l], w=[K.g12])
    P.dma("sp", K.modb.t[:], D["modb"][l], w=[K.modb])
    MW = [K.sb(ph, f"mw{i}", [128, 8, 1024], BF16) for i in range(2)]
    pm = K.ps()
    for g in range(6):
        mw = MW[g % 2]
        P.dma("pool", mw.t[:], D["modw"][l, g], w=[mw])
        for j in range(8):
            o = (g * 8 + j) * 2
            for kc in range(8):
                P.op("pe", lambda e: e.matmul(pm.t[:, o:o + 2], lhsT=mw.t[:, kc, j * 128:(j + 1) * 128],
                                              rhs=K.csb.t[:, kc, :], start=(kc == 0), stop=(kc == 7)),
                     r=[mw, K.csb], w=[pm])
    P.op("dve", lambda e: e.tensor_tensor(out=K.mt.t[:], in0=pm.t[:, 0:96].rearrange("p (j c) -> p j c", c=2),
                                          in1=K.modb.t[:].unsqueeze(2).to_broadcast([128, 48, 2]), op=ALU.add),
         r=[pm, K.modb], w=[K.mt])
    for (mul, gi, so) in [(K.mul1, 0, 8), (K.mul2, 1, 32)]:
        P.op("dve", lambda e: e.tensor_scalar_add(out=mul.t[:], in0=K.mt.t[:, so:so + 8, :], scalar1=1.0),
             r=[K.mt], w=[mul])
        P.op("dve", lambda e: e.tensor_tensor(out=mul.t[:], in0=mul.t[:],
                                              in1=K.g12.t[:, gi, :].unsqueeze(2).to_broadcast([128, 8, 2]),
                                              op=ALU.mult), r=[mul, K.g12], w=[mul])


def phase_norm(K, ph, XT, mul, shift_off, h2f_cb):
    P = K.P
    SQ = K.sb(ph, "n_sq", [128, 8, 512], BF16)
    RS = K.sb(ph, "n_rs", [128, 512], F32)
    TMP = [K.sb(ph, f"n_tmp{i}", [128, 512], F32) for i in range(2)]
    HF = K.sb(ph, "n_hf", [128, 8, 512], F32) if h2f_cb is not None else None
    for (n0, nb) in BLKS:
        col = 1 if n0 == 0 else 0
        P.op("act", lambda e: e.activation(out=SQ.t[:, :, :nb], in_=XT.t[:, :, n0:n0 + nb], func=AF.Square),
             r=[XT], w=[SQ])
        pa = K.ps()
        for c in range(8):
            P.op("pe", lambda e: e.matmul(pa.t[:, :nb], lhsT=K.onesb.t[:], rhs=SQ.t[:, c, :nb],
                                          start=(c == 0), stop=(c == 7)), r=[K.onesb, SQ], w=[pa])
        P.op("act", lambda e: e.activation(out=RS.t[:, :nb], in_=pa.t[:, :nb], func=AF.Sqrt,
                                           bias=RMS_EPS, scale=1.0 / 1024), r=[pa], w=[RS])
        P.op("dve", lambda e: e.reciprocal(out=RS.t[:, :nb], in_=RS.t[:, :nb]), r=[RS], w=[RS])
        for c in range(8):
            tmp = TMP[c % 2]
            P.op("dve", lambda e: e.scalar_tensor_tensor(out=tmp.t[:, :nb], in0=XT.t[:, c, n0:n0 + nb],
                                                         scalar=mul.t[:, c, col:col + 1], in1=RS.t[:, :nb],
                                                         op0=ALU.mult, op1=ALU.mult),
                 r=[XT, mul, RS], w=[tmp])
            P.op("act", lambda e: e.activation(out=K.HT.t[:, c, n0:n0 + nb], in_=tmp.t[:, :nb], func=AF.Identity,
                                               bias=K.mt.t[:, shift_off + c, col:col + 1], scale=1.0),
                 r=[tmp, K.mt], w=[K.HT])
            if HF is not None:
                P.op("act", lambda e: e.activation(out=HF.t[:, c, :nb], in_=tmp.t[:, :nb], func=AF.Identity,
                                                   bias=K.mt.t[:, shift_off + c, col:col + 1], scale=1.0),
                     r=[tmp, K.mt], w=[HF])
        if h2f_cb is not None:
            h2f_cb(n0, nb, HF)


def phase_rwkv(K, ph, YA):
    P, D, l, HT, sb = K.P, K.D, K.l, K.HT, K.sb
    WLX = sb(ph, "wlx", [128, 8, 384], BF16)
    GLB = sb(ph, "glb", [128, 512], BF16)
    RWP = sb(ph, "rwp", [128, 8, 7], F32)
    OMKA = sb(ph, "omka", [128, 8], F32)
    P.dma("pool", WLX.t[:], D["w_lx"][l], w=[WLX])
    P.dma("pool", GLB.t[:], D["glb"][l], w=[GLB])
    P.dma("sp", RWP.t[:], D["rwp"][l], w=[RWP])
    P.op("dve", lambda e: e.tensor_scalar(out=OMKA.t[:], in0=RWP.t[:, :, 3], scalar1=-1.0, scalar2=1.0,
                                          op0=ALU.mult, op1=ALU.add), r=[RWP], w=[OMKA])
    TLW = sb(ph, "tlw", [128, NT], BF16)
    LA = sb(ph, "la", [128, NT], BF16)
    SLG = sb(ph, "slg", [128, NT], BF16)
    for (n0, nb) in BLKS:
        for j, (dst, fn) in enumerate([(TLW, AF.Tanh), (LA, AF.Identity), (SLG, AF.Sigmoid)]):
            p = K.ps()
            for kc in range(8):
                P.op("pe", lambda e: e.matmul(p.t[:, :nb], lhsT=WLX.t[:, kc, j * 128:(j + 1) * 128],
                                              rhs=HT.t[:, kc, n0:n0 + nb], start=(kc == 0), stop=(kc == 7)),
                     r=[WLX, HT], w=[p])
            P.op("act", lambda e: e.activation(out=dst.t[:, n0:n0 + nb], in_=p.t[:, :nb], func=fn), r=[p], w=[dst])

    import os as _os
    STOP = int(_os.environ.get("RW_STOP", "99"))
    P.op("dve", lambda e: e.memset(YA.t[:], 0.0), w=[YA])
    if STOP == 0:
        return
    WR = [sb(ph, f"wr{i}", [128, 8, 320], BF16) for i in range(1)]
    WL = [sb(ph, f"wl{i}", [128, 128], BF16) for i in range(2)]
    AL = [sb(ph, f"al{i}", [128, 128], BF16) for i in range(2)]
    QQ = sb(ph, "qq", [128, NCH, 128], BF16)
    KB = sb(ph, "kb", [128, NT], BF16)
    KKt = sb(ph, "kk", [128, NT], BF16)
    KH = sb(ph, "kh", [64, NCH, 128], BF16)
    BH = sb(ph, "bh", [64, NCH, 128], BF16)
    EG = sb(ph, "eg", [128, NCH], F32)
    EGS = sb(ph, "egs", [128, NCH], F32)
    BS = sb(ph, "bs", [64, NT], BF16)
    V64 = sb(ph, "v64", [64, NCH, 64], BF16)
    VT = sb(ph, "vt", [64, NT], BF16)
    YTd = [sb(ph, f"yt{d}", [64, NT], BF16) for d in range(2)]
    SCR = []
    for pi_ in range(2):
        sc = {n: sb(ph, f"rs{pi_}_" + n, [128, 512], F32) for n in ["rf", "kf", "af", "lw", "g", "s1", "s2", "s3"]}
        sc["kht"] = sb(ph, f"rs{pi_}_kht", [128, 512], BF16)
        sc["bht"] = sb(ph, f"rs{pi_}_bht", [128, 512], BF16)
        sc["tot"] = sb(ph, f"rs{pi_}_tot", [128, 8], F32)
        SCR.append(sc)
    Rf, Kf, Af, LW, G, S1, S2, S3 = [SCR[0][n] for n in ["rf", "kf", "af", "lw", "g", "s1", "s2", "s3"]]
    NBT = 4
    for t__ in (QQ, KB, KKt, KH, BH, EG, EGS, V64):
        t__.rb = [Res() for _ in BLKS]
    bof = lambda c: 0 if c < 4 else 1 + (c - 4) // 8
    NSET = 3
    ATm = [[sb(ph, f"atm{i}{d}", [64, NBT, 128], BF16) for d in range(2)] for i in range(NSET)]
    BTm = [[sb(ph, f"btm{i}{d}", [64, NBT, 128], BF16) for d in range(2)] for i in range(NSET)]
    SSs = [[sb(ph, f"ss{q}{i}", [128, NBT, 192], BF16) for i in range(2)] for q in range(2)]
    SSrs = [[[{k: Res() for k in "MXA"} for d in range(2)] for i in range(2)] for q in range(2)]
    X5 = [[sb(ph, f"x5{i}{d}", [64, NBT, 64], BF16) for d in range(2)] for i in range(NSET)]
    T1 = sb(ph, "t1", [64, 2, 64], BF16)
    NU = sb(ph, "nu", [64, 2, 64], BF16)
    Hf = sb(ph, "hf", [128, 64], F32)
    Hb = sb(ph, "hb", [128, 64], BF16)
    order1 = [3, 2, 1, 0] + list(range(35, 3, -1))

    def v3(ap, j=64):
        return ap.rearrange("p (c j) -> p c j", j=j)

    for h in range(8):
        wr, wl, al = WR[0], WL[h % 2], AL[h % 2]
        P.dma("pool", wr.t[:], D["w_rkv"][l, h], w=[wr])
        P.dma("pool", wl.t[:], D["wlb"][l, h], w=[wl])
        P.dma("pool", al.t[:], D["alb"][l, h], w=[al])
        prm = lambda i, lo=0, hi=128: RWP.t[lo:hi, h, i:i + 1]
        def gen_elem(bi, par_):
            sc_ = SCR[par_]
            Rf, Kf, Af, LW, G, S1, S2, S3 = [sc_[n] for n in ["rf", "kf", "af", "lw", "g", "s1", "s2", "s3"]]
            KHT, BHT, TOT = sc_["kht"], sc_["bht"], sc_["tot"]
            for bi in [bi]:
                n0, nb = BLKS[bi]
                ncb, c0 = nb // 64, n0 // 64
                blk = slice(n0, n0 + nb)

                def proj(dst, lo, hi, M, eng, outdt_t=None):
                    p = K.ps()
                    for kc in range(8):
                        P.op("pe", lambda e: e.matmul(p.t[0:M, :nb], lhsT=wr.t[:, kc, lo:hi], rhs=HT.t[:, kc, blk],
                                                      start=(kc == 0), stop=(kc == 7)), r=[wr, HT], w=[p])
                    if eng == "act":
                        P.op("act", lambda e: e.activation(out=dst, in_=p.t[0:M, :nb], func=AF.Identity), r=[p],
                             w=[outdt_t])
                    else:
                        P.op("dve", lambda e: e.tensor_copy(out=dst, in_=p.t[0:M, :nb]), r=[p], w=[outdt_t])

                yield
                proj(Rf.t[:, :nb], 0, 128, 128, "act", Rf)
                yield
                proj(Kf.t[:, :nb], 128, 256, 128, "dve", Kf)
                yield
                proj(VT.t[:, blk], 256, 320, 64, "act", VT)
                yield
                p = K.ps()
                P.op("pe", lambda e: e.matmul(p.t[:, :nb], lhsT=wl.t[:], rhs=TLW.t[:, blk], start=True, stop=True),
                     r=[wl, TLW], w=[p])
                P.op("act", lambda e: e.activation(out=LW.t[:, :nb], in_=p.t[:, :nb], func=AF.Sigmoid,
                                                   bias=prm(0), scale=1.0), r=[p, RWP], w=[LW])
                yield
                p = K.ps()
                P.op("pe", lambda e: e.matmul(p.t[:, :nb], lhsT=al.t[:], rhs=LA.t[:, blk], start=True, stop=True),
                     r=[al, LA], w=[p])
                P.op("act", lambda e: e.activation(out=Af.t[:, :nb], in_=p.t[:, :nb], func=AF.Sigmoid,
                                                   bias=prm(1), scale=1.0), r=[p, RWP], w=[Af])
                yield
                P.op("dve", lambda e: e.tensor_scalar_mul(out=S1.t[:, :nb], in0=Kf.t[:, :nb], scalar1=prm(2)),
                     r=[Kf, RWP], w=[S1])
                P.op("act", lambda e: e.activation(out=S2.t[:, :nb], in_=S1.t[:, :nb], func=AF.Square), r=[S1], w=[S2])
                yield
                p = K.ps()
                P.op("pe", lambda e: e.matmul(p.t[:, :nb], lhsT=K.bones.t[:], rhs=S2.t[:, :nb], start=True, stop=True),
                     r=[K.bones, S2], w=[p])
                P.op("act", lambda e: e.activation(out=S2.t[:, :nb], in_=p.t[:, :nb], func=AF.Sqrt), r=[p], w=[S2])
                P.op("dve", lambda e: e.tensor_scalar_max(out=S2.t[:, :nb], in0=S2.t[:, :nb], scalar1=1e-6),
                     r=[S2], w=[S2])
                yield
                P.op("dve", lambda e: e.reciprocal(out=S2.t[:, :nb], in_=S2.t[:, :nb]), r=[S2], w=[S2])
                P.op("dve", lambda e: e.tensor_mul(out=S1.t[:, :nb], in0=S1.t[:, :nb], in1=S2.t[:, :nb]),
                     r=[S1, S2], w=[S1])
                P.op("dve", lambda e: e.tensor_scalar(out=S2.t[:, :nb], in0=Af.t[:, :nb], scalar1=prm(3),
                                                      scalar2=OMKA.t[:, h:h + 1], op0=ALU.mult, op1=ALU.add),
                     r=[Af, RWP, OMKA], w=[S2])
                yield
                P.op("dve", lambda e: e.tensor_mul(out=S2.t[:, :nb], in0=S2.t[:, :nb], in1=Kf.t[:, :nb]),
                     r=[S2, Kf], w=[S2])
                P.op("dve", lambda e: e.tensor_mul(out=S3.t[:, :nb], in0=S1.t[:, :nb], in1=Af.t[:, :nb]),
                     r=[S1, Af], w=[S3])
                yield
                P.op("dve", lambda e: e.scalar_tensor_tensor(out=Af.t[0:64, :nb], in0=Rf.t[0:64, :nb], scalar=prm(4, 0, 64),
                                                             in1=Kf.t[0:64, :nb], op0=ALU.mult, op1=ALU.mult),
                     r=[Rf, Kf, RWP], w=[Af])
                yield
                p = K.ps()
                P.op("pe", lambda e: e.matmul(p.t[0:64, :nb], lhsT=K.bones.t[0:64, 0:64], rhs=Af.t[0:64, :nb],
                                              start=True, stop=True), r=[K.bones, Af], w=[p])
                P.op("act", lambda e: e.activation(out=BS.t[:, blk], in_=p.t[0:64, :nb], func=AF.Identity),
                     r=[p], w=[BS])
                if STOP == 1:
                    continue
                yield
                P.op("dve", lambda e: e.tensor_scalar_mul(out=LW.t[:, :nb], in0=LW.t[:, :nb], scalar1=DEC_C),
                     r=[LW], w=[LW])
                P.op("dve", lambda e: e.tensor_tensor_scan(out=G.t[:, :nb], data0=K.rmask.t[:, :nb], data1=LW.t[:, :nb],
                                                           initial=0.0, op0=ALU.mult, op1=ALU.add),
                     r=[K.rmask, LW], w=[G])
                P.op("dve", lambda e: e.tensor_copy(out=TOT.t[64:128, :ncb], in_=v3(G.t[64:128, :nb])[:, :, 63]),
                     r=[G], w=[TOT])
                yield
                P.op("dve", lambda e: e.tensor_sub(out=Af.t[64:128, :nb], in0=LW.t[64:128, :nb], in1=G.t[64:128, :nb]),
                     r=[LW, G], w=[Af])
                P.op("dve", lambda e: e.tensor_tensor(out=v3(G.t[64:128, :nb]), in0=v3(Af.t[64:128, :nb]),
                                                      in1=TOT.t[64:128, :ncb].unsqueeze(2).to_broadcast([64, ncb, 64]),
                                                      op=ALU.add), r=[Af, TOT], w=[G])
                P.op("dve", lambda e: e.tensor_sub(out=LW.t[:, :nb], in0=G.t[:, :nb], in1=LW.t[:, :nb]),
                     r=[G, LW], w=[LW])
                yield
                P.op("act", lambda e: e.activation(out=LW.t[:, :nb], in_=LW.t[:, :nb], func=AF.Exp), r=[LW], w=[LW])
                P.op("dve", lambda e: e.tensor_mul(out=QQ.t[:, c0:c0 + ncb, 0:64], in0=v3(S1.t[:, :nb]),
                                                   in1=v3(LW.t[:, :nb])), r=[S1, LW], w=[QQ.rb[bi]])
                P.op("act", lambda e: e.activation(out=S1.t[:, :nb], in_=G.t[:, :nb], func=AF.Exp), r=[G], w=[S1])
                yield
                P.op("dve", lambda e: e.tensor_mul(out=QQ.t[:, c0:c0 + ncb, 64:128], in0=v3(Rf.t[:, :nb]),
                                                   in1=v3(S1.t[:, :nb])), r=[Rf, S1], w=[QQ.rb[bi]])
                P.op("dve", lambda e: e.tensor_copy(out=EG.t[0:64, c0:c0 + ncb], in_=v3(S1.t[0:64, :nb])[:, :, 63]),
                     r=[S1], w=[EG.rb[bi]])
                P.op("dve", lambda e: e.tensor_copy(out=EG.t[64:128, c0:c0 + ncb], in_=v3(S1.t[64:128, :nb])[:, :, 0]),
                     r=[S1], w=[EG.rb[bi]])
                s_hi = (3 - c0) if c0 < 4 else (39 - c0)
                s_lo = s_hi - ncb
                osl = slice(s_hi, (s_lo if s_lo >= 0 else None), -1)
                P.op("act", lambda e: e.activation(out=EGS.t[0:64, c0:c0 + ncb], in_=v3(S1.t[0:64, :nb])[:, :, 63],
                                                   func=AF.Identity), r=[S1], w=[EGS.rb[bi]])
                P.op("act", lambda e: e.activation(out=EGS.t[64:128, osl], in_=v3(S1.t[64:128, :nb])[:, :, 0],
                                                   func=AF.Identity), r=[S1], w=[EGS.rb[bi]])
                yield
                P.op("act", lambda e: e.activation(out=LW.t[:, :nb], in_=G.t[:, :nb], func=AF.Exp, scale=-1.0),
                     r=[G], w=[LW])
                P.op(ELT_ENG, lambda e: e.tensor_mul(out=KB.t[:, blk], in0=S3.t[:, :nb], in1=LW.t[:, :nb]),
                     r=[S3, LW], w=[KB.rb[bi]])
                P.op(ELT_ENG, lambda e: e.tensor_mul(out=KKt.t[:, blk], in0=S2.t[:, :nb], in1=LW.t[:, :nb]),
                     r=[S2, LW], w=[KKt.rb[bi]])
                egb = EG.t[:, c0:c0 + ncb].unsqueeze(2).to_broadcast([128, ncb, 64])
                yield
                P.op(ELT_ENG, lambda e: e.tensor_tensor(out=v3(KHT.t[:, :nb]), in0=v3(KKt.t[:, blk]), in1=egb, op=ALU.mult),
                     r=[KKt.rb[bi], EG.rb[bi]], w=[KHT])
                P.op(ELT_ENG, lambda e: e.tensor_tensor(out=v3(BHT.t[:, :nb]), in0=v3(KB.t[:, blk]), in1=egb, op=ALU.mult),
                     r=[KB.rb[bi], EG.rb[bi]], w=[BHT])
                if STOP == 2:
                    continue
                yield
                for g0 in range(0, ncb, 8):
                    g1 = min(ncb, g0 + 8)
                    p = K.ps()
                    for j in range(g0, g1):
                        c = c0 + j
                        for kc in range(8):
                            P.op("pe", lambda e: e.matmul(p.t[0:64, (j - g0) * 64:(j - g0 + 1) * 64],
                                                          lhsT=HT.t[:, kc, c * 64:(c + 1) * 64], rhs=wr.t[:, kc, 256:320],
                                                          start=(kc == 0), stop=(kc == 7)), r=[HT, wr], w=[p])
                    P.op("act", lambda e: e.activation(out=V64.t[:, c0 + g0:c0 + g1, :],
                                                       in_=v3(p.t[0:64, 0:(g1 - g0) * 64]), func=AF.Identity),
                         r=[p], w=[V64.rb[bi]])
                    for (src, dst) in [(KHT, KH), (BHT, BH)]:
                        for j in range(g0, g1):
                            c = c0 + j
                            P.op("pe", lambda e: e.transpose(K.PSB.t[0:64, (j - g0) * 128:(j - g0 + 1) * 128],
                                                             src.t[:, j * 64:(j + 1) * 64], K.identb.t[:]),
                                 r=[src, K.identb], w=[K.PSB])
                        P.op("dve", lambda e: e.tensor_copy(out=dst.t[:, c0 + g0:c0 + g1, :],
                                                            in_=v3(K.PSB.t[0:64, 0:(g1 - g0) * 128], 128)),
                             r=[K.PSB], w=[dst.rb[bi]])

                yield

        P.op("dve", lambda e: e.memset(Hf.t[:], 0.0), w=[Hf])
        P.op("dve", lambda e: e.memset(Hb.t[:], 0.0), w=[Hb])
        dP = [slice(0, 64), slice(64, 128)]
        cdir = [list(range(NCH)), order1]
        batches = [list(range(i, min(NCH, i + NBT))) for i in range(0, NCH, NBT)]
        idbb = K.identb.t[0:64, 0:64].unsqueeze(1).to_broadcast([64, NBT, 64])

        def gen_prep(b):
            par = b % NSET
            SS, SSr = SSs[b % 2], SSrs[b % 2]
            bt = batches[b]
            nb_ = len(bt)
            cur = [0, 0]

            def cp(o, i_, rd, wr):
                P.op("act", lambda e: e.activation(out=o, in_=i_, func=AF.Identity), r=rd, w=wr)

            for d in range(2):
                q = dP[d]
                atm, btm = ATm[par][d], BTm[par][d]
                t_, r_ = SS[cur[d]].t, SSr[cur[d]][d]
                p1, p2, p3 = K.ps(), K.ps(), K.ps()
                for j, i in enumerate(bt):
                    c = cdir[d][i]
                    ck = slice(c * 64, c * 64 + 64)
                    P.op("pe", lambda e: e.matmul(p1.t[0:64, j * 128:(j + 1) * 128], lhsT=KB.t[q, ck],
                                                  rhs=QQ.t[q, c, :], start=True, stop=True), r=[KB.rb[bof(c)], QQ.rb[bof(c)]], w=[p1])
                    P.op("pe", lambda e: e.matmul(p2.t[0:64, j * 128:(j + 1) * 128], lhsT=KKt.t[q, ck],
                                                  rhs=QQ.t[q, c, :], start=True, stop=True), r=[KKt.rb[bof(c)], QQ.rb[bof(c)]], w=[p2])
                    P.op("pe", lambda e: e.matmul(p3.t[q, j * 64:(j + 1) * 64], lhsT=QQ.t[q, c, 0:64],
                                                  rhs=KB.t[q, ck], start=True, stop=True), r=[KB.rb[bof(c)], QQ.rb[bof(c)]], w=[p3])
                mcf = K.maskc.t[:, d, :].unsqueeze(1).to_broadcast([64, nb_, 128])
                maf = K.maska2.t[q, :].unsqueeze(1).to_broadcast([64, nb_, 64])
                idq = K.identb.t[q, q].unsqueeze(1).to_broadcast([64, nb_, 64])
                P.op("dve", lambda e: e.tensor_tensor(out=atm.t[:, :nb_, :], in0=v3(p1.t[0:64, 0:nb_ * 128], 128),
                                                      in1=mcf, op=ALU.mult), r=[p1, K.maskc], w=[atm])
                P.op("dve", lambda e: e.tensor_tensor(out=t_[q, :nb_, 128:192], in0=v3(p3.t[q, 0:nb_ * 64]),
                                                      in1=maf, op=ALU.mult), r=[p3, K.maska2], w=[r_["A"]])
                cp(t_[q, :nb_, 64:128], atm.t[:, :nb_, 0:64], [atm], [r_["M"]])
                P.op("dve", lambda e: e.tensor_tensor(out=btm.t[:, :nb_, :], in0=v3(p2.t[0:64, 0:nb_ * 128], 128),
                                                      in1=mcf, op=ALU.mult), r=[p2, K.maskc], w=[btm])
                P.op("dve", lambda e: e.tensor_tensor(out=t_[q, :nb_, 0:64], in0=idq, in1=t_[q, :nb_, 64:128],
                                                      op=ALU.subtract), r=[K.identb, r_["M"]], w=[r_["X"]])
                yield
            for lv in range(1, 6):
                last = lv == 5
                for d in range(2):
                    q = dP[d]
                    t_, r_ = SS[cur[d]].t, SSr[cur[d]][d]
                    nxt = 1 - cur[d]
                    tn, rn = SS[nxt].t, SSr[nxt][d]
                    pj = K.ps()
                    for j in range(nb_):
                        P.op("pe", lambda e: e.matmul(pj.t[q, j * 128 + 64:j * 128 + 128], lhsT=t_[q, j, 64:128],
                                                      rhs=t_[q, j, 128:192], start=True, stop=True), r=[r_["M"], r_["A"]], w=[pj])
                        if not last:
                            P.op("pe", lambda e: e.matmul(pj.t[q, j * 128:j * 128 + 64], lhsT=t_[q, j, 128:192],
                                                          rhs=t_[q, j, 64:128], start=True, stop=True), r=[r_["M"], r_["A"]], w=[pj])
                    pv = v3(pj.t[q, 0:nb_ * 128], 128)
                    if not last:
                        cp(tn[q, :nb_, 64:192], pv, [pj], [rn["M"], rn["A"]])
                    else:
                        cp(tn[q, :nb_, 128:192], pv[:, :, 64:128], [pj], [rn["A"]])
                    yield
                for d in range(2):
                    q = dP[d]
                    t_, r_ = SS[cur[d]].t, SSr[cur[d]][d]
                    nxt = 1 - cur[d]
                    tn, rn = SS[nxt].t, SSr[nxt][d]
                    px = K.ps()
                    for j in range(nb_):
                        P.op("pe", lambda e: e.matmul(px.t[q, j * 64:(j + 1) * 64], lhsT=tn[q, j, 128:192],
                                                      rhs=t_[q, j, 0:64], start=True, stop=True), r=[rn["A"], r_["X"]], w=[px])
                    xo = X5[par][d].t[:, :nb_, :] if last else tn[q, :nb_, 0:64]
                    xr = X5[par][d] if last else rn["X"]
                    P.op("dve", lambda e: e.tensor_tensor(out=xo, in0=v3(px.t[q, 0:nb_ * 64]), in1=t_[q, :nb_, 0:64],
                                                          op=ALU.add), r=[px, r_["X"]], w=[xr])
                    cur[d] = nxt
                    yield

        def gen_chain(b):
            par = b % NSET
            for j, i in enumerate(batches[b]):
                cd = (cdir[0][i], cdir[1][i])
                ck = [slice(cd[d] * 64, cd[d] * 64 + 64) for d in range(2)]
                atm = [ATm[par][d] for d in range(2)]
                btm = [BTm[par][d] for d in range(2)]
                x5 = [X5[par][d] for d in range(2)]
                pt, py, phh = K.ps(), K.ps(), K.ps()

                def mm(o, l_, r_, st_, sp_, rd, wr):
                    P.op("pe", lambda e: e.matmul(o, lhsT=l_, rhs=r_, start=st_, stop=sp_), r=rd, w=[wr])

                for d in range(2):
                    mm(pt.t[0:64, d * 64:(d + 1) * 64], btm[d].t[:, j, 0:64], V64.t[:, cd[d], :], True, False, [btm[d], V64.rb[bof(cd[d])]], pt)
                    mm(pt.t[0:64, d * 64:(d + 1) * 64], QQ.t[dP[d], cd[d], 0:64], Hb.t[dP[d], :], False, True, [QQ.rb[bof(cd[d])], Hb], pt)
                P.op("act", lambda e: e.activation(out=T1.t[:], in_=v3(pt.t[0:64, 0:128]), func=AF.Identity),
                     r=[pt], w=[T1])
                yield
                pu = K.ps()
                for d in range(2):
                    mm(pu.t[0:64, d * 64:(d + 1) * 64], x5[d].t[:, j, :], T1.t[:, d, :], True, True, [x5[d], T1], pu)
                P.op("dve", lambda e: e.tensor_scalar_mul(out=NU.t[:], in0=v3(pu.t[0:64, 0:128]), scalar1=-1.0),
                     r=[pu], w=[NU])
                yield
                for d in range(2):
                    mm(phh.t[dP[d], 0:64], KH.t[:, cd[d], dP[d]], V64.t[:, cd[d], :], True, False, [KH.rb[bof(cd[d])], V64.rb[bof(cd[d])]], phh)
                    mm(phh.t[dP[d], 0:64], BH.t[:, cd[d], dP[d]], NU.t[:, d, :], False, True, [BH.rb[bof(cd[d])], NU], phh)
                for d in range(2):
                    o = py.t[0:64, d * 64:(d + 1) * 64]
                    mm(o, V64.t[:, cd[d], :], btm[d].t[:, j, 64:128], True, False, [V64.rb[bof(cd[d])], btm[d]], py)
                    mm(o, NU.t[:, d, :], atm[d].t[:, j, 64:128], False, False, [NU, atm[d]], py)
                    mm(o, Hb.t[dP[d], :], QQ.t[dP[d], cd[d], 64:128], False, True, [Hb, QQ.rb[bof(cd[d])]], py)
                egr = [EGS.rb[bof(cd[0])], EGS.rb[bof(cd[1])]]
                P.op("dve", lambda e: e.scalar_tensor_tensor(out=Hb.t[:], in0=Hf.t[:], scalar=EGS.t[:, i:i + 1],
                                                             in1=phh.t[:, 0:64], op0=ALU.mult, op1=ALU.add),
                     r=[Hf, phh] + egr, w=[Hb])
                P.op("dve", lambda e: e.scalar_tensor_tensor(out=Hf.t[:], in0=Hf.t[:], scalar=EGS.t[:, i:i + 1],
                                                             in1=phh.t[:, 0:64], op0=ALU.mult, op1=ALU.add),
                     r=[Hf, phh] + egr, w=[Hf])
                for d in range(2):
                    P.op("act", lambda e: e.activation(out=YTd[d].t[:, ck[d]], in_=py.t[0:64, d * 64:(d + 1) * 64],
                                                       func=AF.Identity), r=[py], w=[YTd[d]])
                yield

        def drive(gens):
            gens = list(gens)
            while gens:
                for g in list(gens):
                    try:
                        next(g)
                    except StopIteration:
                        gens.remove(g)

        nbt_ = len(batches)
        preps = {}
        prep_done = set()
        st_ = {"next_prep": 0, "next_chain": 0, "fin_chain": 0, "chain": None}
        e_pend = [0, 1, 4, 2, 3]
        e_act = []
        e_free = [0, 1]
        e_done = set()
        req_ = lambda k: {0} if k == 0 else ({0, 1, 4} if k < 3 else {0, 1, 2, 3, 4})
        while st_["fin_chain"] < nbt_:
            while e_pend and e_free:
                pr_ = e_free.pop(0)
                bi_ = e_pend.pop(0)
                e_act.append((gen_elem(bi_, pr_), pr_, bi_))
            while (st_["next_prep"] < nbt_ and len(preps) < 2 and st_["next_prep"] < st_["fin_chain"] + NSET
                   and req_(st_["next_prep"]) <= e_done):
                k_ = st_["next_prep"]
                preps[k_] = gen_prep(k_)
                st_["next_prep"] += 1
            if st_["chain"] is None and st_["next_chain"] in prep_done:
                st_["chain"] = gen_chain(st_["next_chain"])
                st_["next_chain"] += 1
            if st_["chain"] is not None:
                try:
                    next(st_["chain"])
                except StopIteration:
                    st_["chain"] = None
                    st_["fin_chain"] += 1
            for k_ in list(preps):
                try:
                    next(preps[k_])
                except StopIteration:
                    del preps[k_]
                    prep_done.add(k_)
            for it_ in list(e_act):
                try:
                    next(it_[0])
                except StopIteration:
                    e_act.remove(it_)
                    e_free.append(it_[1])
                    e_done.add(it_[2])
        if STOP <= 5:
            continue
        hp = slice((h % 2) * 64, (h % 2) * 64 + 64)
        o64 = K.bones.t[0:64, 0:64]
        def gen_out(bi, par_):
            sc_ = SCR[par_]
            S1, S2, S3 = sc_["s1"], sc_["s2"], sc_["s3"]
            for (n0, nb) in [BLKS[bi]]:
                blk = slice(n0, n0 + nb)
                yield
                p = K.ps()
                P.op("dve", lambda e: e.tensor_add(out=S3.t[0:64, :nb], in0=YTd[0].t[:, blk], in1=YTd[1].t[:, blk]),
                     r=[YTd[0], YTd[1]], w=[S3])
                P.op("pe", lambda e: e.matmul(p.t[0:64, :nb], lhsT=o64, rhs=S3.t[0:64, :nb], start=True, stop=True),
                     r=[K.bones, S3], w=[p])
                P.op("dve", lambda e: e.scalar_tensor_tensor(out=S1.t[0:64, :nb], in0=p.t[0:64, :nb], scalar=-1.0 / 64,
                                                             in1=S3.t[0:64, :nb], op0=ALU.mult, op1=ALU.add),
                     r=[p, S3], w=[S1])
                P.op("act", lambda e: e.activation(out=S2.t[0:64, :nb], in_=S1.t[0:64, :nb], func=AF.Square),
                     r=[S1], w=[S2])
                yield
                p = K.ps()
                P.op("pe", lambda e: e.matmul(p.t[0:64, :nb], lhsT=o64, rhs=S2.t[0:64, :nb], start=True, stop=True),
                     r=[K.bones, S2], w=[p])
                P.op("act", lambda e: e.activation(out=S2.t[0:64, :nb], in_=p.t[0:64, :nb], func=AF.Sqrt,
                                                   bias=GN_EPS, scale=1.0 / 64), r=[p], w=[S2])
                P.op("dve", lambda e: e.reciprocal(out=S2.t[0:64, :nb], in_=S2.t[0:64, :nb]), r=[S2], w=[S2])
                P.op("dve", lambda e: e.tensor_mul(out=S1.t[0:64, :nb], in0=S1.t[0:64, :nb], in1=S2.t[0:64, :nb]),
                     r=[S1, S2], w=[S1])
                P.op("act", lambda e: e.activation(out=S1.t[0:64, :nb], in_=S1.t[0:64, :nb], func=AF.Identity,
                                                   bias=prm(6, 0, 64), scale=prm(5, 0, 64)), r=[S1, RWP], w=[S1])
                P.op("dve", lambda e: e.tensor_mul(out=S3.t[0:64, :nb], in0=BS.t[:, blk], in1=VT.t[:, blk]),
                     r=[BS, VT], w=[S3])
                P.op("dve", lambda e: e.tensor_add(out=S1.t[0:64, :nb], in0=S1.t[0:64, :nb], in1=S3.t[0:64, :nb]),
                     r=[S1, S3], w=[S1])
                yield
                p = K.ps()
                P.op("pe", lambda e: e.matmul(p.t[0:64, :nb], lhsT=GLB.t[:, h * 64:(h + 1) * 64], rhs=SLG.t[:, blk],
                                              start=True, stop=True), r=[GLB, SLG], w=[p])
                P.op("dve", lambda e: e.tensor_mul(out=YA.t[hp, h // 2, blk], in0=S1.t[0:64, :nb], in1=p.t[0:64, :nb]),
                     r=[S1, p], w=[YA])
                yield

        pend_ = list(range(len(BLKS)))
        act2 = []
        free2 = [0, 1]
        while pend_ or act2:
            while pend_ and free2:
                pr_ = free2.pop(0)
                act2.append((gen_out(pend_.pop(0), pr_), pr_))
            for it_ in list(act2):
                try:
                    next(it_[0])
                except StopIteration:
                    act2.remove(it_)
                    free2.append(it_[1])


def phase_na(K, ph, YB):
    P, D, l, HT, sb = K.P, K.D, K.l, K.HT, K.sb

    def v3(ap, j=64):
        return ap.rearrange("p (c j) -> p c j", j=j)

    WVB = sb(ph, "wvb", [128, 8, 512], BF16)
    P.dma("pool", WVB.t[:], D["w_vb"][l], w=[WVB])
    NAP = sb(ph, "nap", [64, 2], F32)
    P.dma("sp", NAP.t[:], D["nap"][l], w=[NAP])
    P.op("dve", lambda e: e.tensor_scalar_mul(out=NAP.t[:, 0:1], in0=NAP.t[:, 0:1], scalar1=0.125), r=[NAP], w=[NAP])
    VAe = sb(ph, "vae", [128, 18, 8, 65], BF16)
    VAo = sb(ph, "vao", [128, 17, 8, 65], BF16)
    P.op("dve", lambda e: e.memset(VAe.t[:, :, :, 64:65], 1.0), w=[VAe])
    P.op("dve", lambda e: e.memset(VAo.t[:, :, :, 64:65], 1.0), w=[VAo])
    cnt = 0
    for (va, off, n_) in [(VAe, 0, 18), (VAo, 64, 17)]:
        for c in range(n_):
            p = K.ps()
            t0 = off + c * 128
            for kc in range(8):
                P.op("pe", lambda e: e.matmul(p.t[:, :], lhsT=HT.t[:, kc, t0:t0 + 128], rhs=WVB.t[:, kc, :],
                                              start=(kc == 0), stop=(kc == 7)), r=[HT, WVB], w=[p])
            if cnt % 2 == 0:
                P.op("act", lambda e: e.activation(out=va.t[:, c, :, 0:64], in_=v3(p.t[:, :]), func=AF.Identity),
                     r=[p], w=[va])
            else:
                P.op("dve", lambda e: e.tensor_copy(out=va.t[:, c, :, 0:64], in_=v3(p.t[:, :])), r=[p], w=[va])
            cnt += 1

    def vtile(ck):
        return (VAe, ck // 2) if ck % 2 == 0 else (VAo, (ck - 1) // 2)

    WQK = [sb(ph, f"wqk{i}", [128, 8, 128], BF16) for i in range(2)]
    BIAS = [[sb(ph, f"bias{i}{par}", [128, 7, 64], F32) for par in range(2)] for i in range(2)]
    QT = sb(ph, "qt", [64, NT], BF16)
    KT = sb(ph, "kt", [64, NT], BF16)
    SQ = [sb(ph, f"na_sq{i}", [64, 512], BF16) for i in range(2)]
    RS = [sb(ph, f"na_rs{i}", [64, 512], F32) for i in range(2)]
    TM = [sb(ph, f"na_tm{i}", [64, 512], F32) for i in range(2)]
    SB_ = [sb(ph, f"na_sb{i}", [128, 4, 64], F32) for i in range(2)]
    PT = [sb(ph, f"na_pt{i}", [128, 6, 64], BF16) for i in range(2)]
    RD = sb(ph, "na_rd", [65, 512], F32)
    RB = sb(ph, "na_rb", [64, 512], F32)
    o64 = K.bones.t[0:64, 0:64]
    for h in range(8):
        wqk, bias = WQK[h % 2], BIAS[h % 2]
        hp = slice((h % 2) * 64, (h % 2) * 64 + 64)
        P.dma("pool", wqk.t[:], D["w_qk"][l, h], w=[wqk])
        for par in range(2):
            for e_ in range(2):
                s0 = par + e_
                P.dma("sp", bias[par].t[e_ * 64:(e_ + 1) * 64, :, :], D["rpbT"][l, h][:, s0:s0 + 13:2, :], w=[bias[par]])
            P.op("dve", lambda e: e.tensor_tensor(out=bias[par].t[:], in0=bias[par].t[:],
                                                  in1=K.namask.t[:].unsqueeze(1).to_broadcast([128, 7, 64]), op=ALU.add),
                 r=[bias[par], K.namask], w=[bias[par]])
        for (n0, nb) in BLKS:
            blk = slice(n0, n0 + nb)
            dsts = [QT, KT]
            pp = [K.ps(), K.ps()]
            for qi in range(2):
                for kc in range(8):
                    P.op("pe", lambda e: e.matmul(pp[qi].t[0:64, :nb], lhsT=wqk.t[:, kc, qi * 64:(qi + 1) * 64],
                                                  rhs=HT.t[:, kc, blk], start=(kc == 0), stop=(kc == 7)),
                         r=[wqk, HT], w=[pp[qi]])
            for qi in range(2):
                P.op("act", lambda e: e.activation(out=SQ[qi].t[:, :nb], in_=pp[qi].t[0:64, :nb], func=AF.Square),
                     r=[pp[qi]], w=[SQ[qi]])
            p2 = [K.ps(), K.ps()]
            for qi in range(2):
                P.op("pe", lambda e: e.matmul(p2[qi].t[0:64, :nb], lhsT=K.onesb.t[0:64, 0:64], rhs=SQ[qi].t[:, :nb],
                                              start=True, stop=True), r=[K.onesb, SQ[qi]], w=[p2[qi]])
            for qi in range(2):
                P.op("act", lambda e: e.activation(out=RS[qi].t[:, :nb], in_=p2[qi].t[0:64, :nb], func=AF.Sqrt,
                                                   bias=RMS_EPS, scale=1.0 / 64), r=[p2[qi]], w=[RS[qi]])
            for qi in range(2):
                P.op("dve", lambda e: e.reciprocal(out=RS[qi].t[:, :nb], in_=RS[qi].t[:, :nb]), r=[RS[qi]], w=[RS[qi]])
            for qi in range(2):
                P.op("dve", lambda e: e.tensor_mul(out=TM[qi].t[:, :nb], in0=pp[qi].t[0:64, :nb], in1=RS[qi].t[:, :nb]),
                     r=[pp[qi], RS[qi]], w=[TM[qi]])
            for qi in range(2):
                P.op("act", lambda e: e.activation(out=dsts[qi].t[:, blk], in_=TM[qi].t[:, :nb], func=AF.Identity,
                                                   scale=NAP.t[:, qi:qi + 1]), r=[TM[qi], NAP], w=[dsts[qi]])
        groups = [([0, 1, 2, 3], True)] + [(list(range(4 + g * 8, 12 + g * 8)), False) for g in range(4)]
        items = []
        for gi, (qcs, isctx) in enumerate(groups):
            for qi_, cq in enumerate(qcs):
                items.append((gi, qi_, cq, isctx, len(qcs), qcs[0]))
        POs = [K.PSL, K.PSL2]
        state = {}

        def emit_S(n):
            gi, qi_, cq, isctx, ng, q0 = items[n]
            qs = slice(cq * 64, cq * 64 + 64)
            pt = PT[n % 2]
            sbt = SB_[n % 2]
            kcs = []
            if not isctx:
                i = cq - 4
                r0 = min(max(i - 4, 0), 24)
                d0 = r0 - i + 7
                par, m0 = d0 % 2, d0 // 2
                pS = K.ps()
                for m in range(4):
                    ck = 4 + r0 + 2 * m
                    kcs.append(ck)
                    P.op("pe", lambda e: e.matmul(pS.t[:, m * 64:(m + 1) * 64], lhsT=KT.t[:, ck * 64:ck * 64 + 128],
                                                  rhs=QT.t[:, qs], start=True, stop=True), r=[KT, QT], w=[pS])
                P.op("dve", lambda e: e.tensor_tensor(out=sbt.t[:], in0=v3(pS.t[:, 0:256]), in1=bias[par].t[:, m0:m0 + 4, :],
                                                      op=ALU.add), r=[pS, bias[par]], w=[sbt])
                P.op("act", lambda e: e.activation(out=pt.t[:, 0:4, :], in_=sbt.t[:], func=AF.Exp), r=[sbt], w=[pt])
            pC = K.ps()
            for a in range(2):
                P.op("pe", lambda e: e.matmul(pC.t[:, a * 64:(a + 1) * 64], lhsT=KT.t[:, a * 128:(a + 1) * 128],
                                              rhs=QT.t[:, qs], start=True, stop=True), r=[KT, QT], w=[pC])
            P.op("act", lambda e: e.activation(out=pt.t[:, 4:6, :], in_=v3(pC.t[:, 0:128]), func=AF.Exp),
                 r=[pC], w=[pt])
            state[n] = kcs

        def emit_PV(n):
            gi, qi_, cq, isctx, ng, q0 = items[n]
            pt = PT[n % 2]
            po = POs[gi % 2]
            kcs = state.pop(n)
            keys = [(m, kcs[m]) for m in range(len(kcs))] + [(4, 0), (5, 2)]
            for n_, (slot, ck) in enumerate(keys):
                va, vi = vtile(ck)
                P.op("pe", lambda e: e.matmul(po.t[0:65, qi_ * 64:(qi_ + 1) * 64], lhsT=va.t[:, vi, h, :],
                                              rhs=pt.t[:, slot, :], start=(n_ == 0), stop=(n_ == len(keys) - 1)),
                     r=[va, pt], w=[po])
            if qi_ == ng - 1:
                W = ng * 64
                n0 = q0 * 64
                P.op("dve", lambda e: e.reciprocal(out=RD.t[64:65, :W], in_=po.t[64:65, :W]), r=[po], w=[RD])
                pb = K.ps()
                P.op("pe", lambda e: e.matmul(pb.t[0:64, :W], lhsT=K.onesf.t[64:65, 0:64], rhs=RD.t[64:65, :W],
                                              start=True, stop=True), r=[K.onesf, RD], w=[pb])
                P.op("act", lambda e: e.activation(out=RB.t[:, :W], in_=pb.t[0:64, :W], func=AF.Identity), r=[pb], w=[RB])
                P.op("dve", lambda e: e.tensor_mul(out=YB.t[hp, h // 2, n0:n0 + W], in0=po.t[0:64, :W], in1=RB.t[:, :W]),
                     r=[po, RB], w=[YB])

        emit_S(0)
        for n in range(len(items)):
            if n + 1 < len(items):
                emit_S(n + 1)
            emit_PV(n)


def phase_merge(K, ph, YA, YB, xres):
    P, D, l, HT, sb = K.P, K.D, K.l, K.HT, K.sb
    WG = sb(ph, "wg", [128, 8, 2048], BF16)
    PA = sb(ph, "pa", [128, 4, 1024], BF16)
    PB = sb(ph, "pb", [128, 4, 1024], BF16)
    WO = sb(ph, "wo", [128, 8, 1024], BF16)
    for i in range(4):
        P.dma("pool", WG.t[:, :, i * 512:(i + 1) * 512], D["w_g"][l][:, :, i * 512:(i + 1) * 512], w=[WG])
    P.dma("pool", PA.t[:], D["pa"][l], w=[PA])
    P.dma("pool", PB.t[:], D["pb"][l], w=[PB])
    P.dma("pool", WO.t[:], D["wo"][l], w=[WO])
    MG = sb(ph, "mg", [128, 8, 512], BF16)
    XB = [sb(ph, f"xb{i}", [128, 8, 512], F32) for i in range(1)]
    SA = sb(ph, "m_sa", [128, 512], F32)
    SBt = sb(ph, "m_sb", [128, 512], F32)
    M1 = sb(ph, "m_m1", [128, 512], F32)
    M2 = sb(ph, "m_m2", [128, 512], F32)
    for bi, (n0, nb) in enumerate(BLKS):
        blk = slice(n0, n0 + nb)
        col = 1 if n0 == 0 else 0
        xb = XB[0]
        P.dma("sp", xb.t[:, :, :nb], xres[:, :, blk], w=[xb])
        for oc in range(8):
            ocs = slice(oc * 128, (oc + 1) * 128)
            pga, pgb, ppa, ppb = K.ps(), K.ps(), K.ps(), K.ps()
            for kc in range(8):
                P.op("pe", lambda e: e.matmul(pga.t[:, :nb], lhsT=WG.t[:, kc, oc * 128:(oc + 1) * 128],
                                              rhs=HT.t[:, kc, blk], start=(kc == 0), stop=(kc == 7)), r=[WG, HT], w=[pga])
            for kc in range(8):
                P.op("pe", lambda e: e.matmul(pgb.t[:, :nb], lhsT=WG.t[:, kc, 1024 + oc * 128:1024 + (oc + 1) * 128],
                                              rhs=HT.t[:, kc, blk], start=(kc == 0), stop=(kc == 7)), r=[WG, HT], w=[pgb])
            for kc in range(4):
                P.op("pe", lambda e: e.matmul(ppa.t[:, :nb], lhsT=PA.t[:, kc, ocs], rhs=YA.t[:, kc, blk],
                                              start=(kc == 0), stop=(kc == 3)), r=[PA, YA], w=[ppa])
            for kc in range(4):
                P.op("pe", lambda e: e.matmul(ppb.t[:, :nb], lhsT=PB.t[:, kc, ocs], rhs=YB.t[:, kc, blk],
                                              start=(kc == 0), stop=(kc == 3)), r=[PB, YB], w=[ppb])
            P.op("act", lambda e: e.activation(out=SA.t[:, :nb], in_=pga.t[:, :nb], func=AF.Sigmoid), r=[pga], w=[SA])
            P.op("act", lambda e: e.activation(out=SBt.t[:, :nb], in_=pgb.t[:, :nb], func=AF.Sigmoid), r=[pgb], w=[SBt])
            P.op("dve", lambda e: e.tensor_mul(out=M1.t[:, :nb], in0=SA.t[:, :nb], in1=ppa.t[:, :nb]), r=[SA, ppa], w=[M1])
            P.op("dve", lambda e: e.tensor_mul(out=M2.t[:, :nb], in0=SBt.t[:, :nb], in1=ppb.t[:, :nb]), r=[SBt, ppb], w=[M2])
            P.op("dve", lambda e: e.tensor_add(out=MG.t[:, oc, :nb], in0=M1.t[:, :nb], in1=M2.t[:, :nb]),
                 r=[M1, M2], w=[MG])
        for oc in range(8):
            po = K.ps()
            for kc in range(8):
                P.op("pe", lambda e: e.matmul(po.t[:, :nb], lhsT=WO.t[:, kc, oc * 128:(oc + 1) * 128],
                                              rhs=MG.t[:, kc, :nb], start=(kc == 0), stop=(kc == 7)), r=[WO, MG], w=[po])
            P.op("dve", lambda e: e.scalar_tensor_tensor(out=xb.t[:, oc, :nb], in0=po.t[:, :nb],
                                                         scalar=K.mt.t[:, 16 + oc, col:col + 1], in1=xb.t[:, oc, :nb],
                                                         op0=ALU.mult, op1=ALU.add), r=[po, K.mt, xb], w=[xb])
        P.dma("sp", xres[:, :, blk], xb.t[:, :, :nb], r=[xb], w=[Res()])


def phase_moe(K, ph, XT):
    P, D, l, HT, sb = K.P, K.D, K.l, K.HT, K.sb
    GT = sb(ph, "gt", [16, NT], BF16)
    K.sel16 = sb(ph, "sel16", [16, 16, 128], BF16)
    P.dma("pool", K.sel16.t[:], D["c_sel16"], w=[K.sel16])
    RT = [sb(ph, f"r_{n}", [128, 16], F32) for n in ["s", "sel", "msk", "num"]]
    R4 = [sb(ph, f"r4_{i}", [128, 4], F32) for i in range(10)]
    R1 = [sb(ph, f"r1_{i}", [128, 1], F32) for i in range(3)]
    GTM = sb(ph, "gtm", [128, 4, 16], F32)

    def router(n0, nb, HF):
        S, SEL, MSK, NUM = RT
        ntile = nb // 128
        for ti in range(ntile):
            pr = K.ps()
            for c in range(8):
                P.op("pe", lambda e: e.matmul(pr.t[:, 0:16], lhsT=HF.t[:, c, ti * 128:(ti + 1) * 128], rhs=K.rwt.t[:, c, :],
                                              start=(c == 0), stop=(c == 7)), r=[HF, K.rwt], w=[pr])
            P.op("act", lambda e: e.activation(out=S.t[:], in_=pr.t[:, 0:16], func=AF.Sigmoid), r=[pr], w=[S])
            P.op("dve", lambda e: e.tensor_add(out=SEL.t[:], in0=S.t[:], in1=K.rbias.t[:]), r=[S, K.rbias], w=[SEL])
            sv = SEL.t[:].rearrange("p (g j) -> p g j", j=4)
            a, b, c_, d_ = [sv[:, :, j] for j in range(4)]
            pq, qq, rr, ss, m1, t1, t2, m2, gs, gm = R4

            def tt(o, x, y, op, rd, wr):
                P.op("dve", lambda e: e.tensor_tensor(out=o, in0=x, in1=y, op=op), r=rd, w=wr)

            tt(pq.t[:], a, b, ALU.max, [SEL], [pq])
            tt(qq.t[:], a, b, ALU.min, [SEL], [qq])
            tt(rr.t[:], c_, d_, ALU.max, [SEL], [rr])
            tt(ss.t[:], c_, d_, ALU.min, [SEL], [ss])
            tt(m1.t[:], pq.t[:], rr.t[:], ALU.max, [pq, rr], [m1])
            tt(t1.t[:], pq.t[:], rr.t[:], ALU.min, [pq, rr], [t1])
            tt(t2.t[:], qq.t[:], ss.t[:], ALU.max, [qq, ss], [t2])
            tt(m2.t[:], t1.t[:], t2.t[:], ALU.max, [t1, t2], [m2])
            tt(gs.t[:], m1.t[:], m2.t[:], ALU.add, [m1, m2], [gs])
            gmax, den, rden = R1
            P.op("dve", lambda e: e.tensor_reduce(out=gmax.t[:], in_=gs.t[:], axis=AX.X, op=ALU.max), r=[gs], w=[gmax])
            P.op("dve", lambda e: e.tensor_scalar(out=gm.t[:], in0=gs.t[:], scalar1=gmax.t[:, 0:1], scalar2=None,
                                                  op0=ALU.is_ge), r=[gs, gmax], w=[gm])
            mv = MSK.t[:].rearrange("p (g j) -> p g j", j=4)
            P.op("dve", lambda e: e.tensor_tensor(out=mv, in0=sv, in1=m2.t[:].unsqueeze(2).to_broadcast([128, 4, 4]),
                                                  op=ALU.is_ge), r=[SEL, m2], w=[MSK])
            P.op("dve", lambda e: e.tensor_tensor(out=mv, in0=mv, in1=gm.t[:].unsqueeze(2).to_broadcast([128, 4, 4]),
                                                  op=ALU.mult), r=[MSK, gm], w=[MSK])
            tt(NUM.t[:], MSK.t[:], S.t[:], ALU.mult, [MSK, S], [NUM])
            P.op("dve", lambda e: e.tensor_reduce(out=den.t[:], in_=NUM.t[:], axis=AX.X, op=ALU.add), r=[NUM], w=[den])
            P.op("dve", lambda e: e.reciprocal(out=rden.t[:], in_=den.t[:]), r=[den], w=[rden])
            P.op("dve", lambda e: e.tensor_scalar_mul(out=GTM.t[:, ti, :], in0=NUM.t[:], scalar1=rden.t[:, 0:1]),
                 r=[NUM, rden], w=[GTM])
        pT = K.ps()
        for ti in range(ntile):
            P.op("pe", lambda e: e.transpose(pT.t[0:16, ti * 128:(ti + 1) * 128], GTM.t[:, ti, :], K.identf.t[:]),
                 r=[GTM, K.identf], w=[pT])
        P.op("act", lambda e: e.activation(out=GT.t[:, n0:n0 + nb], in_=pT.t[0:16, :nb], func=AF.Identity), r=[pT], w=[GT])

    with ExitStack() as nph:
        phase_norm(K, nph, XT, K.mul2, 24, router)
        P.barrier()
    W1 = [sb(ph, f"w1_{i}", [128, 8, 512], BF16) for i in range(2)]
    W3 = [sb(ph, f"w3_{i}", [128, 8, 512], BF16) for i in range(2)]
    W2 = [sb(ph, f"w2_{i}", [128, 4, 1024], BF16) for i in range(2)]
    GB = sb(ph, "gb", [128, 512], F32)
    SL = [sb(ph, f"sl{i}", [128, 512], F32) for i in range(2)]
    T2 = [sb(ph, f"t2{i}", [128, 512], F32) for i in range(2)]
    HG = sb(ph, "hg", [128, 4, 512], BF16)

    def load(e):
        P.dma("pool", W1[e % 2].t[:], D["w1"][l, e], w=[W1[e % 2]])
        P.dma("pool", W3[e % 2].t[:], D["w3"][l, e], w=[W3[e % 2]])
        P.dma("pool", W2[e % 2].t[:], D["w2"][l, e], w=[W2[e % 2]])

    import os as _os
    NOLOAD = _os.environ.get("MOE_NOLOAD") == "1"
    load(0)
    for ex in range(16):
        if ex + 1 < 16 and not (NOLOAD and ex >= 1):
            load(ex + 1)
        w1, w3, w2 = W1[ex % 2], W3[ex % 2], W2[ex % 2]
        for (n0, nb) in BLKS:
            blk = slice(n0, n0 + nb)
            col = 1 if n0 == 0 else 0
            pg = K.ps()
            P.op("pe", lambda e: e.matmul(pg.t[:, :nb], lhsT=K.sel16.t[:, ex, :], rhs=GT.t[:, blk], start=True, stop=True),
                 r=[K.sel16, GT], w=[pg])
            P.op("act", lambda e: e.activation(out=GB.t[:, :nb], in_=pg.t[:, :nb], func=AF.Identity), r=[pg], w=[GB])
            for hc in range(4):
                hs = slice(hc * 128, (hc + 1) * 128)
                p1, p3 = K.ps(), K.ps()
                for kc in range(8):
                    P.op("pe", lambda e: e.matmul(p1.t[:, :nb], lhsT=w1.t[:, kc, hs], rhs=HT.t[:, kc, blk],
                                                  start=(kc == 0), stop=(kc == 7)), r=[w1, HT], w=[p1])
                for kc in range(8):
                    P.op("pe", lambda e: e.matmul(p3.t[:, :nb], lhsT=w3.t[:, kc, hs], rhs=HT.t[:, kc, blk],
                                                  start=(kc == 0), stop=(kc == 7)), r=[w3, HT], w=[p3])
                sl, t2 = SL[hc % 2], T2[hc % 2]
                P.op("act", lambda e: e.activation(out=sl.t[:, :nb], in_=p1.t[:, :nb], func=AF.Silu), r=[p1], w=[sl])
                P.op("dve", lambda e: e.tensor_mul(out=t2.t[:, :nb], in0=sl.t[:, :nb], in1=p3.t[:, :nb]), r=[sl, p3], w=[t2])
                P.op("dve", lambda e: e.tensor_mul(out=HG.t[:, hc, :nb], in0=t2.t[:, :nb], in1=GB.t[:, :nb]),
                     r=[t2, GB], w=[HG])
            for oc in range(8):
                po = K.ps()
                for hc in range(4):
                    P.op("pe", lambda e: e.matmul(po.t[:, :nb], lhsT=w2.t[:, hc, oc * 128:(oc + 1) * 128],
                                                  rhs=HG.t[:, hc, :nb], start=(hc == 0), stop=(hc == 3)), r=[w2, HG], w=[po])
                P.op("dve", lambda e: e.scalar_tensor_tensor(out=XT.t[:, oc, blk], in0=po.t[:, :nb],
                                                             scalar=K.mt.t[:, 40 + oc, col:col + 1], in1=XT.t[:, oc, blk],
                                                             op0=ALU.mult, op1=ALU.add), r=[po, K.mt, XT], w=[XT])


def kernel(**inputs):
    maps = _prep(inputs)
    nc = build()
    res = run_bass_kernel_spmd(nc, maps, core_ids=list(range(8)))
    out = np.zeros((8, 2048, 1024), np.float32)
    for b in range(8):
        y = res.results[b]["yout"]
        out[b] = y.transpose(2, 1, 0).reshape(2048, 1024)
    return out
```

```python
import numpy as np
import concourse.bass as bass
import concourse.mybir as mybir
from concourse.bass_utils import run_bass_kernel_spmd
from contextlib import ExitStack

F32 = mybir.dt.float32
BF16 = mybir.dt.bfloat16
ALU = mybir.AluOpType
AF = mybir.ActivationFunctionType
AX = mybir.AxisListType

L = 2
NT = 2304
NCTX = 256
NCH = 36
BLKS = [(0, 256), (256, 512), (768, 512), (1280, 512), (1792, 512)]
RMS_EPS = 1e-6
GN_EPS = 64e-5
DEC_C = -0.6065306597126334
NEG = -30000.0

SEM_LIMIT = 30000
NDMA = 16
NSW = 12
import os as _os0
ELT_ENG = _os0.environ.get('ELT_ENG', 'dve')
SW_CLEAR = False


class Res:
    __slots__ = ("lw", "rd", "grp")

    def __init__(self):
        self.lw = None
        self.rd = {}
        self.grp = None


class PEProxy:
    def __init__(self, prog):
        self.P = prog
        self.pe = prog.nc.tensor
        self.cur_w = None

    def _chk(self, k_ap):
        grp = (k_ap.base_partition(), k_ap.partition_size())
        for x in self.cur_w:
            if x.grp is not None and x.grp != grp and not x.rd:
                self.P.fence_pe()
            x.grp = grp

    def matmul(self, out, lhsT, rhs, **kw):
        self._chk(lhsT)
        return self.pe.matmul(out, lhsT=lhsT, rhs=rhs, **kw)

    def transpose(self, out, in_, identity):
        self._chk(in_)
        return self.pe.transpose(out, in_, identity)


class Tl:
    def __init__(self, t):
        self.t = t
        self.r = Res()


class Prog:
    def __init__(self, nc, stack):
        self.nc = nc
        self.stack = stack
        self.engs = {"pe": nc.tensor, "dve": nc.vector, "act": nc.scalar,
                     "pool": nc.gpsimd, "sp": nc.sync}
        self.nsem = 0
        self.sem = {k: self._newsem(k) for k in self.engs}
        self.cnt = {k: 0 for k in self.engs}
        self.known = {k: {} for k in self.engs}
        self.dma_sems = [self._newsem("dma") for _ in range(NDMA)]
        self.dma_cnt = [0] * NDMA
        self.dma_next = 0
        self.n_ins = 0
        self.n_wait = 0
        self.last_ev = {}
        self.pep = PEProxy(self)

    def _newsem(self, name):
        self.nsem += 1
        return self.stack.enter_context(self.nc.semaphore(f"{name}_{self.nsem}"))

    def _learn(self, eng, sem, val, snap):
        kn = self.known[eng]
        k = id(sem)
        if kn.get(k, 0) < val:
            kn[k] = val
        for k2, v2 in snap.items():
            if kn.get(k2, 0) < v2:
                kn[k2] = v2

    def _wait(self, eng, deps):
        e = self.engs[eng]
        kn = self.known[eng]
        todo = [d for d in deps if d is not None and not (d[2] == "pe" and eng == "pe")]
        todo.sort(key=lambda d: -d[1])
        for (sem, val, src, snap) in todo:
            if kn.get(id(sem), 0) >= val:
                continue
            e.wait_ge(sem, val)
            self.n_wait += 1
            self._learn(eng, sem, val, snap)

    @staticmethod
    def _deps(r, w):
        deps = []
        for x in r:
            deps.append(x.lw)
        for x in w:
            deps.append(x.lw)
            deps.extend(x.rd.values())
        return deps

    def op(self, eng, fn, r=(), w=()):
        r = [x.r if isinstance(x, Tl) else x for x in r]
        w = [x.r if isinstance(x, Tl) else x for x in w]
        self._wait(eng, self._deps(r, w))
        if eng == "pe":
            self.pep.cur_w = w
            ins = fn(self.pep)
        else:
            ins = fn(self.engs[eng])
        if self.cnt[eng] >= SEM_LIMIT:
            self.sem[eng] = self._newsem(eng)
            self.cnt[eng] = 0
        self.cnt[eng] += 1
        ins.then_inc(self.sem[eng], 1)
        ev = (self.sem[eng], self.cnt[eng], eng, dict(self.known[eng]))
        self.last_ev[eng] = ev
        for x in r:
            x.rd[eng] = ev
        for x in w:
            x.lw = ev
            x.rd = {}
        self.n_ins += 1
        return ins

    def dma(self, q, out, in_, r=(), w=(), **kw):
        r = [x.r if isinstance(x, Tl) else x for x in r]
        w = [x.r if isinstance(x, Tl) else x for x in w]
        deps = self._deps(r, w)
        i = self.dma_next
        self.dma_next = (i + 1) % NDMA
        sem = self.dma_sems[i]
        if self.dma_cnt[i] > 0:
            deps.append((sem, self.dma_cnt[i], "dma", {}))
        self._wait(q, deps)
        ins = self.engs[q].dma_start(out=out, in_=in_, **kw)
        self.dma_cnt[i] += 16
        ins.then_inc(sem, 16)
        ev = (sem, self.dma_cnt[i], "dma", dict(self.known[q]))
        key = ("dma", i)
        for x in r:
            x.rd[key] = ev
        for x in w:
            x.lw = ev
            x.rd = {}
        self.n_ins += 1
        return ins

    def fence_pe(self):
        ev = self.last_ev.get("pe")
        if ev is not None:
            sem, val, _, snap = ev
            self.engs["pe"].wait_ge(sem, val)
            self._learn("pe", sem, val, snap)
            self.n_wait += 1

    def barrier(self):
        evs = list(self.last_ev.values())
        for i in range(NDMA):
            if self.dma_cnt[i] > 0:
                evs.append((self.dma_sems[i], self.dma_cnt[i], "dma", {}))
        for e in self.engs:
            self._wait(e, [x for x in evs if x[2] != e])

    def finish(self, res_list):
        deps = [x.r.lw if isinstance(x, Tl) else x.lw for x in res_list]
        self._wait("sp", deps)


def _cm(a, K):
    a = np.asarray(a)
    return np.ascontiguousarray(a.reshape(K, 128, -1).transpose(1, 0, 2))


def _consts():
    c = {}
    s = np.arange(64)[:, None]
    t = np.arange(64)[None, :]
    mc = np.zeros((64, 2, 128), np.float32)
    mc[:, 0, :64] = (s < t)
    mc[:, 0, 64:] = (s <= t)
    mc[:, 1, :64] = (s > t)
    mc[:, 1, 64:] = (s >= t)
    c["c_maskc"] = mc
    ma = np.zeros((64, 2, 64), np.float32)
    ma[:, 0, :] = (t < s)
    ma[:, 1, :] = (t > s)
    c["c_maska"] = ma
    c["c_maska2"] = np.ascontiguousarray(ma.transpose(1, 0, 2).reshape(128, 64))
    c["c_ident"] = np.eye(128, dtype=np.float32)
    rm = np.ones((128, 512), np.float32)
    rm[:, ::64] = 0.0
    c["c_rmask"] = rm
    bo = np.zeros((128, 128), np.float32)
    bo[:64, :64] = 1.0
    bo[64:, 64:] = 1.0
    c["c_bones"] = bo
    c["c_ones"] = np.ones((128, 128), np.float32)
    sel = np.zeros((16, 16, 128), np.float32)
    for e in range(16):
        sel[e, e, :] = 1.0
    c["c_sel16"] = sel
    jk = np.arange(64)[:, None]
    jq = np.arange(64)[None, :]
    cs = np.clip(jq - 8, 0, 48)
    inwin = (jk >= cs) & (jk < cs + 16)
    nm = np.where(inwin, 0.0, NEG).astype(np.float32)
    c["c_namask"] = np.ascontiguousarray(np.concatenate([nm, nm], 0))
    return c


def _prep(inp):
    g = {k: np.asarray(v) for k, v in inp.items()}
    sh = {}
    sh["modw"] = np.ascontiguousarray(g["mod_w"].reshape(L, 8, 128, 6, 1024).transpose(0, 3, 2, 1, 4))
    sh["modb"] = np.ascontiguousarray(g["mod_b"].reshape(L, 48, 128).transpose(0, 2, 1))
    sh["g12"] = np.ascontiguousarray(
        np.stack([g["norm1_g"], g["norm2_g"]], 1).reshape(L, 2, 8, 128).transpose(0, 3, 1, 2))
    Wc = np.stack([_cm(g["w_in"][l], 8) for l in range(L)])
    sh["w_lx"] = np.ascontiguousarray(Wc[:, :, :, 1536:1920])
    rkv = np.zeros((L, 8, 128, 8, 320), np.float32)
    wqk = np.zeros((L, 8, 128, 8, 128), np.float32)
    for h in range(8):
        r_ = Wc[:, :, :, h * 64:(h + 1) * 64]
        k_ = Wc[:, :, :, 512 + h * 64:512 + (h + 1) * 64]
        v_ = Wc[:, :, :, 1024 + h * 64:1024 + (h + 1) * 64]
        rkv[:, h] = np.concatenate([r_, r_, k_, k_, v_], -1)
        wqk[:, h] = np.concatenate([Wc[:, :, :, 1920 + h * 64:1920 + (h + 1) * 64],
                                    Wc[:, :, :, 2432 + h * 64:2432 + (h + 1) * 64]], -1)
    sh["w_rkv"] = rkv
    sh["w_qk"] = wqk
    sh["w_vb"] = np.ascontiguousarray(Wc[:, :, :, 2944:3456])
    sh["w_g"] = np.ascontiguousarray(Wc[:, :, :, 3456:5504])
    wlb = np.zeros((L, 8, 128, 128), np.float32)
    alb = np.zeros((L, 8, 128, 128), np.float32)
    rwp = np.zeros((L, 128, 8, 7), np.float32)
    for h in range(8):
        hs = slice(h * 64, (h + 1) * 64)
        for d in range(2):
            ds = slice(d * 64, (d + 1) * 64)
            wlb[:, h, ds, ds] = g["rw_w_lora_b"][:, d, :, hs]
            alb[:, h, ds, ds] = g["rw_a_lora_b"][:, d, :, hs]
            rwp[:, ds, h, 0] = g["rw_w0"][:, d, hs]
            rwp[:, ds, h, 1] = g["rw_a0"][:, d, hs]
            rwp[:, ds, h, 2] = g["rw_k_k"][:, hs]
            rwp[:, ds, h, 3] = g["rw_k_a"][:, hs]
            rwp[:, ds, h, 4] = g["rw_r_k"][:, h, :]
            rwp[:, ds, h, 5] = g["rw_ln_g"][:, hs]
            rwp[:, ds, h, 6] = g["rw_ln_b"][:, hs]
    sh["wlb"] = wlb
    sh["alb"] = alb
    sh["rwp"] = rwp
    sh["glb"] = np.ascontiguousarray(g["rw_g_lora_b"])
    sh["nap"] = np.ascontiguousarray(np.stack([g["na_q_g"], g["na_k_g"]], -1))
    jk = np.arange(64)[:, None]
    jq = np.arange(64)[None, :]
    dcol = np.clip(jk - jq, -15, 15) + 15
    rp = g["na_rpb"][:, :, :, dcol]
    sh["rpbT"] = np.ascontiguousarray(rp.transpose(0, 1, 3, 2, 4))
    sh["pa"] = np.stack([_cm(g["proj_a"][l], 4) for l in range(L)])
    sh["pb"] = np.stack([_cm(g["proj_b"][l], 4) for l in range(L)])
    sh["wo"] = np.stack([_cm(g["w_out"][l], 8) for l in range(L)])
    sh["rw"] = _cm(g["router_w"], 8)
    sh["rbias"] = np.ascontiguousarray(np.broadcast_to(g["router_bias"][None, :], (128, 16)))
    sh["w1"] = np.ascontiguousarray(g["moe_w1"].reshape(L, 16, 8, 128, 512).transpose(0, 1, 3, 2, 4))
    sh["w3"] = np.ascontiguousarray(g["moe_w3"].reshape(L, 16, 8, 128, 512).transpose(0, 1, 3, 2, 4))
    sh["w2"] = np.ascontiguousarray(g["moe_w2"].reshape(L, 16, 4, 128, 1024).transpose(0, 1, 3, 2, 4))
    sh.update(_consts())
    maps = []
    for b in range(8):
        m = dict(sh)
        cat = np.concatenate([g["ctx"][b], g["x"][b]], 0)
        m["xin"] = _cm(np.ascontiguousarray(cat.T), 8)
        m["cs"] = _cm(np.stack([g["c"][b], g["c_ctx"]], -1), 8)
        maps.append(m)
    return maps


SHAPES = {
    "xin": [128, 8, NT], "cs": [128, 8, 2],
    "modw": [L, 6, 128, 8, 1024], "modb": [L, 128, 48], "g12": [L, 128, 2, 8],
    "w_lx": [L, 128, 8, 384], "w_rkv": [L, 8, 128, 8, 320], "w_qk": [L, 8, 128, 8, 128],
    "w_vb": [L, 128, 8, 512], "w_g": [L, 128, 8, 2048],
    "wlb": [L, 8, 128, 128], "alb": [L, 8, 128, 128], "rwp": [L, 128, 8, 7], "glb": [L, 128, 512],
    "nap": [L, 64, 2], "rpbT": [L, 8, 64, 15, 64],
    "pa": [L, 128, 4, 1024], "pb": [L, 128, 4, 1024], "wo": [L, 128, 8, 1024],
    "rw": [128, 8, 16], "rbias": [128, 16],
    "w1": [L, 16, 128, 8, 512], "w3": [L, 16, 128, 8, 512], "w2": [L, 16, 128, 4, 1024],
    "c_maskc": [64, 2, 128], "c_maska": [64, 2, 64], "c_maska2": [128, 64], "c_ident": [128, 128], "c_rmask": [128, 512],
    "c_bones": [128, 128], "c_ones": [128, 128], "c_sel16": [16, 16, 128], "c_namask": [128, 64],
}


class Ctx:
    pass


def build(n_layers=L, dbg=(), skip=()):
    nc = bass.Bass("TRN2", target_bir_lowering=False)
    D = {k: nc.dram_tensor(k, s, F32, kind="ExternalInput").ap() for k, s in SHAPES.items()}
    yout = nc.dram_tensor("yout", [128, 8, NT - NCTX], F32, kind="ExternalOutput").ap()
    xres = nc.dram_tensor("xres", [128, 8, NT], F32, kind="Internal").ap()
    dbg_out = {}
    for name in dbg:
        dbg_out[name] = nc.dram_tensor("dbg_" + name, [128, 8, NT], F32, kind="ExternalOutput").ap()
    K = Ctx()
    K.nc, K.D, K.dbg = nc, D, dbg_out
    K.skip = skip
    with ExitStack() as st:
        P = Prog(nc, st)
        K.P = P

        K.nsb = 0

        def sb(stack, name, shape, dt):
            K.nsb += 1
            return Tl(stack.enter_context(nc.sbuf_tensor(f"s{K.nsb}_{name}", shape, dt)))

        K.sb = sb
        K.PS = [Tl(st.enter_context(nc.psum_tensor(f"ps{i}", [128, 512], F32))) for i in range(7)]
        K.PSB = Tl(st.enter_context(nc.psum_tensor("psb", [128, 1024], BF16)))
        K.ps_i = 0

        def ps():
            t = K.PS[K.ps_i % 5]
            K.ps_i += 1
            return t

        K.ps = ps
        K.PSL = K.PS[6]
        K.PSL2 = K.PS[5]
        K.identf = sb(st, "identf", [128, 128], F32)
        K.identb = sb(st, "identb", [128, 128], BF16)
        K.onesb = sb(st, "onesb", [128, 128], BF16)
        K.onesf = sb(st, "onesf", [128, 128], F32)
        K.bones = sb(st, "bones", [128, 128], F32)
        K.maskc = sb(st, "maskc", [64, 2, 128], F32)
        K.maska = sb(st, "maska", [64, 2, 64], F32)
        K.maska2 = sb(st, "maska2", [128, 64], F32)
        K.rmask = sb(st, "rmask", [128, 512], F32)
        K.namask = sb(st, "namask", [128, 64], F32)
        K.rwt = sb(st, "rwt", [128, 8, 16], F32)
        K.rbias = sb(st, "rbias", [128, 16], F32)
        K.cs = sb(st, "cs", [128, 8, 2], F32)
        K.csb = sb(st, "csb", [128, 8, 2], BF16)
        for t, nm in [(K.identf, "c_ident"), (K.onesf, "c_ones"), (K.bones, "c_bones"), (K.maskc, "c_maskc"),
                      (K.maska, "c_maska"), (K.maska2, "c_maska2"), (K.rmask, "c_rmask"), (K.namask, "c_namask"),
                      (K.rwt, "rw"), (K.rbias, "rbias"), (K.cs, "cs")]:
            P.dma("sp", t.t[:], D[nm], w=[t])
        P.dma("pool", K.identb.t[:], D["c_ident"], w=[K.identb])
        P.dma("pool", K.onesb.t[:], D["c_ones"], w=[K.onesb])
        K.maskcb = sb(st, "maskcb", [64, 2, 128], BF16)
        K.maskab = sb(st, "maskab", [64, 2, 64], BF16)
        P.dma("pool", K.maskcb.t[:], D["c_maskc"], w=[K.maskcb])
        P.dma("pool", K.maskab.t[:], D["c_maska"], w=[K.maskab])
        P.op("act", lambda e: e.activation(out=K.csb.t[:], in_=K.cs.t[:], func=AF.Silu), r=[K.cs], w=[K.csb])
        K.mt = sb(st, "mt", [128, 48, 2], F32)
        K.mul1 = sb(st, "mul1", [128, 8, 2], F32)
        K.mul2 = sb(st, "mul2", [128, 8, 2], F32)
        K.g12 = sb(st, "g12", [128, 2, 8], F32)
        K.modb = sb(st, "modb", [128, 48], F32)
        K.HT = sb(st, "HT", [128, 8, NT], BF16)

        for l in range(n_layers):
            K.l = l
            with ExitStack() as ph:
                XT = sb(ph, "XT", [128, 8, NT], F32)
                P.dma("sp", XT.t[:], D["xin"] if l == 0 else xres, w=[XT])
                phase_adaln(K, ph)
                phase_norm(K, ph, XT, K.mul1, 0, None)
                if "h%d" % l in dbg_out:
                    dump_bf(K, ph, K.HT, dbg_out["h%d" % l])
                if l == 0:
                    P.dma("sp", xres, XT.t[:], r=[XT], w=[Res()])
                P.barrier()
            with ExitStack() as ph:
                YA = sb(ph, "YA", [128, 4, NT], BF16)
                if "rwkv" in K.skip:
                    P.op("dve", lambda e: e.memset(YA.t[:], 0.0), w=[YA])
                else:
                    with ExitStack() as ph2:
                        phase_rwkv(K, ph2, YA)
                        P.barrier()
                if "ya%d" % l in dbg_out:
                    with ExitStack() as ph2:
                        dump_bf(K, ph2, YA, dbg_out["ya%d" % l], 4)
                        P.barrier()
                YB = sb(ph, "YB", [128, 4, NT], BF16)
                if "na" in K.skip:
                    P.op("dve", lambda e: e.memset(YB.t[:], 0.0), w=[YB])
                else:
                    with ExitStack() as ph2:
                        phase_na(K, ph2, YB)
                        P.barrier()
                if "yb%d" % l in dbg_out:
                    with ExitStack() as ph2:
                        dump_bf(K, ph2, YB, dbg_out["yb%d" % l], 4)
                        P.barrier()
                with ExitStack() as ph2:
                    phase_merge(K, ph2, YA, YB, xres)
                    P.barrier()
            with ExitStack() as ph:
                XT = sb(ph, "XT2", [128, 8, NT], F32)
                P.dma("sp", XT.t[:], xres, w=[XT])
                if "xm%d" % l in dbg_out:
                    P.dma("sp", dbg_out["xm%d" % l], XT.t[:], r=[XT], w=[Res()])
                if "moe" not in K.skip:
                    phase_moe(K, ph, XT)
                if l == n_layers - 1:
                    ry = Res()
                    P.dma("sp", yout, XT.t[:, :, NCTX:NT], r=[XT], w=[ry])
                    P.finish([ry])
                else:
                    rx = Res()
                    P.dma("sp", xres, XT.t[:], r=[XT], w=[rx])
                if "xo%d" % l in dbg_out:
                    P.dma("sp", dbg_out["xo%d" % l], XT.t[:], r=[XT], w=[Res()])
                P.barrier()
        print("instructions", P.n_ins, "waits", P.n_wait, "sems", P.nsem)
    return nc


def dump_bf(K, ph, src, dst, nchunk=8):
    P = K.P
    tmp = K.sb(ph, "dbgtmp", [128, NT], F32)
    for c in range(nchunk):
        P.op("dve", lambda e: e.tensor_copy(out=tmp.t[:], in_=src.t[:, c, :]), r=[src], w=[tmp])
        P.dma("sp", dst[:, c, :], tmp.t[:], r=[tmp], w=[Res()])


def phase_adaln(K, ph):
    P, D, l = K.P, K.D, K.l
    P.dma("sp", K.g12.t[:], D["g12"][l], w=[K.g12])
    P.dma("sp", K.modb.t[:], D["modb"][l], w=[K.modb])
    MW = [K.sb(ph, f"mw{i}", [128, 8, 1024], BF16) for i in range(2)]
    pm = K.ps()
    for g in range(6):
        mw = MW[g % 2]
        P.dma("pool", mw.t[:], D["modw"][l, g], w=[mw])
        for j in range(8):
            o = (g * 8 + j) * 2
            for kc in range(8):
                P.op("pe", lambda e: e.matmul(pm.t[:, o:o + 2], lhsT=mw.t[:, kc, j * 128:(j + 1) * 128],
                                              rhs=K.csb.t[:, kc, :], start=(kc == 0), stop=(kc == 7)),
                     r=[mw, K.csb], w=[pm])
    P.op("dve", lambda e: e.tensor_tensor(out=K.mt.t[:], in0=pm.t[:, 0:96].rearrange("p (j c) -> p j c", c=2),
                                          in1=K.modb.t[:].unsqueeze(2).to_broadcast([128, 48, 2]), op=ALU.add),
         r=[pm, K.modb], w=[K.mt])
    for (mul, gi, so) in [(K.mul1, 0, 8), (K.mul2, 1, 32)]:
        P.op("dve", lambda e: e.tensor_scalar_add(out=mul.t[:], in0=K.mt.t[:, so:so + 8, :], scalar1=1.0),
             r=[K.mt], w=[mul])
        P.op("dve", lambda e: e.tensor_tensor(out=mul.t[:], in0=mul.t[:],
                                              in1=K.g12.t[:, gi, :].unsqueeze(2).to_broadcast([128, 8, 2]),
                                              op=ALU.mult), r=[mul, K.g12], w=[mul])


def phase_norm(K, ph, XT, mul, shift_off, h2f_cb):
    P = K.P
    SQ = K.sb(ph, "n_sq", [128, 8, 512], BF16)
    RS = K.sb(ph, "n_rs", [128, 512], F32)
    TMP = [K.sb(ph, f"n_tmp{i}", [128, 512], F32) for i in range(2)]
    HF = K.sb(ph, "n_hf", [128, 8, 512], F32) if h2f_cb is not None else None
    for (n0, nb) in BLKS:
        col = 1 if n0 == 0 else 0
        P.op("act", lambda e: e.activation(out=SQ.t[:, :, :nb], in_=XT.t[:, :, n0:n0 + nb], func=AF.Square),
             r=[XT], w=[SQ])
        pa = K.ps()
        for c in range(8):
            P.op("pe", lambda e: e.matmul(pa.t[:, :nb], lhsT=K.onesb.t[:], rhs=SQ.t[:, c, :nb],
                                          start=(c == 0), stop=(c == 7)), r=[K.onesb, SQ], w=[pa])
        P.op("act", lambda e: e.activation(out=RS.t[:, :nb], in_=pa.t[:, :nb], func=AF.Sqrt,
                                           bias=RMS_EPS, scale=1.0 / 1024), r=[pa], w=[RS])
        P.op("dve", lambda e: e.reciprocal(out=RS.t[:, :nb], in_=RS.t[:, :nb]), r=[RS], w=[RS])
        for c in range(8):
            tmp = TMP[c % 2]
            P.op("dve", lambda e: e.scalar_tensor_tensor(out=tmp.t[:, :nb], in0=XT.t[:, c, n0:n0 + nb],
                                                         scalar=mul.t[:, c, col:col + 1], in1=RS.t[:, :nb],
                                                         op0=ALU.mult, op1=ALU.mult),
                 r=[XT, mul, RS], w=[tmp])
            P.op("act", lambda e: e.activation(out=K.HT.t[:, c, n0:n0 + nb], in_=tmp.t[:, :nb], func=AF.Identity,
                                               bias=K.mt.t[:, shift_off + c, col:col + 1], scale=1.0),
                 r=[tmp, K.mt], w=[K.HT])
            if HF is not None:
                P.op("act", lambda e: e.activation(out=HF.t[:, c, :nb], in_=tmp.t[:, :nb], func=AF.Identity,
                                                   bias=K.mt.t[:, shift_off + c, col:col + 1], scale=1.0),
                     r=[tmp, K.mt], w=[HF])
        if h2f_cb is not None:
            h2f_cb(n0, nb, HF)


def phase_rwkv(K, ph, YA):
    P, D, l, HT, sb = K.P, K.D, K.l, K.HT, K.sb
    WLX = sb(ph, "wlx", [128, 8, 384], BF16)
    GLB = sb(ph, "glb", [128, 512], BF16)
    RWP = sb(ph, "rwp", [128, 8, 7], F32)
    OMKA = sb(ph, "omka", [128, 8], F32)
    P.dma("pool", WLX.t[:], D["w_lx"][l], w=[WLX])
    P.dma("pool", GLB.t[:], D["glb"][l], w=[GLB])
    P.dma("sp", RWP.t[:], D["rwp"][l], w=[RWP])
    P.op("dve", lambda e: e.tensor_scalar(out=OMKA.t[:], in0=RWP.t[:, :, 3], scalar1=-1.0, scalar2=1.0,
                                          op0=ALU.mult, op1=ALU.add), r=[RWP], w=[OMKA])
    TLW = sb(ph, "tlw", [128, NT], BF16)
    LA = sb(ph, "la", [128, NT], BF16)
    SLG = sb(ph, "slg", [128, NT], BF16)
    for (n0, nb) in BLKS:
        for j, (dst, fn) in enumerate([(TLW, AF.Tanh), (LA, AF.Identity), (SLG, AF.Sigmoid)]):
            p = K.ps()
            for kc in range(8):
                P.op("pe", lambda e: e.matmul(p.t[:, :nb], lhsT=WLX.t[:, kc, j * 128:(j + 1) * 128],
                                              rhs=HT.t[:, kc, n0:n0 + nb], start=(kc == 0), stop=(kc == 7)),
                     r=[WLX, HT], w=[p])
            P.op("act", lambda e: e.activation(out=dst.t[:, n0:n0 + nb], in_=p.t[:, :nb], func=fn), r=[p], w=[dst])

    import os as _os
    STOP = int(_os.environ.get("RW_STOP", "99"))
    P.op("dve", lambda e: e.memset(YA.t[:], 0.0), w=[YA])
    if STOP == 0:
        return
    WR = [sb(ph, f"wr{i}", [128, 8, 320], BF16) for i in range(1)]
    WL = [sb(ph, f"wl{i}", [128, 128], BF16) for i in range(2)]
    AL = [sb(ph, f"al{i}", [128, 128], BF16) for i in range(2)]
    QQ = sb(ph, "qq", [128, NCH, 128], BF16)
    KB = sb(ph, "kb", [128, NT], BF16)
    KKt = sb(ph, "kk", [128, NT], BF16)
    KH = sb(ph, "kh", [64, NCH, 128], BF16)
    BH = sb(ph, "bh", [64, NCH, 128], BF16)
    EG = sb(ph, "eg", [128, NCH], F32)
    EGS = sb(ph, "egs", [128, NCH], F32)
    BS = sb(ph, "bs", [64, NT], BF16)
    V64 = sb(ph, "v64", [64, NCH, 64], BF16)
    VT = sb(ph, "vt", [64, NT], BF16)
    YTd = [sb(ph, f"yt{d}", [64, NT], BF16) for d in range(2)]
    SCR = []
    for pi_ in range(2):
        sc = {n: sb(ph, f"rs{pi_}_" + n, [128, 512], F32) for n in ["rf", "kf", "af", "lw", "g", "s1", "s2", "s3"]}
        sc["kht"] = sb(ph, f"rs{pi_}_kht", [128, 512], BF16)
        sc["bht"] = sb(ph, f"rs{pi_}_bht", [128, 512], BF16)
        sc["tot"] = sb(ph, f"rs{pi_}_tot", [128, 8], F32)
        SCR.append(sc)
    Rf, Kf, Af, LW, G, S1, S2, S3 = [SCR[0][n] for n in ["rf", "kf", "af", "lw", "g", "s1", "s2", "s3"]]
    NBT = 4
    for t__ in (QQ, KB, KKt, KH, BH, EG, EGS, V64):
        t__.rb = [Res() for _ in BLKS]
    bof = lambda c: 0 if c < 4 else 1 + (c - 4) // 8
    NSET = 3
    ATm = [[sb(ph, f"atm{i}{d}", [64, NBT, 128], BF16) for d in range(2)] for i in range(NSET)]
    BTm = [[sb(ph, f"btm{i}{d}", [64, NBT, 128], BF16) for d in range(2)] for i in range(NSET)]
    SSs = [[sb(ph, f"ss{q}{i}", [128, NBT, 192], BF16) for i in range(2)] for q in range(2)]
    SSrs = [[[{k: Res() for k in "MXA"} for d in range(2)] for i in range(2)] for q in range(2)]
    X5 = [[sb(ph, f"x5{i}{d}", [64, NBT, 64], BF16) for d in range(2)] for i in range(NSET)]
    T1 = sb(ph, "t1", [64, 2, 64], BF16)
    NU = sb(ph, "nu", [64, 2, 64], BF16)
    Hf = sb(ph, "hf", [128, 64], F32)
    Hb = sb(ph, "hb", [128, 64], BF16)
    order1 = [3, 2, 1, 0] + list(range(35, 3, -1))

    def v3(ap, j=64):
        return ap.rearrange("p (c j) -> p c j", j=j)

    for h in range(8):
        wr, wl, al = WR[0], WL[h % 2], AL[h % 2]
        P.dma("pool", wr.t[:], D["w_rkv"][l, h], w=[wr])
        P.dma("pool", wl.t[:], D["wlb"][l, h], w=[wl])
        P.dma("pool", al.t[:], D["alb"][l, h], w=[al])
        prm = lambda i, lo=0, hi=128: RWP.t[lo:hi, h, i:i + 1]
        def gen_elem(bi, par_):
            sc_ = SCR[par_]
            Rf, Kf, Af, LW, G, S1, S2, S3 = [sc_[n] for n in ["rf", "kf", "af", "lw", "g", "s1", "s2", "s3"]]
            KHT, BHT, TOT = sc_["kht"], sc_["bht"], sc_["tot"]
            for bi in [bi]:
                n0, nb = BLKS[bi]
                ncb, c0 = nb // 64, n0 // 64
                blk = slice(n0, n0 + nb)

                def proj(dst, lo, hi, M, eng, outdt_t=None):
                    p = K.ps()
                    for kc in range(8):
                        P.op("pe", lambda e: e.matmul(p.t[0:M, :nb], lhsT=wr.t[:, kc, lo:hi], rhs=HT.t[:, kc, blk],
                                                      start=(kc == 0), stop=(kc == 7)), r=[wr, HT], w=[p])
                    if eng == "act":
                        P.op("act", lambda e: e.activation(out=dst, in_=p.t[0:M, :nb], func=AF.Identity), r=[p],
                             w=[outdt_t])
                    else:
                        P.op("dve", lambda e: e.tensor_copy(out=dst, in_=p.t[0:M, :nb]), r=[p], w=[outdt_t])

                yield
                proj(Rf.t[:, :nb], 0, 128, 128, "act", Rf)
                yield
                proj(Kf.t[:, :nb], 128, 256, 128, "dve", Kf)
                yield
                proj(VT.t[:, blk], 256, 320, 64, "act", VT)
                yield
                p = K.ps()
                P.op("pe", lambda e: e.matmul(p.t[:, :nb], lhsT=wl.t[:], rhs=TLW.t[:, blk], start=True, stop=True),
                     r=[wl, TLW], w=[p])
                P.op("act", lambda e: e.activation(out=LW.t[:, :nb], in_=p.t[:, :nb], func=AF.Sigmoid,
                                                   bias=prm(0), scale=1.0), r=[p, RWP], w=[LW])
                yield
                p = K.ps()
                P.op("pe", lambda e: e.matmul(p.t[:, :nb], lhsT=al.t[:], rhs=LA.t[:, blk], start=True, stop=True),
                     r=[al, LA], w=[p])
                P.op("act", lambda e: e.activation(out=Af.t[:, :nb], in_=p.t[:, :nb], func=AF.Sigmoid,
                                                   bias=prm(1), scale=1.0), r=[p, RWP], w=[Af])
                yield
                P.op("dve", lambda e: e.tensor_scalar_mul(out=S1.t[:, :nb], in0=Kf.t[:, :nb], scalar1=prm(2)),
                     r=[Kf, RWP], w=[S1])
                P.op("act", lambda e: e.activation(out=S2.t[:, :nb], in_=S1.t[:, :nb], func=AF.Square), r=[S1], w=[S2])
                yield
                p = K.ps()
                P.op("pe", lambda e: e.matmul(p.t[:, :nb], lhsT=K.bones.t[:], rhs=S2.t[:, :nb], start=True, stop=True),
                     r=[K.bones, S2], w=[p])
                P.op("act", lambda e: e.activation(out=S2.t[:, :nb], in_=p.t[:, :nb], func=AF.Sqrt), r=[p], w=[S2])
                P.op("dve", lambda e: e.tensor_scalar_max(out=S2.t[:, :nb], in0=S2.t[:, :nb], scalar1=1e-6),
                     r=[S2], w=[S2])
                yield
                P.op("dve", lambda e: e.reciprocal(out=S2.t[:, :nb], in_=S2.t[:, :nb]), r=[S2], w=[S2])
                P.op("dve", lambda e: e.tensor_mul(out=S1.t[:, :nb], in0=S1.t[:, :nb], in1=S2.t[:, :nb]),
                     r=[S1, S2], w=[S1])
                P.op("dve", lambda e: e.tensor_scalar(out=S2.t[:, :nb], in0=Af.t[:, :nb], scalar1=prm(3),
                                                      scalar2=OMKA.t[:, h:h + 1], op0=ALU.mult, op1=ALU.add),
                     r=[Af, RWP, OMKA], w=[S2])
                yield
                P.op("dve", lambda e: e.tensor_mul(out=S2.t[:, :nb], in0=S2.t[:, :nb], in1=Kf.t[:, :nb]),
                     r=[S2, Kf], w=[S2])
                P.op("dve", lambda e: e.tensor_mul(out=S3.t[:, :nb], in0=S1.t[:, :nb], in1=Af.t[:, :nb]),
                     r=[S1, Af], w=[S3])
                yield
                P.op("dve", lambda e: e.scalar_tensor_tensor(out=Af.t[0:64, :nb], in0=Rf.t[0:64, :nb], scalar=prm(4, 0, 64),
                                                             in1=Kf.t[0:64, :nb], op0=ALU.mult, op1=ALU.mult),
                     r=[Rf, Kf, RWP], w=[Af])
                yield
                p = K.ps()
                P.op("pe", lambda e: e.matmul(p.t[0:64, :nb], lhsT=K.bones.t[0:64, 0:64], rhs=Af.t[0:64, :nb],
                                              start=True, stop=True), r=[K.bones, Af], w=[p])
                P.op("act", lambda e: e.activation(out=BS.t[:, blk], in_=p.t[0:64, :nb], func=AF.Identity),
                     r=[p], w=[BS])
                if STOP == 1:
                    continue
                yield
                P.op("dve", lambda e: e.tensor_scalar_mul(out=LW.t[:, :nb], in0=LW.t[:, :nb], scalar1=DEC_C),
                     r=[LW], w=[LW])
                P.op("dve", lambda e: e.tensor_tensor_scan(out=G.t[:, :nb], data0=K.rmask.t[:, :nb], data1=LW.t[:, :nb],
                                                           initial=0.0, op0=ALU.mult, op1=ALU.add),
                     r=[K.rmask, LW], w=[G])
                P.op("dve", lambda e: e.tensor_copy(out=TOT.t[64:128, :ncb], in_=v3(G.t[64:128, :nb])[:, :, 63]),
                     r=[G], w=[TOT])
                yield
                P.op("dve", lambda e: e.tensor_sub(out=Af.t[64:128, :nb], in0=LW.t[64:128, :nb], in1=G.t[64:128, :nb]),
                     r=[LW, G], w=[Af])
                P.op("dve", lambda e: e.tensor_tensor(out=v3(G.t[64:128, :nb]), in0=v3(Af.t[64:128, :nb]),
                                                      in1=TOT.t[64:128, :ncb].unsqueeze(2).to_broadcast([64, ncb, 64]),
                                                      op=ALU.add), r=[Af, TOT], w=[G])
                P.op("dve", lambda e: e.tensor_sub(out=LW.t[:, :nb], in0=G.t[:, :nb], in1=LW.t[:, :nb]),
                     r=[G, LW], w=[LW])
                yield
                P.op("act", lambda e: e.activation(out=LW.t[:, :nb], in_=LW.t[:, :nb], func=AF.Exp), r=[LW], w=[LW])
                P.op("dve", lambda e: e.tensor_mul(out=QQ.t[:, c0:c0 + ncb, 0:64], in0=v3(S1.t[:, :nb]),
                                                   in1=v3(LW.t[:, :nb])), r=[S1, LW], w=[QQ.rb[bi]])
                P.op("act", lambda e: e.activation(out=S1.t[:, :nb], in_=G.t[:, :nb], func=AF.Exp), r=[G], w=[S1])
                yield
                P.op("dve", lambda e: e.tensor_mul(out=QQ.t[:, c0:c0 + ncb, 64:128], in0=v3(Rf.t[:, :nb]),
                                                   in1=v3(S1.t[:, :nb])), r=[Rf, S1], w=[QQ.rb[bi]])
                P.op("dve", lambda e: e.tensor_copy(out=EG.t[0:64, c0:c0 + ncb], in_=v3(S1.t[0:64, :nb])[:, :, 63]),
                     r=[S1], w=[EG.rb[bi]])
                P.op("dve", lambda e: e.tensor_copy(out=EG.t[64:128, c0:c0 + ncb], in_=v3(S1.t[64:128, :nb])[:, :, 0]),
                     r=[S1], w=[EG.rb[bi]])
                s_hi = (3 - c0) if c0 < 4 else (39 - c0)
                s_lo = s_hi - ncb
                osl = slice(s_hi, (s_lo if s_lo >= 0 else None), -1)
                P.op("act", lambda e: e.activation(out=EGS.t[0:64, c0:c0 + ncb], in_=v3(S1.t[0:64, :nb])[:, :, 63],
                                                   func=AF.Identity), r=[S1], w=[EGS.rb[bi]])
                P.op("act", lambda e: e.activation(out=EGS.t[64:128, osl], in_=v3(S1.t[64:128, :nb])[:, :, 0],
                                                   func=AF.Identity), r=[S1], w=[EGS.rb[bi]])
                yield
                P.op("act", lambda e: e.activation(out=LW.t[:, :nb], in_=G.t[:, :nb], func=AF.Exp, scale=-1.0),
                     r=[G], w=[LW])
                P.op(ELT_ENG, lambda e: e.tensor_mul(out=KB.t[:, blk], in0=S3.t[:, :nb], in1=LW.t[:, :nb]),
                     r=[S3, LW], w=[KB.rb[bi]])
                P.op(ELT_ENG, lambda e: e.tensor_mul(out=KKt.t[:, blk], in0=S2.t[:, :nb], in1=LW.t[:, :nb]),
                     r=[S2, LW], w=[KKt.rb[bi]])
                egb = EG.t[:, c0:c0 + ncb].unsqueeze(2).to_broadcast([128, ncb, 64])
                yield
                P.op(ELT_ENG, lambda e: e.tensor_tensor(out=v3(KHT.t[:, :nb]), in0=v3(KKt.t[:, blk]), in1=egb, op=ALU.mult),
                     r=[KKt.rb[bi], EG.rb[bi]], w=[KHT])
                P.op(ELT_ENG, lambda e: e.tensor_tensor(out=v3(BHT.t[:, :nb]), in0=v3(KB.t[:, blk]), in1=egb, op=ALU.mult),
                     r=[KB.rb[bi], EG.rb[bi]], w=[BHT])
                if STOP == 2:
                    continue
                yield
                for g0 in range(0, ncb, 8):
                    g1 = min(ncb, g0 + 8)
                    p = K.ps()
                    for j in range(g0, g1):
                        c = c0 + j
                        for kc in range(8):
                            P.op("pe", lambda e: e.matmul(p.t[0:64, (j - g0) * 64:(j - g0 + 1) * 64],
                                                          lhsT=HT.t[:, kc, c * 64:(c + 1) * 64], rhs=wr.t[:, kc, 256:320],
                                                          start=(kc == 0), stop=(kc == 7)), r=[HT, wr], w=[p])
                    P.op("act", lambda e: e.activation(out=V64.t[:, c0 + g0:c0 + g1, :],
                                                       in_=v3(p.t[0:64, 0:(g1 - g0) * 64]), func=AF.Identity),
                         r=[p], w=[V64.rb[bi]])
                    for (src, dst) in [(KHT, KH), (BHT, BH)]:
                        for j in range(g0, g1):
                            c = c0 + j
                            P.op("pe", lambda e: e.transpose(K.PSB.t[0:64, (j - g0) * 128:(j - g0 + 1) * 128],
                                                             src.t[:, j * 64:(j + 1) * 64], K.identb.t[:]),
                                 r=[src, K.identb], w=[K.PSB])
                        P.op("dve", lambda e: e.tensor_copy(out=dst.t[:, c0 + g0:c0 + g1, :],
                                                            in_=v3(K.PSB.t[0:64, 0:(g1 - g0) * 128], 128)),
                             r=[K.PSB], w=[dst.rb[bi]])

                yield

        P.op("dve", lambda e: e.memset(Hf.t[:], 0.0), w=[Hf])
        P.op("dve", lambda e: e.memset(Hb.t[:], 0.0), w=[Hb])
        dP = [slice(0, 64), slice(64, 128)]
        cdir = [list(range(NCH)), order1]
        batches = [list(range(i, min(NCH, i + NBT))) for i in range(0, NCH, NBT)]
        idbb = K.identb.t[0:64, 0:64].unsqueeze(1).to_broadcast([64, NBT, 64])

        def gen_prep(b):
            par = b % NSET
            SS, SSr = SSs[b % 2], SSrs[b % 2]
            bt = batches[b]
            nb_ = len(bt)
            cur = [0, 0]

            def cp(o, i_, rd, wr):
                P.op("act", lambda e: e.activation(out=o, in_=i_, func=AF.Identity), r=rd, w=wr)

            for d in range(2):
                q = dP[d]
                atm, btm = ATm[par][d], BTm[par][d]
                t_, r_ = SS[cur[d]].t, SSr[cur[d]][d]
                p1, p2, p3 = K.ps(), K.ps(), K.ps()
                for j, i in enumerate(bt):
                    c = cdir[d][i]
                    ck = slice(c * 64, c * 64 + 64)
                    P.op("pe", lambda e: e.matmul(p1.t[0:64, j * 128:(j + 1) * 128], lhsT=KB.t[q, ck],
                                                  rhs=QQ.t[q, c, :], start=True, stop=True), r=[KB.rb[bof(c)], QQ.rb[bof(c)]], w=[p1])
                    P.op("pe", lambda e: e.matmul(p2.t[0:64, j * 128:(j + 1) * 128], lhsT=KKt.t[q, ck],
                                                  rhs=QQ.t[q, c, :], start=True, stop=True), r=[KKt.rb[bof(c)], QQ.rb[bof(c)]], w=[p2])
                    P.op("pe", lambda e: e.matmul(p3.t[q, j * 64:(j + 1) * 64], lhsT=QQ.t[q, c, 0:64],
                                                  rhs=KB.t[q, ck], start=True, stop=True), r=[KB.rb[bof(c)], QQ.rb[bof(c)]], w=[p3])
                mcf = K.maskc.t[:, d, :].unsqueeze(1).to_broadcast([64, nb_, 128])
                maf = K.maska2.t[q, :].unsqueeze(1).to_broadcast([64, nb_, 64])
                idq = K.identb.t[q, q].unsqueeze(1).to_broadcast([64, nb_, 64])
                P.op("dve", lambda e: e.tensor_tensor(out=atm.t[:, :nb_, :], in0=v3(p1.t[0:64, 0:nb_ * 128], 128),
                                                      in1=mcf, op=ALU.mult), r=[p1, K.maskc], w=[atm])
                P.op("dve", lambda e: e.tensor_tensor(out=t_[q, :nb_, 128:192], in0=v3(p3.t[q, 0:nb_ * 64]),
                                                      in1=maf, op=ALU.mult), r=[p3, K.maska2], w=[r_["A"]])
                cp(t_[q, :nb_, 64:128], atm.t[:, :nb_, 0:64], [atm], [r_["M"]])
                P.op("dve", lambda e: e.tensor_tensor(out=btm.t[:, :nb_, :], in0=v3(p2.t[0:64, 0:nb_ * 128], 128),
                                                      in1=mcf, op=ALU.mult), r=[p2, K.maskc], w=[btm])
                P.op("dve", lambda e: e.tensor_tensor(out=t_[q, :nb_, 0:64], in0=idq, in1=t_[q, :nb_, 64:128],
                                                      op=ALU.subtract), r=[K.identb, r_["M"]], w=[r_["X"]])
                yield
            for lv in range(1, 6):
                last = lv == 5
                for d in range(2):
                    q = dP[d]
                    t_, r_ = SS[cur[d]].t, SSr[cur[d]][d]
                    nxt = 1 - cur[d]
                    tn, rn = SS[nxt].t, SSr[nxt][d]
                    pj = K.ps()
                    for j in range(nb_):
                        P.op("pe", lambda e: e.matmul(pj.t[q, j * 128 + 64:j * 128 + 128], lhsT=t_[q, j, 64:128],
                                                      rhs=t_[q, j, 128:192], start=True, stop=True), r=[r_["M"], r_["A"]], w=[pj])
                        if not last:
                            P.op("pe", lambda e: e.matmul(pj.t[q, j * 128:j * 128 + 64], lhsT=t_[q, j, 128:192],
                                                          rhs=t_[q, j, 64:128], start=True, stop=True), r=[r_["M"], r_["A"]], w=[pj])
                    pv = v3(pj.t[q, 0:nb_ * 128], 128)
                    if not last:
                        cp(tn[q, :nb_, 64:192], pv, [pj], [rn["M"], rn["A"]])
                    else:
                        cp(tn[q, :nb_, 128:192], pv[:, :, 64:128], [pj], [rn["A"]])
                    yield
                for d in range(2):
                    q = dP[d]
                    t_, r_ = SS[cur[d]].t, SSr[cur[d]][d]
                    nxt = 1 - cur[d]
                    tn, rn = SS[nxt].t, SSr[nxt][d]
                    px = K.ps()
                    for j in range(nb_):
                        P.op("pe", lambda e: e.matmul(px.t[q, j * 64:(j + 1) * 64], lhsT=tn[q, j, 128:192],
                                                      rhs=t_[q, j, 0:64], start=True, stop=True), r=[rn["A"], r_["X"]], w=[px])
                    xo = X5[par][d].t[:, :nb_, :] if last else tn[q, :nb_, 0:64]
                    xr = X5[par][d] if last else rn["X"]
                    P.op("dve", lambda e: e.tensor_tensor(out=xo, in0=v3(px.t[q, 0:nb_ * 64]), in1=t_[q, :nb_, 0:64],
                                                          op=ALU.add), r=[px, r_["X"]], w=[xr])
                    cur[d] = nxt
                    yield

        def gen_chain(b):
            par = b % NSET
            for j, i in enumerate(batches[b]):
                cd = (cdir[0][i], cdir[1][i])
                ck = [slice(cd[d] * 64, cd[d] * 64 + 64) for d in range(2)]
                atm = [ATm[par][d] for d in range(2)]
                btm = [BTm[par][d] for d in range(2)]
                x5 = [X5[par][d] for d in range(2)]
                pt, py, phh = K.ps(), K.ps(), K.ps()

                def mm(o, l_, r_, st_, sp_, rd, wr):
                    P.op("pe", lambda e: e.matmul(o, lhsT=l_, rhs=r_, start=st_, stop=sp_), r=rd, w=[wr])

                for d in range(2):
                    mm(pt.t[0:64, d * 64:(d + 1) * 64], btm[d].t[:, j, 0:64], V64.t[:, cd[d], :], True, False, [btm[d], V64.rb[bof(cd[d])]], pt)
                    mm(pt.t[0:64, d * 64:(d + 1) * 64], QQ.t[dP[d], cd[d], 0:64], Hb.t[dP[d], :], False, True, [QQ.rb[bof(cd[d])], Hb], pt)
                P.op("act", lambda e: e.activation(out=T1.t[:], in_=v3(pt.t[0:64, 0:128]), func=AF.Identity),
                     r=[pt], w=[T1])
                yield
                pu = K.ps()
                for d in range(2):
                    mm(pu.t[0:64, d * 64:(d + 1) * 64], x5[d].t[:, j, :], T1.t[:, d, :], True, True, [x5[d], T1], pu)
                P.op("dve", lambda e: e.tensor_scalar_mul(out=NU.t[:], in0=v3(pu.t[0:64, 0:128]), scalar1=-1.0),
                     r=[pu], w=[NU])
                yield
                for d in range(2):
                    mm(phh.t[dP[d], 0:64], KH.t[:, cd[d], dP[d]], V64.t[:, cd[d], :], True, False, [KH.rb[bof(cd[d])], V64.rb[bof(cd[d])]], phh)
                    mm(phh.t[dP[d], 0:64], BH.t[:, cd[d], dP[d]], NU.t[:, d, :], False, True, [BH.rb[bof(cd[d])], NU], phh)
                for d in range(2):
                    o = py.t[0:64, d * 64:(d + 1) * 64]
                    mm(o, V64.t[:, cd[d], :], btm[d].t[:, j, 64:128], True, False, [V64.rb[bof(cd[d])], btm[d]], py)
                    mm(o, NU.t[:, d, :], atm[d].t[:, j, 64:128], False, False, [NU, atm[d]], py)
                    mm(o, Hb.t[dP[d], :], QQ.t[dP[d], cd[d], 64:128], False, True, [Hb, QQ.rb[bof(cd[d])]], py)
                egr = [EGS.rb[bof(cd[0])], EGS.rb[bof(cd[1])]]
                P.op("dve", lambda e: e.scalar_tensor_tensor(out=Hb.t[:], in0=Hf.t[:], scalar=EGS.t[:, i:i + 1],
                                                             in1=phh.t[:, 0:64], op0=ALU.mult, op1=ALU.add),
                     r=[Hf, phh] + egr, w=[Hb])
                P.op("dve", lambda e: e.scalar_tensor_tensor(out=Hf.t[:], in0=Hf.t[:], scalar=EGS.t[:, i:i + 1],
                                                             in1=phh.t[:, 0:64], op0=ALU.mult, op1=ALU.add),
                     r=[Hf, phh] + egr, w=[Hf])
                for d in range(2):
                    P.op("act", lambda e: e.activation(out=YTd[d].t[:, ck[d]], in_=py.t[0:64, d * 64:(d + 1) * 64],
                                                       func=AF.Identity), r=[py], w=[YTd[d]])
                yield

        def drive(gens):
            gens = list(gens)
            while gens:
                for g in list(gens):
                    try:
                        next(g)
                    except StopIteration:
                        gens.remove(g)

        nbt_ = len(batches)
        preps = {}
        prep_done = set()
        st_ = {"next_prep": 0, "next_chain": 0, "fin_chain": 0, "chain": None}
        e_pend = [0, 1, 4, 2, 3]
        e_act = []
        e_free = [0, 1]
        e_done = set()
        req_ = lambda k: {0} if k == 0 else ({0, 1, 4} if k < 3 else {0, 1, 2, 3, 4})
        while st_["fin_chain"] < nbt_:
            while e_pend and e_free:
                pr_ = e_free.pop(0)
                bi_ = e_pend.pop(0)
                e_act.append((gen_elem(bi_, pr_), pr_, bi_))
            while (st_["next_prep"] < nbt_ and len(preps) < 2 and st_["next_prep"] < st_["fin_chain"] + NSET
                   and req_(st_["next_prep"]) <= e_done):
                k_ = st_["next_prep"]
                preps[k_] = gen_prep(k_)
                st_["next_prep"] += 1
            if st_["chain"] is None and st_["next_chain"] in prep_done:
                st_["chain"] = gen_chain(st_["next_chain"])
                st_["next_chain"] += 1
            if st_["chain"] is not None:
                try:
                    next(st_["chain"])
                except StopIteration:
                    st_["chain"] = None
                    st_["fin_chain"] += 1
            for k_ in list(preps):
                try:
                    next(preps[k_])
                except StopIteration:
                    del preps[k_]
                    prep_done.add(k_)
            for it_ in list(e_act):
                try:
                    next(it_[0])
                except StopIteration:
                    e_act.remove(it_)
                    e_free.append(it_[1])
                    e_done.add(it_[2])
        if STOP <= 5:
            continue
        hp = slice((h % 2) * 64, (h % 2) * 64 + 64)
        o64 = K.bones.t[0:64, 0:64]
        def gen_out(bi, par_):
            sc_ = SCR[par_]
            S1, S2, S3 = sc_["s1"], sc_["s2"], sc_["s3"]
            for (n0, nb) in [BLKS[bi]]:
                blk = slice(n0, n0 + nb)
                yield
                p = K.ps()
                P.op("dve", lambda e: e.tensor_add(out=S3.t[0:64, :nb], in0=YTd[0].t[:, blk], in1=YTd[1].t[:, blk]),
                     r=[YTd[0], YTd[1]], w=[S3])
                P.op("pe", lambda e: e.matmul(p.t[0:64, :nb], lhsT=o64, rhs=S3.t[0:64, :nb], start=True, stop=True),
                     r=[K.bones, S3], w=[p])
                P.op("dve", lambda e: e.scalar_tensor_tensor(out=S1.t[0:64, :nb], in0=p.t[0:64, :nb], scalar=-1.0 / 64,
                                                             in1=S3.t[0:64, :nb], op0=ALU.mult, op1=ALU.add),
                     r=[p, S3], w=[S1])
                P.op("act", lambda e: e.activation(out=S2.t[0:64, :nb], in_=S1.t[0:64, :nb], func=AF.Square),
                     r=[S1], w=[S2])
                yield
                p = K.ps()
                P.op("pe", lambda e: e.matmul(p.t[0:64, :nb], lhsT=o64, rhs=S2.t[0:64, :nb], start=True, stop=True),
                     r=[K.bones, S2], w=[p])
                P.op("act", lambda e: e.activation(out=S2.t[0:64, :nb], in_=p.t[0:64, :nb], func=AF.Sqrt,
                                                   bias=GN_EPS, scale=1.0 / 64), r=[p], w=[S2])
                P.op("dve", lambda e: e.reciprocal(out=S2.t[0:64, :nb], in_=S2.t[0:64, :nb]), r=[S2], w=[S2])
                P.op("dve", lambda e: e.tensor_mul(out=S1.t[0:64, :nb], in0=S1.t[0:64, :nb], in1=S2.t[0:64, :nb]),
                     r=[S1, S2], w=[S1])
                P.op("act", lambda e: e.activation(out=S1.t[0:64, :nb], in_=S1.t[0:64, :nb], func=AF.Identity,
                                                   bias=prm(6, 0, 64), scale=prm(5, 0, 64)), r=[S1, RWP], w=[S1])
                P.op("dve", lambda e: e.tensor_mul(out=S3.t[0:64, :nb], in0=BS.t[:, blk], in1=VT.t[:, blk]),
                     r=[BS, VT], w=[S3])
                P.op("dve", lambda e: e.tensor_add(out=S1.t[0:64, :nb], in0=S1.t[0:64, :nb], in1=S3.t[0:64, :nb]),
                     r=[S1, S3], w=[S1])
                yield
                p = K.ps()
                P.op("pe", lambda e: e.matmul(p.t[0:64, :nb], lhsT=GLB.t[:, h * 64:(h + 1) * 64], rhs=SLG.t[:, blk],
                                              start=True, stop=True), r=[GLB, SLG], w=[p])
                P.op("dve", lambda e: e.tensor_mul(out=YA.t[hp, h // 2, blk], in0=S1.t[0:64, :nb], in1=p.t[0:64, :nb]),
                     r=[S1, p], w=[YA])
                yield

        pend_ = list(range(len(BLKS)))
        act2 = []
        free2 = [0, 1]
        while pend_ or act2:
            while pend_ and free2:
                pr_ = free2.pop(0)
                act2.append((gen_out(pend_.pop(0), pr_), pr_))
            for it_ in list(act2):
                try:
                    next(it_[0])
                except StopIteration:
                    act2.remove(it_)
                    free2.append(it_[1])


def phase_na(K, ph, YB):
    P, D, l, HT, sb = K.P, K.D, K.l, K.HT, K.sb

    def v3(ap, j=64):
        return ap.rearrange("p (c j) -> p c j", j=j)

    WVB = sb(ph, "wvb", [128, 8, 512], BF16)
    P.dma("pool", WVB.t[:], D["w_vb"][l], w=[WVB])
    NAP = sb(ph, "nap", [64, 2], F32)
    P.dma("sp", NAP.t[:], D["nap"][l], w=[NAP])
    P.op("dve", lambda e: e.tensor_scalar_mul(out=NAP.t[:, 0:1], in0=NAP.t[:, 0:1], scalar1=0.125), r=[NAP], w=[NAP])
    VAe = sb(ph, "vae", [128, 18, 8, 65], BF16)
    VAo = sb(ph, "vao", [128, 17, 8, 65], BF16)
    P.op("dve", lambda e: e.memset(VAe.t[:, :, :, 64:65], 1.0), w=[VAe])
    P.op("dve", lambda e: e.memset(VAo.t[:, :, :, 64:65], 1.0), w=[VAo])
    cnt = 0
    for (va, off, n_) in [(VAe, 0, 18), (VAo, 64, 17)]:
        for c in range(n_):
            p = K.ps()
            t0 = off + c * 128
            for kc in range(8):
                P.op("pe", lambda e: e.matmul(p.t[:, :], lhsT=HT.t[:, kc, t0:t0 + 128], rhs=WVB.t[:, kc, :],
                                              start=(kc == 0), stop=(kc == 7)), r=[HT, WVB], w=[p])
            if cnt % 2 == 0:
                P.op("act", lambda e: e.activation(out=va.t[:, c, :, 0:64], in_=v3(p.t[:, :]), func=AF.Identity),
                     r=[p], w=[va])
            else:
                P.op("dve", lambda e: e.tensor_copy(out=va.t[:, c, :, 0:64], in_=v3(p.t[:, :])), r=[p], w=[va])
            cnt += 1

    def vtile(ck):
        return (VAe, ck // 2) if ck % 2 == 0 else (VAo, (ck - 1) // 2)

    WQK = [sb(ph, f"wqk{i}", [128, 8, 128], BF16) for i in range(2)]
    BIAS = [[sb(ph, f"bias{i}{par}", [128, 7, 64], F32) for par in range(2)] for i in range(2)]
    QT = sb(ph, "qt", [64, NT], BF16)
    KT = sb(ph, "kt", [64, NT], BF16)
    SQ = [sb(ph, f"na_sq{i}", [64, 512], F32) for i in range(2)]
    RS = [sb(ph, f"na_rs{i}", [64, 512], F32) for i in range(2)]
    TM = [sb(ph, f"na_tm{i}", [64, 512], F32) for i in range(2)]
    SB_ = [sb(ph, f"na_sb{i}", [128, 4, 64], F32) for i in range(3)]
    PT = [sb(ph, f"na_pt{i}", [128, 6, 64], BF16) for i in range(3)]
    RD = sb(ph, "na_rd", [65, 512], F32)
    RB = sb(ph, "na_rb", [64, 512], F32)
    o64 = K.bones.t[0:64, 0:64]
    for h in range(8):
        wqk, bias = WQK[h % 2], BIAS[h % 2]
        hp = slice((h % 2) * 64, (h % 2) * 64 + 64)
        P.dma("pool", wqk.t[:], D["w_qk"][l, h], w=[wqk])
        for par in range(2):
            for e_ in range(2):
                s0 = par + e_
                P.dma("sp", bias[par].t[e_ * 64:(e_ + 1) * 64, :, :], D["rpbT"][l, h][:, s0:s0 + 13:2, :], w=[bias[par]])
            P.op("dve", lambda e: e.tensor_tensor(out=bias[par].t[:], in0=bias[par].t[:],
                                                  in1=K.namask.t[:].unsqueeze(1).to_broadcast([128, 7, 64]), op=ALU.add),
                 r=[bias[par], K.namask], w=[bias[par]])
        for (n0, nb) in BLKS:
            blk = slice(n0, n0 + nb)
            dsts = [QT, KT]
            pp = [K.ps(), K.ps()]
            for qi in range(2):
                for kc in range(8):
                    P.op("pe", lambda e: e.matmul(pp[qi].t[0:64, :nb], lhsT=wqk.t[:, kc, qi * 64:(qi + 1) * 64],
                                                  rhs=HT.t[:, kc, blk], start=(kc == 0), stop=(kc == 7)),
                         r=[wqk, HT], w=[pp[qi]])
            for qi in range(2):
                P.op("act", lambda e: e.activation(out=SQ[qi].t[:, :nb], in_=pp[qi].t[0:64, :nb], func=AF.Square),
                     r=[pp[qi]], w=[SQ[qi]])
            p2 = [K.ps(), K.ps()]
            for qi in range(2):
                P.op("pe", lambda e: e.matmul(p2[qi].t[0:64, :nb], lhsT=o64, rhs=SQ[qi].t[:, :nb], start=True, stop=True),
                     r=[K.bones, SQ[qi]], w=[p2[qi]])
            for qi in range(2):
                P.op("act", lambda e: e.activation(out=RS[qi].t[:, :nb], in_=p2[qi].t[0:64, :nb], func=AF.Sqrt,
                                                   bias=RMS_EPS, scale=1.0 / 64), r=[p2[qi]], w=[RS[qi]])
            for qi in range(2):
                P.op("dve", lambda e: e.reciprocal(out=RS[qi].t[:, :nb], in_=RS[qi].t[:, :nb]), r=[RS[qi]], w=[RS[qi]])
            for qi in range(2):
                P.op("dve", lambda e: e.tensor_mul(out=TM[qi].t[:, :nb], in0=pp[qi].t[0:64, :nb], in1=RS[qi].t[:, :nb]),
                     r=[pp[qi], RS[qi]], w=[TM[qi]])
            for qi in range(2):
                P.op("act", lambda e: e.activation(out=dsts[qi].t[:, blk], in_=TM[qi].t[:, :nb], func=AF.Identity,
                                                   scale=NAP.t[:, qi:qi + 1]), r=[TM[qi], NAP], w=[dsts[qi]])
        groups = [([0, 1, 2, 3], True)] + [(list(range(4 + g * 8, 12 + g * 8)), False) for g in range(4)]
        items = []
        for gi, (qcs, isctx) in enumerate(groups):
            for qi_, cq in enumerate(qcs):
                items.append((gi, qi_, cq, isctx, len(qcs), qcs[0]))
        POs = [K.PSL, K.PSL2]
        state = {}

        def emit_S(n):
            gi, qi_, cq, isctx, ng, q0 = items[n]
            qs = slice(cq * 64, cq * 64 + 64)
            pt = PT[n % 3]
            sbt = SB_[n % 3]
            kcs = []
            if not isctx:
                i = cq - 4
                r0 = min(max(i - 4, 0), 24)
                d0 = r0 - i + 7
                par, m0 = d0 % 2, d0 // 2
                pS = K.ps()
                for m in range(4):
                    ck = 4 + r0 + 2 * m
                    kcs.append(ck)
                    P.op("pe", lambda e: e.matmul(pS.t[:, m * 64:(m + 1) * 64], lhsT=KT.t[:, ck * 64:ck * 64 + 128],
                                                  rhs=QT.t[:, qs], start=True, stop=True), r=[KT, QT], w=[pS])
                P.op("dve", lambda e: e.tensor_tensor(out=sbt.t[:], in0=v3(pS.t[:, 0:256]), in1=bias[par].t[:, m0:m0 + 4, :],
                                                      op=ALU.add), r=[pS, bias[par]], w=[sbt])
                P.op("act", lambda e: e.activation(out=pt.t[:, 0:4, :], in_=sbt.t[:], func=AF.Exp), r=[sbt], w=[pt])
            pC = K.ps()
            for a in range(2):
                P.op("pe", lambda e: e.matmul(pC.t[:, a * 64:(a + 1) * 64], lhsT=KT.t[:, a * 128:(a + 1) * 128],
                                              rhs=QT.t[:, qs], start=True, stop=True), r=[KT, QT], w=[pC])
            P.op("act", lambda e: e.activation(out=pt.t[:, 4:6, :], in_=v3(pC.t[:, 0:128]), func=AF.Exp),
                 r=[pC], w=[pt])
            state[n] = kcs

        def emit_PV(n):
            gi, qi_, cq, isctx, ng, q0 = items[n]
            pt = PT[n % 3]
            po = POs[gi % 2]
            kcs = state.pop(n)
            keys = [(m, kcs[m]) for m in range(len(kcs))] + [(4, 0), (5, 2)]
            for n_, (slot, ck) in enumerate(keys):
                va, vi = vtile(ck)
                P.op("pe", lambda e: e.matmul(po.t[0:65, qi_ * 64:(qi_ + 1) * 64], lhsT=va.t[:, vi, h, :],
                                              rhs=pt.t[:, slot, :], start=(n_ == 0), stop=(n_ == len(keys) - 1)),
                     r=[va, pt], w=[po])
            if qi_ == ng - 1:
                W = ng * 64
                n0 = q0 * 64
                P.op("dve", lambda e: e.reciprocal(out=RD.t[64:65, :W], in_=po.t[64:65, :W]), r=[po], w=[RD])
                pb = K.ps()
                P.op("pe", lambda e: e.matmul(pb.t[0:64, :W], lhsT=K.onesf.t[64:65, 0:64], rhs=RD.t[64:65, :W],
                                              start=True, stop=True), r=[K.onesf, RD], w=[pb])
                P.op("act", lambda e: e.activation(out=RB.t[:, :W], in_=pb.t[0:64, :W], func=AF.Identity), r=[pb], w=[RB])
                P.op("dve", lambda e: e.tensor_mul(out=YB.t[hp, h // 2, n0:n0 + W], in0=po.t[0:64, :W], in1=RB.t[:, :W]),
                     r=[po, RB], w=[YB])

        emit_S(0)
        emit_S(1)
        for n in range(len(items)):
            if n + 2 < len(items):
                emit_S(n + 2)
            emit_PV(n)


def phase_merge(K, ph, YA, YB, xres):
    P, D, l, HT, sb = K.P, K.D, K.l, K.HT, K.sb
    WG = sb(ph, "wg", [128, 8, 2048], BF16)
    PA = sb(ph, "pa", [128, 4, 1024], BF16)
    PB = sb(ph, "pb", [128, 4, 1024], BF16)
    WO = sb(ph, "wo", [128, 8, 1024], BF16)
    for i in range(4):
        P.dma("pool", WG.t[:, :, i * 512:(i + 1) * 512], D["w_g"][l][:, :, i * 512:(i + 1) * 512], w=[WG])
    P.dma("pool", PA.t[:], D["pa"][l], w=[PA])
    P.dma("pool", PB.t[:], D["pb"][l], w=[PB])
    P.dma("pool", WO.t[:], D["wo"][l], w=[WO])
    MG = sb(ph, "mg", [128, 8, 512], BF16)
    XB = [sb(ph, f"xb{i}", [128, 8, 512], F32) for i in range(1)]
    SA = sb(ph, "m_sa", [128, 512], F32)
    SBt = sb(ph, "m_sb", [128, 512], F32)
    M1 = sb(ph, "m_m1", [128, 512], F32)
    M2 = sb(ph, "m_m2", [128, 512], F32)
    for bi, (n0, nb) in enumerate(BLKS):
        blk = slice(n0, n0 + nb)
        col = 1 if n0 == 0 else 0
        xb = XB[0]
        P.dma("sp", xb.t[:, :, :nb], xres[:, :, blk], w=[xb])
        for oc in range(8):
            ocs = slice(oc * 128, (oc + 1) * 128)
            pga, pgb, ppa, ppb = K.ps(), K.ps(), K.ps(), K.ps()
            for kc in range(8):
                P.op("pe", lambda e: e.matmul(pga.t[:, :nb], lhsT=WG.t[:, kc, oc * 128:(oc + 1) * 128],
                                              rhs=HT.t[:, kc, blk], start=(kc == 0), stop=(kc == 7)), r=[WG, HT], w=[pga])
            for kc in range(8):
                P.op("pe", lambda e: e.matmul(pgb.t[:, :nb], lhsT=WG.t[:, kc, 1024 + oc * 128:1024 + (oc + 1) * 128],
                                              rhs=HT.t[:, kc, blk], start=(kc == 0), stop=(kc == 7)), r=[WG, HT], w=[pgb])
            for kc in range(4):
                P.op("pe", lambda e: e.matmul(ppa.t[:, :nb], lhsT=PA.t[:, kc, ocs], rhs=YA.t[:, kc, blk],
                                              start=(kc == 0), stop=(kc == 3)), r=[PA, YA], w=[ppa])
            for kc in range(4):
                P.op("pe", lambda e: e.matmul(ppb.t[:, :nb], lhsT=PB.t[:, kc, ocs], rhs=YB.t[:, kc, blk],
                                              start=(kc == 0), stop=(kc == 3)), r=[PB, YB], w=[ppb])
            P.op("act", lambda e: e.activation(out=SA.t[:, :nb], in_=pga.t[:, :nb], func=AF.Sigmoid), r=[pga], w=[SA])
            P.op("act", lambda e: e.activation(out=SBt.t[:, :nb], in_=pgb.t[:, :nb], func=AF.Sigmoid), r=[pgb], w=[SBt])
            P.op("dve", lambda e: e.tensor_mul(out=M1.t[:, :nb], in0=SA.t[:, :nb], in1=ppa.t[:, :nb]), r=[SA, ppa], w=[M1])
            P.op("dve", lambda e: e.tensor_mul(out=M2.t[:, :nb], in0=SBt.t[:, :nb], in1=ppb.t[:, :nb]), r=[SBt, ppb], w=[M2])
            P.op("dve", lambda e: e.tensor_add(out=MG.t[:, oc, :nb], in0=M1.t[:, :nb], in1=M2.t[:, :nb]),
                 r=[M1, M2], w=[MG])
        for oc in range(8):
            po = K.ps()
            for kc in range(8):
                P.op("pe", lambda e: e.matmul(po.t[:, :nb], lhsT=WO.t[:, kc, oc * 128:(oc + 1) * 128],
                                              rhs=MG.t[:, kc, :nb], start=(kc == 0), stop=(kc == 7)), r=[WO, MG], w=[po])
            P.op("dve", lambda e: e.scalar_tensor_tensor(out=xb.t[:, oc, :nb], in0=po.t[:, :nb],
                                                         scalar=K.mt.t[:, 16 + oc, col:col + 1], in1=xb.t[:, oc, :nb],
                                                         op0=ALU.mult, op1=ALU.add), r=[po, K.mt, xb], w=[xb])
        P.dma("sp", xres[:, :, blk], xb.t[:, :, :nb], r=[xb], w=[Res()])


def phase_moe(K, ph, XT):
    P, D, l, HT, sb = K.P, K.D, K.l, K.HT, K.sb
    GT = sb(ph, "gt", [16, NT], BF16)
    K.sel16 = sb(ph, "sel16", [16, 16, 128], BF16)
    P.dma("pool", K.sel16.t[:], D["c_sel16"], w=[K.sel16])
    RT = [sb(ph, f"r_{n}", [128, 16], F32) for n in ["s", "sel", "msk", "num"]]
    R4 = [sb(ph, f"r4_{i}", [128, 4], F32) for i in range(10)]
    R1 = [sb(ph, f"r1_{i}", [128, 1], F32) for i in range(3)]
    GTM = sb(ph, "gtm", [128, 4, 16], F32)

    def router(n0, nb, HF):
        S, SEL, MSK, NUM = RT
        ntile = nb // 128
        for ti in range(ntile):
            pr = K.ps()
            for c in range(8):
                P.op("pe", lambda e: e.matmul(pr.t[:, 0:16], lhsT=HF.t[:, c, ti * 128:(ti + 1) * 128], rhs=K.rwt.t[:, c, :],
                                              start=(c == 0), stop=(c == 7)), r=[HF, K.rwt], w=[pr])
            P.op("act", lambda e: e.activation(out=S.t[:], in_=pr.t[:, 0:16], func=AF.Sigmoid), r=[pr], w=[S])
            P.op("dve", lambda e: e.tensor_add(out=SEL.t[:], in0=S.t[:], in1=K.rbias.t[:]), r=[S, K.rbias], w=[SEL])
            sv = SEL.t[:].rearrange("p (g j) -> p g j", j=4)
            a, b, c_, d_ = [sv[:, :, j] for j in range(4)]
            pq, qq, rr, ss, m1, t1, t2, m2, gs, gm = R4

            def tt(o, x, y, op, rd, wr):
                P.op("dve", lambda e: e.tensor_tensor(out=o, in0=x, in1=y, op=op), r=rd, w=wr)

            tt(pq.t[:], a, b, ALU.max, [SEL], [pq])
            tt(qq.t[:], a, b, ALU.min, [SEL], [qq])
            tt(rr.t[:], c_, d_, ALU.max, [SEL], [rr])
            tt(ss.t[:], c_, d_, ALU.min, [SEL], [ss])
            tt(m1.t[:], pq.t[:], rr.t[:], ALU.max, [pq, rr], [m1])
            tt(t1.t[:], pq.t[:], rr.t[:], ALU.min, [pq, rr], [t1])
            tt(t2.t[:], qq.t[:], ss.t[:], ALU.max, [qq, ss], [t2])
            tt(m2.t[:], t1.t[:], t2.t[:], ALU.max, [t1, t2], [m2])
            tt(gs.t[:], m1.t[:], m2.t[:], ALU.add, [m1, m2], [gs])
            gmax, den, rden = R1
            P.op("dve", lambda e: e.tensor_reduce(out=gmax.t[:], in_=gs.t[:], axis=AX.X, op=ALU.max), r=[gs], w=[gmax])
            P.op("dve", lambda e: e.tensor_scalar(out=gm.t[:], in0=gs.t[:], scalar1=gmax.t[:, 0:1], scalar2=None,
                                                  op0=ALU.is_ge), r=[gs, gmax], w=[gm])
            mv = MSK.t[:].rearrange("p (g j) -> p g j", j=4)
            P.op("dve", lambda e: e.tensor_tensor(out=mv, in0=sv, in1=m2.t[:].unsqueeze(2).to_broadcast([128, 4, 4]),
                                                  op=ALU.is_ge), r=[SEL, m2], w=[MSK])
            P.op("dve", lambda e: e.tensor_tensor(out=mv, in0=mv, in1=gm.t[:].unsqueeze(2).to_broadcast([128, 4, 4]),
                                                  op=ALU.mult), r=[MSK, gm], w=[MSK])
            tt(NUM.t[:], MSK.t[:], S.t[:], ALU.mult, [MSK, S], [NUM])
            P.op("dve", lambda e: e.tensor_reduce(out=den.t[:], in_=NUM.t[:], axis=AX.X, op=ALU.add), r=[NUM], w=[den])
            P.op("dve", lambda e: e.reciprocal(out=rden.t[:], in_=den.t[:]), r=[den], w=[rden])
            P.op("dve", lambda e: e.tensor_scalar_mul(out=GTM.t[:, ti, :], in0=NUM.t[:], scalar1=rden.t[:, 0:1]),
                 r=[NUM, rden], w=[GTM])
        pT = K.ps()
        for ti in range(ntile):
            P.op("pe", lambda e: e.transpose(pT.t[0:16, ti * 128:(ti + 1) * 128], GTM.t[:, ti, :], K.identf.t[:]),
                 r=[GTM, K.identf], w=[pT])
        P.op("act", lambda e: e.activation(out=GT.t[:, n0:n0 + nb], in_=pT.t[0:16, :nb], func=AF.Identity), r=[pT], w=[GT])

    with ExitStack() as nph:
        phase_norm(K, nph, XT, K.mul2, 24, router)
        P.barrier()
    W1 = [sb(ph, f"w1_{i}", [128, 8, 512], BF16) for i in range(2)]
    W3 = [sb(ph, f"w3_{i}", [128, 8, 512], BF16) for i in range(2)]
    W2 = [sb(ph, f"w2_{i}", [128, 4, 1024], BF16) for i in range(2)]
    GB = sb(ph, "gb", [128, 512], F32)
    SL = [sb(ph, f"sl{i}", [128, 512], F32) for i in range(2)]
    T2 = [sb(ph, f"t2{i}", [128, 512], F32) for i in range(2)]
    HG = sb(ph, "hg", [128, 4, 512], BF16)

    def load(e):
        P.dma("pool", W1[e % 2].t[:], D["w1"][l, e], w=[W1[e % 2]])
        P.dma("pool", W3[e % 2].t[:], D["w3"][l, e], w=[W3[e % 2]])
        P.dma("pool", W2[e % 2].t[:], D["w2"][l, e], w=[W2[e % 2]])

    import os as _os
    NOLOAD = _os.environ.get("MOE_NOLOAD") == "1"
    load(0)
    for ex in range(16):
        if ex + 1 < 16 and not (NOLOAD and ex >= 1):
            load(ex + 1)
        w1, w3, w2 = W1[ex % 2], W3[ex % 2], W2[ex % 2]
        for (n0, nb) in BLKS:
            blk = slice(n0, n0 + nb)
            col = 1 if n0 == 0 else 0
            pg = K.ps()
            P.op("pe", lambda e: e.matmul(pg.t[:, :nb], lhsT=K.sel16.t[:, ex, :], rhs=GT.t[:, blk], start=True, stop=True),
                 r=[K.sel16, GT], w=[pg])
            P.op("act", lambda e: e.activation(out=GB.t[:, :nb], in_=pg.t[:, :nb], func=AF.Identity), r=[pg], w=[GB])
            for hc in range(4):
                hs = slice(hc * 128, (hc + 1) * 128)
                p1, p3 = K.ps(), K.ps()
                for kc in range(8):
                    P.op("pe", lambda e: e.matmul(p1.t[:, :nb], lhsT=w1.t[:, kc, hs], rhs=HT.t[:, kc, blk],
                                                  start=(kc == 0), stop=(kc == 7)), r=[w1, HT], w=[p1])
                for kc in range(8):
                    P.op("pe", lambda e: e.matmul(p3.t[:, :nb], lhsT=w3.t[:, kc, hs], rhs=HT.t[:, kc, blk],
                                                  start=(kc == 0), stop=(kc == 7)), r=[w3, HT], w=[p3])
                sl, t2 = SL[hc % 2], T2[hc % 2]
                P.op("act", lambda e: e.activation(out=sl.t[:, :nb], in_=p1.t[:, :nb], func=AF.Silu), r=[p1], w=[sl])
                P.op("dve", lambda e: e.tensor_mul(out=t2.t[:, :nb], in0=sl.t[:, :nb], in1=p3.t[:, :nb]), r=[sl, p3], w=[t2])
                P.op("dve", lambda e: e.tensor_mul(out=HG.t[:, hc, :nb], in0=t2.t[:, :nb], in1=GB.t[:, :nb]),
                     r=[t2, GB], w=[HG])
            for oc in range(8):
                po = K.ps()
                for hc in range(4):
                    P.op("pe", lambda e: e.matmul(po.t[:, :nb], lhsT=w2.t[:, hc, oc * 128:(oc + 1) * 128],
                                                  rhs=HG.t[:, hc, :nb], start=(hc == 0), stop=(hc == 3)), r=[w2, HG], w=[po])
                P.op("dve", lambda e: e.scalar_tensor_tensor(out=XT.t[:, oc, blk], in0=po.t[:, :nb],
                                                             scalar=K.mt.t[:, 40 + oc, col:col + 1], in1=XT.t[:, oc, blk],
                                                             op0=ALU.mult, op1=ALU.add), r=[po, K.mt, XT], w=[XT])


def kernel(**inputs):
    maps = _prep(inputs)
    nc = build()
    res = run_bass_kernel_spmd(nc, maps, core_ids=list(range(8)))
    out = np.zeros((8, 2048, 1024), np.float32)
    for b in range(8):
        y = res.results[b]["yout"]
        out[b] = y.transpose(2, 1, 0).reshape(2048, 1024)
    return out
```
